# Optimizing a Trainium2 kernel written in Bass

```python
import jax, jax.numpy as jnp
from jax import lax
import numpy as np

D_MODEL = 2048
BATCH = 8
SEQ = 4096
DEPTH = 1
DEC_BATCH = 8
DEC_SEQ = 64
PAST_LEN = 1024

CHUNK = 64
HEAD_DIM = 128
N_FOX_HEADS = 8
N_SB_HEADS = 8
FOX_WIDTH = N_FOX_HEADS * HEAD_DIM
SB_WIDTH = N_SB_HEADS * HEAD_DIM
MIX_WIDTH = FOX_WIDTH + SB_WIDTH
IN_COLS = 3 * FOX_WIDTH + N_FOX_HEADS + 3 * SB_WIDTH
Q_BLOCK = 128
N_GROUPS = 4
EXPERTS_PER_GROUP = 8
N_EXPERTS = N_GROUPS * EXPERTS_PER_GROUP
TOP_K = 2
D_EXPERT = 1024
MOE_BLOCK = 128
FORGET_BIAS_INIT = 3.0
EPS = 1e-6

kernel_name = "fox_stickbreak_hier_moe_stream_step"


def _rmsnorm(x, g):
    xf = x.astype(jnp.float32)
    y = xf * lax.rsqrt(jnp.mean(xf * xf, axis=-1, keepdims=True) + EPS)
    return (y * g.astype(jnp.float32)).astype(x.dtype)


def _project(hn, w_in, b_f):
    B, T, _ = hn.shape
    p = hn @ w_in
    splits = [FOX_WIDTH, 2 * FOX_WIDTH, 3 * FOX_WIDTH, 3 * FOX_WIDTH + N_FOX_HEADS,
              3 * FOX_WIDTH + N_FOX_HEADS + SB_WIDTH, 3 * FOX_WIDTH + N_FOX_HEADS + 2 * SB_WIDTH]
    qf, kf, vf, fl, qs, ks, vs = jnp.split(p, splits, axis=-1)
    hf = lambda a: a.reshape(B, T, N_FOX_HEADS, HEAD_DIM)
    hs = lambda a: a.reshape(B, T, N_SB_HEADS, HEAD_DIM)
    logf = jax.nn.log_sigmoid(fl.astype(jnp.float32) + b_f.astype(jnp.float32))
    return hf(qf), hf(kf), hf(vf), logf, hs(qs), hs(ks), hs(vs)


def _fox_block(q, cq, qpos, k, v, ck, kpos):
    s = jnp.einsum('bqhd,bkhd->bhqk', q, k).astype(jnp.float32) * (HEAD_DIM ** -0.5)
    s = s + jnp.swapaxes(cq, 1, 2)[:, :, :, None] - jnp.swapaxes(ck, 1, 2)[:, :, None, :]
    mask = kpos[None, :] <= qpos[:, None]
    p = jax.nn.softmax(jnp.where(mask, s, -jnp.inf), axis=-1)
    return jnp.einsum('bhqk,bkhd->bqhd', p.astype(v.dtype), v)


def _sb_block(q, qpos, k, v, kpos):
    z = jnp.einsum('bqhd,bkhd->bhqk', q, k).astype(jnp.float32) * (HEAD_DIM ** -0.5)
    valid = kpos[None, :] < qpos[:, None]
    l = jnp.where(valid, jax.nn.log_sigmoid(-z), 0.0)
    suffix = lax.cumsum(l, axis=3, reverse=True) - l
    a = jnp.where(valid, jnp.exp(jax.nn.log_sigmoid(z) + suffix), 0.0)
    return jnp.einsum('bhqk,bkhd->bqhd', a.astype(v.dtype), v)


def _to_blocks(a):
    B, S = a.shape[:2]
    return jnp.swapaxes(a.reshape((B, S // Q_BLOCK, Q_BLOCK) + a.shape[2:]), 0, 1)


def _from_blocks(a):
    nb, B, qb = a.shape[:3]
    return jnp.swapaxes(a, 0, 1).reshape((B, nb * qb) + a.shape[3:])


def _prompt_attention(qf, kf, vf, cf, qs, ks, vs):
    S = qf.shape[1]
    pos = jnp.arange(S, dtype=jnp.int32)
    pos_b = pos.reshape(S // Q_BLOCK, Q_BLOCK)
    o_f = lax.map(lambda a: _fox_block(a[0], a[1], a[2], kf, vf, cf, pos),
                  (_to_blocks(qf), _to_blocks(cf), pos_b))
    o_s = lax.map(lambda a: _sb_block(a[0], a[1], ks, vs, pos), (_to_blocks(qs), pos_b))
    return _from_blocks(o_f), _from_blocks(o_s)


def _merge(o_f, o_s, g_of, g_os, w_out):
    B, T = o_f.shape[:2]
    nf = _rmsnorm(o_f, g_of.reshape(N_FOX_HEADS, HEAD_DIM)).reshape(B, T, FOX_WIDTH)
    ns = _rmsnorm(o_s, g_os.reshape(N_SB_HEADS, HEAD_DIM)).reshape(B, T, SB_WIDTH)
    return jnp.concatenate([nf, ns], axis=-1) @ w_out


def _hier_moe(x, w_group, b_group, w_router, b_router, w1, w3, w2):
    B, T, D = x.shape
    h = x.reshape(B * T, D)
    N = B * T
    g_logits = (h @ w_group).astype(jnp.float32) + b_group.astype(jnp.float32)
    g_sel = jnp.argmax(g_logits, axis=-1)
    p_g = jnp.take_along_axis(jax.nn.softmax(g_logits, axis=-1), g_sel[:, None], axis=1)[:, 0]
    e_logits = ((h @ w_router).astype(jnp.float32) + b_router.astype(jnp.float32)).reshape(N, N_GROUPS, EXPERTS_PER_GROUP)
    in_grp = jnp.take_along_axis(e_logits, g_sel[:, None, None], axis=1)[:, 0]
    top_p, top_i = lax.top_k(jax.nn.softmax(in_grp, axis=-1), TOP_K)
    gates = p_g[:, None] * top_p / jnp.sum(top_p, axis=-1, keepdims=True)
    expert = g_sel[:, None].astype(jnp.int32) * EXPERTS_PER_GROUP + top_i.astype(jnp.int32)
    M = N * TOP_K
    flat_e = expert.reshape(M)
    flat_tok = jnp.repeat(jnp.arange(N, dtype=jnp.int32), TOP_K)
    flat_g = gates.reshape(M)
    order = jnp.argsort(flat_e)
    se, stok, sg = flat_e[order], flat_tok[order], flat_g[order]
    counts = jnp.bincount(flat_e, length=N_EXPERTS).astype(jnp.int32)
    start = jnp.cumsum(counts) - counts
    padded = (counts + MOE_BLOCK - 1) // MOE_BLOCK * MOE_BLOCK
    pend = jnp.cumsum(padded)
    pstart = pend - padded
    dest = pstart[se] + (jnp.arange(M, dtype=jnp.int32) - start[se])
    n_blocks = -(-(M + N_EXPERTS * (MOE_BLOCK - 1)) // MOE_BLOCK)
    P = n_blocks * MOE_BLOCK
    row_tok = jnp.zeros((P,), jnp.int32).at[dest].set(stok)
    row_gate = jnp.zeros((P,), h.dtype).at[dest].set(sg.astype(h.dtype))
    block_start = jnp.arange(n_blocks, dtype=jnp.int32) * MOE_BLOCK
    block_expert = jnp.minimum(jnp.searchsorted(pend, block_start, side='right'), N_EXPERTS - 1)
    xs = h[row_tok].reshape(n_blocks, MOE_BLOCK, D)

    def _expert(args):
        xb, e = args
        return (jax.nn.silu(xb @ w1[e]) * (xb @ w3[e])) @ w2[e]

    out = lax.map(_expert, (xs, block_expert)).reshape(P, D)
    y = jnp.zeros((N, D), h.dtype).at[row_tok].add(out * row_gate[:, None])
    return y.reshape(B, T, D)


def setup_inputs(seed: int = 0) -> dict:
    key = jax.random.key(seed)
    k = jax.random.split(key, 24)
    nrm = lambda kk, shape, scale: scale * jax.random.normal(kk, shape, jnp.float32)
    fox_shape = (DEPTH, DEC_BATCH, PAST_LEN, N_FOX_HEADS, HEAD_DIM)
    sb_shape = (DEPTH, DEC_BATCH, PAST_LEN, N_SB_HEADS, HEAD_DIM)
    return {
        "x_prompt": nrm(k[0], (BATCH, SEQ, D_MODEL), 1.0),
        "x_sample": nrm(k[1], (DEC_BATCH, DEC_SEQ, D_MODEL), 1.0),
        "cache_fox_k": nrm(k[2], fox_shape, 1.0),
        "cache_fox_v": nrm(k[3], fox_shape, 1.0),
        "cache_fox_logf": jax.nn.log_sigmoid(FORGET_BIAS_INIT + nrm(k[4], (DEPTH, DEC_BATCH, PAST_LEN, N_FOX_HEADS), 1.0)),
        "cache_sb_k": nrm(k[5], sb_shape, 1.0),
        "cache_sb_v": nrm(k[6], sb_shape, 1.0),
        "w_in": nrm(k[7], (DEPTH, D_MODEL, IN_COLS), D_MODEL ** -0.5),
        "b_f": FORGET_BIAS_INIT + nrm(k[8], (DEPTH, N_FOX_HEADS), 0.5),
        "g_attn": 1.0 + nrm(k[9], (DEPTH, D_MODEL), 0.1),
        "g_out_fox": 1.0 + nrm(k[10], (DEPTH, FOX_WIDTH), 0.1),
        "g_out_sb": 1.0 + nrm(k[11], (DEPTH, SB_WIDTH), 0.1),
        "w_out": nrm(k[12], (DEPTH, MIX_WIDTH, D_MODEL), MIX_WIDTH ** -0.5),
        "g_ffn": 1.0 + nrm(k[13], (DEPTH, D_MODEL), 0.1),
        "w_group": nrm(k[14], (DEPTH, D_MODEL, N_GROUPS), D_MODEL ** -0.5),
        "b_group": nrm(k[15], (DEPTH, N_GROUPS), 0.01),
        "w_router": nrm(k[16], (DEPTH, D_MODEL, N_EXPERTS), D_MODEL ** -0.5),
        "b_router": nrm(k[17], (DEPTH, N_EXPERTS), 0.01),
        "w1": nrm(k[18], (DEPTH, N_EXPERTS, D_MODEL, D_EXPERT), D_MODEL ** -0.5),
        "w3": nrm(k[19], (DEPTH, N_EXPERTS, D_MODEL, D_EXPERT), D_MODEL ** -0.5),
        "w2": nrm(k[20], (DEPTH, N_EXPERTS, D_EXPERT, D_MODEL), D_EXPERT ** -0.5),
        "g_final": 1.0 + nrm(k[21], (D_MODEL,), 0.1),
    }


def reference(x_prompt, x_sample, cache_fox_k, cache_fox_v, cache_fox_logf, cache_sb_k, cache_sb_v,
              w_in, b_f, g_attn, g_out_fox, g_out_sb, w_out, g_ffn, w_group, b_group, w_router, b_router,
              w1, w3, w2, g_final):
    hp, hs = x_prompt, x_sample
    past = cache_fox_logf.shape[2]
    T = hs.shape[1]
    qpos_s = past + jnp.arange(T, dtype=jnp.int32)
    kpos_s = jnp.arange(past + T, dtype=jnp.int32)
    p_fk, p_fv, p_fl, p_sk, p_sv = [], [], [], [], []
    s_fk, s_fv, s_fl, s_sk, s_sv = [], [], [], [], []
    for l in range(DEPTH):
        qf, kf, vf, logf, qs, ks, vs = _project(_rmsnorm(hp, g_attn[l]), w_in[l], b_f[l])
        cf = jnp.cumsum(logf, axis=1)
        o_f, o_s = _prompt_attention(qf, kf, vf, cf, qs, ks, vs)
        hp = hp + _merge(o_f, o_s, g_out_fox[l], g_out_sb[l], w_out[l])
        hp = hp + _hier_moe(_rmsnorm(hp, g_ffn[l]), w_group[l], b_group[l], w_router[l], b_router[l], w1[l], w3[l], w2[l])
        p_fk.append(kf); p_fv.append(vf); p_fl.append(logf); p_sk.append(ks); p_sv.append(vs)
        qf2, kf2, vf2, logf2, qs2, ks2, vs2 = _project(_rmsnorm(hs, g_attn[l]), w_in[l], b_f[l])
        Kf = jnp.concatenate([cache_fox_k[l].astype(kf2.dtype), kf2], axis=1)
        Vf = jnp.concatenate([cache_fox_v[l].astype(vf2.dtype), vf2], axis=1)
        cf_all = jnp.cumsum(jnp.concatenate([cache_fox_logf[l].astype(jnp.float32), logf2], axis=1), axis=1)
        Ks = jnp.concatenate([cache_sb_k[l].astype(ks2.dtype), ks2], axis=1)
        Vs = jnp.concatenate([cache_sb_v[l].astype(vs2.dtype), vs2], axis=1)
        o_f2 = _fox_block(qf2, cf_all[:, past:], qpos_s, Kf, Vf, cf_all, kpos_s)
        o_s2 = _sb_block(qs2, qpos_s, Ks, Vs, kpos_s)
        hs = hs + _merge(o_f2, o_s2, g_out_fox[l], g_out_sb[l], w_out[l])
        hs = hs + _hier_moe(_rmsnorm(hs, g_ffn[l]), w_group[l], b_group[l], w_router[l], b_router[l], w1[l], w3[l], w2[l])
        s_fk.append(kf2); s_fv.append(vf2); s_fl.append(logf2); s_sk.append(ks2); s_sv.append(vs2)
    y_prompt = _rmsnorm(hp, g_final)
    y_sample = _rmsnorm(hs, g_final)
    return (y_prompt, y_sample,
            jnp.stack(p_fk), jnp.stack(p_fv), jnp.stack(p_fl), jnp.stack(p_sk), jnp.stack(p_sv),
            jnp.stack(s_fk), jnp.stack(s_fv), jnp.stack(s_fl), jnp.stack(s_sk), jnp.stack(s_sv))
```

```python
import contextlib
import numpy as np
import ml_dtypes
import concourse.bass as bass
import concourse.mybir as mybir
from concourse.bass_utils import run_bass_kernel_spmd

F32 = mybir.dt.float32
BF16 = mybir.dt.bfloat16
I32 = mybir.dt.int32
AF = mybir.ActivationFunctionType
ALU = mybir.AluOpType
AX = mybir.AxisListType

D = 2048
DC = D // 128
HD = 128
NH = 16
NFH = 8
IN_COLS = 6152
EPS = 1e-6
DEXP = 1024


def default_cfg():
    return dict(S=4096, T=64, PAST=1024, NG=4, EPG=8, CAP=384, debug=False, stop_after=99)


class Buf:
    __slots__ = ("name", "t", "w", "rs", "sem", "cnt", "is_dram", "is_psum")

    def __init__(self, name, t, is_dram=False, is_psum=False):
        self.is_psum = is_psum
        self.name = name
        self.t = t
        self.w = None
        self.rs = {}
        self.sem = None
        self.cnt = 0
        self.is_dram = is_dram


EPOCH = 30000


class KB:
    COMPUTE = ("pe", "act", "dve", "pool")

    def __init__(self, nc):
        self.nc = nc
        self.eng = {"pe": nc.tensor, "act": nc.scalar, "dve": nc.vector, "pool": nc.gpsimd, "sp": nc.sync}
        self.cnt = {e: 0 for e in self.COMPUTE}
        self.seen = {}
        self.outer = contextlib.ExitStack()
        self.phase_stack = None
        self.sems = {}
        self.dma_toks = []
        self.nbuf = 0
        self.pbufs = []
        self.limit = 10 ** 9
        self.nops = 0
        self.trace = None
        self.free_sems = []
        self.phase_sem_bufs = []
        for e in self.COMPUTE:
            for ep in range(4):
                self._csem(e, ep)

    def _ctx(self, persistent):
        return self.outer if persistent else self.phase_stack

    def sb(self, name, shape, dt, persistent=False):
        t = self._ctx(persistent).enter_context(self.nc.sbuf_tensor(name, list(shape), dt))
        b = Buf(name, t)
        if persistent:
            self.pbufs.append(b)
        return b

    def ps(self, name, shape, dt, persistent=False):
        t = self._ctx(persistent).enter_context(self.nc.psum_tensor(name, list(shape), dt))
        return Buf(name, t, is_psum=True)

    def dram(self, name, t):
        b = Buf(name, t, is_dram=True)
        self.pbufs.append(b)
        return b

    def _sem(self, name, persistent=False):
        return self.outer.enter_context(self.nc.semaphore(name))

    def _dma_sem(self, b):
        if self.free_sems:
            sem, cnt = self.free_sems.pop(0)
        else:
            sem, cnt = self._sem(f"d_{self.nbuf}"), 0
            self.nbuf += 1
        b.sem = sem
        b.cnt = cnt
        self.phase_sem_bufs.append(b)

    def _csem(self, eng, epoch):
        k = (eng, epoch)
        if k not in self.sems:
            self.sems[k] = self._sem(f"c_{eng}_{epoch}", persistent=True)
        return self.sems[k]

    def _wait(self, eng, toks):
        for tok in toks:
            if tok is None:
                continue
            kind = tok[0]
            if kind == "c":
                _, src, epoch, val = tok
                if src == eng and eng == "pe":
                    continue
                key = ("c", src, epoch)
                sem = self._csem(src, epoch)
            else:
                _, sem, val, semid = tok
                key = ("d", semid)
            if self.seen.get((eng, key), 0) >= val:
                continue
            self.seen[(eng, key)] = val
            self.eng[eng].wait_ge(sem, val)

    def _deps(self, reads, writes):
        deps = []
        for b in reads:
            if b.w is not None:
                deps.append(b.w)
            if b.is_psum:
                deps.extend(b.rs.values())
        for b in writes:
            if b.w is not None:
                deps.append(b.w)
            deps.extend(b.rs.values())
        return deps

    @staticmethod
    def _tkey(tok):
        return tok[:3] if tok[0] == "c" else ("d", tok[3])

    def _commit(self, tok, reads, writes):
        k = self._tkey(tok)
        for b in reads:
            old = b.rs.get(k)
            if old is None or old[-1 if tok[0] == "c" else 2] <= tok[-1 if tok[0] == "c" else 2]:
                b.rs[k] = tok
        for b in writes:
            b.w = tok
            b.rs = {}

    def op(self, eng, fn, reads=(), writes=(), signal=True, extra=()):
        self.nops += 1
        if self.nops > self.limit:
            return None
        if self.trace is not None:
            import sys as _s
            f = _s._getframe(1)
            self.trace.append((self.nops, eng, f.f_lineno, f.f_code.co_name))
        self._wait(eng, self._deps(reads, writes) + list(extra))
        inst = fn()
        n = self.cnt[eng]
        if signal:
            n += 1
            self.cnt[eng] = n
            epoch, val = divmod(n - 1, EPOCH)
            inst.then_inc(self._csem(eng, epoch), 1)
            tok = ("c", eng, epoch, val + 1)
        else:
            epoch, val = divmod(n, EPOCH)
            tok = ("c", eng, epoch, val + 1)
        self._commit(tok, reads, writes)
        return tok

    def dma(self, q, out_ap, in_ap, reads=(), writes=(), fn=None, extra=()):
        self.nops += 1
        if self.nops > self.limit:
            return None
        if self.trace is not None:
            import sys as _s
            f = _s._getframe(1)
            self.trace.append((self.nops, "dma-" + q, f.f_lineno, f.f_code.co_name))
        self._wait(q, self._deps(reads, writes) + list(extra))
        cands = [b for b in list(writes) + list(reads) if not b.is_dram]
        b = cands[0] if cands else (list(writes) + list(reads))[0]
        if b.sem is None:
            self._dma_sem(b)
        sem = b.sem
        b.cnt += 16
        val = b.cnt
        if fn is None:
            inst = self.eng[q].dma_start(out=out_ap, in_=in_ap)
        else:
            inst = fn()
        inst.then_inc(sem, 16)
        tok = ("d", sem, val, id(sem))
        self.dma_toks.append(tok)
        self._commit(tok, reads, writes)
        return tok

    def begin_phase(self):
        self.phase_stack = contextlib.ExitStack()
        self.dma_toks = []

    def end_phase(self):
        toks = list(self.dma_toks)
        for e in self.COMPUTE:
            n = self.cnt[e]
            if n > 0:
                epoch, val = divmod(n - 1, EPOCH)
                toks.append(("c", e, epoch, val + 1))
        for e in ("pe", "act", "dve", "pool", "sp"):
            if self.nops <= self.limit:
                self._wait(e, toks)
        self.phase_stack.close()
        self.phase_stack = None
        self.dma_toks = []
        for b in self.phase_sem_bufs:
            self.free_sems.append((b.sem, b.cnt))
            b.sem = None
            b.cnt = 0
        self.phase_sem_bufs = []
        for b in self.pbufs:
            b.sem = None
            b.cnt = 0
            b.w = None
            b.rs = {}

    def finish(self):
        self.outer.close()


def make_consts(cfg):
    bf = ml_dtypes.bfloat16
    p = np.arange(128)
    c = {}
    c["ident_bf"] = np.eye(128, dtype=np.float32).astype(bf)
    c["ident_f"] = np.eye(128, dtype=np.float32)
    c["ones_f"] = np.ones((128, 128), np.float32)
    c["ones_bf"] = np.ones((128, 128), np.float32).astype(bf)
    c["tri_incl_f"] = (p[:, None] <= p[None, :]).astype(np.float32)
    c["tri_strict_f"] = (p[:, None] < p[None, :]).astype(np.float32)
    c["sel0_f"] = np.zeros((128, 128), np.float32)
    c["sel0_f"][0, :] = 1.0
    q = np.arange(512)
    mf = np.zeros((128, 4, 512), np.float32)
    ms = np.zeros((128, 4, 512), np.float32)
    for r in range(4):
        mf[:, r, :] = (q[None, :] >= 128 * r + p[:, None])
        ms[:, r, :] = (q[None, :] > 128 * r + p[:, None])
    c["mask_fox"] = mf.astype(bf)
    c["mask_sb"] = ms.astype(bf)
    c["negtri_bf"] = (-(p[:, None] >= p[None, :]).astype(np.float32)).astype(bf)
    c["negcompl_bf"] = (-(p[:, None] < p[None, :]).astype(np.float32)).astype(bf)
    NE = cfg["NG"] * cfg["EPG"]
    c["ebase_f"] = np.tile((np.arange(NE, dtype=np.float32) * cfg["CAP"])[None, :], (128, 1))
    c["eidx_f"] = np.tile(np.arange(NE, dtype=np.float32)[None, :], (128, 1))
    c["tri_strict_bf"] = c["tri_strict_f"].astype(bf)
    rvv = (p < cfg["T"]).astype(np.float32)
    c["rowvalid"] = rvv[:, None].copy()
    c["trash_p"] = ((1.0 - rvv) * (NE * cfg["CAP"] + p))[:, None].astype(np.float32)
    return c


CONST_DT = {"tri_strict_bf": BF16, "ident_bf": BF16, "ones_bf": BF16, "mask_fox": BF16, "mask_sb": BF16, "negtri_bf": BF16,
            "negcompl_bf": BF16}


def build(cfg):
    S, T, PAST = cfg["S"], cfg["T"], cfg["PAST"]
    NG, EPG, CAP = cfg["NG"], cfg["EPG"], cfg["CAP"]
    NE = NG * EPG
    NT = S // 128
    NPT = PAST // 128
    NTS = NPT + 1
    SS = NTS * 128
    NTOK_T = NT + 1
    debug = cfg["debug"]
    stop_after = cfg["stop_after"]

    nc = bass.Bass("TRN2", target_bir_lowering=False)
    kb = KB(nc)
    kb.limit = cfg.get("limit", 10 ** 9)
    global LAST_KB
    LAST_KB = kb
    if cfg.get("trace"):
        kb.trace = []

    def din(name, shape, dt=F32):
        return nc.dram_tensor(name, list(shape), dt, kind="ExternalInput").ap()

    def dout(name, shape, dt=F32):
        return nc.dram_tensor(name, list(shape), dt, kind="ExternalOutput").ap()

    def dscr(name, shape, dt):
        kind = "ExternalOutput" if debug else "Internal"
        return nc.dram_tensor(name, list(shape), dt, kind=kind).ap()

    x_p = din("x_p", [S, D])
    x_s = din("x_s", [T, D])
    c_fk = din("c_fk", [PAST, 1024])
    c_fv = din("c_fv", [PAST, 1024])
    c_fl = din("c_fl", [PAST, 8])
    c_sk = din("c_sk", [PAST, 1024])
    c_sv = din("c_sv", [PAST, 1024])
    w_in = din("w_in", [D, IN_COLS])
    b_f = din("b_f", [1, 8])
    g_attn = din("g_attn", [1, D])
    g_of = din("g_of", [1, 1024])
    g_os = din("g_os", [1, 1024])
    w_out = din("w_out", [D, D])
    g_ffn = din("g_ffn", [1, D])
    w_rt = din("w_rt", [D, NG + NE])
    b_rt = din("b_rt", [1, NG + NE])
    w1 = din("w1", [NE, D, DEXP])
    w3 = din("w3", [NE, D, DEXP])
    w2 = din("w2", [NE, DEXP, D])
    g_fin = din("g_fin", [1, D])
    consts = make_consts(cfg)
    cin = {k: din("k_" + k, v.shape, CONST_DT.get(k, F32)) for k, v in consts.items()}

    y_p = dout("y_p", [S, D])
    y_s = dout("y_s", [T, D])
    o_pfk = dout("o_pfk", [S, 1024]); o_pfv = dout("o_pfv", [S, 1024]); o_pfl = dout("o_pfl", [S, 8])
    o_psk = dout("o_psk", [S, 1024]); o_psv = dout("o_psv", [S, 1024])
    o_sfk = dout("o_sfk", [T, 1024]); o_sfv = dout("o_sfv", [T, 1024]); o_sfl = dout("o_sfl", [T, 8])
    o_ssk = dout("o_ssk", [T, 1024]); o_ssv = dout("o_ssv", [T, 1024])

    o_cnt = dout("o_cnt", [128, NE])
    w_in_bf = dscr("w_in_bf", [D, IN_COLS], BF16) if False else nc.dram_tensor("w_in_bf", [D, IN_COLS], BF16, kind="Internal").ap()
    QT = {"p": dscr("QT_p", [NH, 128, S], BF16), "s": dscr("QT_s", [NH, 128, 128], BF16)}
    KT = {"p": dscr("KT_p", [NH, 128, S], BF16), "s": dscr("KT_s", [NH, 128, SS], BF16)}
    VV = {"p": dscr("VV_p", [S, D], BF16), "s": dscr("VV_s", [SS, D], BF16)}
    NFT = dscr("NFT", [D, NTOK_T * 128], BF16)
    Hs = dscr("Hs", [NTOK_T * 128, D], F32)
    w1_bf = nc.dram_tensor("w1_bf", [NE, D, DEXP], BF16, kind="Internal").ap()
    w3_bf = nc.dram_tensor("w3_bf", [NE, D, DEXP], BF16, kind="Internal").ap()
    w2_bf = nc.dram_tensor("w2_bf", [NE, DEXP, D], BF16, kind="Internal").ap()
    NSLOT = NE * CAP
    XS = dscr("XS", [NSLOT + 128, D], BF16)
    YS = dscr("YS", [NSLOT + 128, D], BF16)

    E = kb.eng
    pe, act, dve, pool, sp = E["pe"], E["act"], E["dve"], E["pool"], E["sp"]

    kb.begin_phase()
    C = {}
    for k, v in consts.items():
        C[k] = kb.sb("c_" + k, v.shape, CONST_DT.get(k, F32), persistent=True)
        kb.dma("sp", C[k].t[:], cin[k][:], writes=[C[k]])
    NTA = NT + NTS
    LF = kb.sb("LF", [128, NTA, 8], F32, persistent=True)
    CB = kb.sb("CB", [128, NTA, 8], F32, persistent=True)
    CREF = kb.sb("CREF", [128, NTA, 8], F32, persistent=True)
    bf_bc = kb.sb("bf_bc", [128, 8], F32, persistent=True)

    def bcast_row(ap_row, n):
        return ap_row.broadcast_to([128, n])

    kb.dma("sp", bf_bc.t[:], bcast_row(b_f[0:1, :], 8), writes=[bf_bc])


    g_bc = kb.sb("g_bc", [128, D], F32)
    kb.dma("sp", g_bc.t[:], bcast_row(g_attn[0:1, :], D), writes=[g_bc])
    xt = [kb.sb(f"xt{i}", [128, D], F32) for i in range(2)]
    sq_junk = kb.sb("sq_junk", [128, D], BF16)
    ssq = kb.sb("ssq", [128, 1], F32)
    rstd = kb.sb("rstd", [128, 1], F32)
    hn = kb.sb("hn", [128, D], BF16)
    hnTs = [[kb.sb(f"hnT{a}_{i}", [128, DC, 128], BF16) for i in range(4)] for a in range(2)]
    wch = [kb.sb(f"wch{i}", [128, DC, 512], BF16) for i in range(3)]
    wfl = kb.sb("wfl", [128, DC, 8], BF16)
    ps_tr = [kb.ps(f"ps_tr{i}", [128, 4, 128], BF16) for i in range(2)]
    ps_mm = [kb.ps(f"ps_mm{i}", [128, 512], F32) for i in range(3)]
    ps_fl_t = kb.ps("ps_fl", [128, 4, 8], F32)
    ps_fl = ps_fl_t
    ps_flb = [Buf(f"ps_flb{i}", ps_fl_t.t, is_psum=True) for i in range(4)]
    ps_t2 = [kb.ps(f"ps_t2{i}", [128, 4, 128], BF16) for i in range(2)]
    st_f = [kb.sb(f"st_f{i}", [128, 512], F32) for i in range(3)]
    st_b = [kb.sb(f"st_b{i}", [128, 512], BF16) for i in range(3)]
    QTst = kb.sb("QTst", [128, NH, 512], BF16)
    KTst = kb.sb("KTst", [128, NH, 512], BF16)
    fl_t = kb.sb("fl_t", [128, 8], F32)
    lfacc = kb.sb("lfacc", [128, 8], F32)
    cst_f = [kb.sb(f"cst_f{i}", [128, 1024], F32) for i in range(2)]
    cst_b = [kb.sb(f"cst_b{i}", [128, 1024], BF16) for i in range(2)]
    KTc = kb.sb("KTc", [128, NH, 128], BF16)

    SCALE = float(HD) ** -0.5
    chunks = []
    for kind, c0, h0 in (("q", 0, 0), ("k", 1024, 0), ("v", 2048, 0)):
        for i in range(2):
            chunks.append((c0 + 512 * i, 512, kind, h0 + 4 * i))
    chunks.append((3072, 8, "fl", 0))
    for kind, c0, h0 in (("q", 3080, 8), ("k", 4104, 8), ("v", 5128, 8)):
        for i in range(2):
            chunks.append((c0 + 512 * i, 512, kind, h0 + 4 * i))
    w_in_v = w_in_bf.rearrange("(c p) n -> p c n", p=128)
    w_in_bf_bufs = []
    for ci_, (c0_, cw_, kind_, h0_) in enumerate(chunks):
        bb = kb.dram(f"w_in_bf{ci_}", w_in_bf)
        w_in_bf_bufs.append(bb)
        if cw_ >= 512:
            for hh in range(2):
                r0, r1 = hh * (D // 2), (hh + 1) * (D // 2)
                kb.dma("pool", w_in_bf[r0:r1, c0_:c0_ + cw_], w_in[r0:r1, c0_:c0_ + cw_], writes=[bb])
        else:
            with nc.allow_non_contiguous_dma(reason="8-column forget-gate slice"):
                kb.dma("pool", w_in_bf[:, c0_:c0_ + cw_], w_in[:, c0_:c0_ + cw_], writes=[bb])

    rr = {"x": 0, "tr": 0, "mm": 0, "t2": 0, "sf": 0, "sb": 0, "w": 0, "c": 0}

    def nxt(key, n):
        v = rr[key]
        rr[key] = (v + 1) % n
        return v

    def rmsnorm_tile(xb, g_b, out_b, rows=128):
        kb.op("act", lambda: act.activation(out=sq_junk.t[:], in_=xb.t[:], func=AF.Square), reads=[xb], writes=[sq_junk])
        kb.op("dve", lambda: dve.reduce_sum(out=ssq.t[:], in_=sq_junk.t[:], axis=AX.X), reads=[sq_junk], writes=[ssq])
        kb.op("dve", lambda: dve.tensor_scalar(out=rstd.t[:], in0=ssq.t[:], scalar1=1.0 / D, scalar2=EPS,
                                                op0=ALU.mult, op1=ALU.add), reads=[ssq], writes=[rstd])
        kb.op("act", lambda: act.activation(out=rstd.t[:], in_=rstd.t[:], func=AF.Sqrt), reads=[rstd], writes=[rstd])
        kb.op("dve", lambda: dve.reciprocal(out=rstd.t[:], in_=rstd.t[:]), reads=[rstd], writes=[rstd])
        kb.op("dve", lambda: dve.scalar_tensor_tensor(out=out_b.t[:], in0=xb.t[:], scalar=rstd.t[:, 0:1], in1=g_b.t[:],
                                                      op0=ALU.mult, op1=ALU.mult), reads=[xb, rstd, g_b], writes=[out_b])

    def transpose_to(dst_b, dst_ap_fn, src_b, src_ap_fn, n, ident, evac="dve", psl=None):
        pt = ps_tr[nxt("tr", 2)] if psl is None else psl[nxt("t2", 2)]
        for i in range(n):
            kb.op("pe", lambda i=i: pe.transpose(pt.t[:, i, :], src_ap_fn(i), ident.t[:]),
                  reads=[src_b, ident], writes=[pt], signal=(i == n - 1))
        if evac == "dve":
            kb.op("dve", lambda: dve.tensor_copy(out=dst_ap_fn(), in_=pt.t[:, 0:n, :]), reads=[pt], writes=[dst_b])
        else:
            kb.op("act", lambda: act.activation(out=dst_ap_fn(), in_=pt.t[:, 0:n, :], func=AF.Copy), reads=[pt], writes=[dst_b])

    def logf_from_psum(ti_all, rows_valid, out_ap, pbuf, li):
        kb.op("dve", lambda: dve.tensor_tensor(out=fl_t.t[:], in0=pbuf.t[:, li, :], in1=bf_bc.t[:], op=ALU.add),
              reads=[pbuf, bf_bc], writes=[fl_t])
        kb.op("act", lambda: act.activation(out=fl_t.t[:], in_=fl_t.t[:], func=AF.Exp, scale=-1.0), reads=[fl_t], writes=[fl_t])
        kb.op("act", lambda: act.activation(out=fl_t.t[:], in_=fl_t.t[:], func=AF.Ln, bias=1.0), reads=[fl_t], writes=[fl_t])
        kb.op("dve", lambda: dve.tensor_scalar(out=LF.t[:, ti_all, :], in0=fl_t.t[:], scalar1=-1.0, scalar2=None, op0=ALU.mult),
              reads=[fl_t], writes=[LF])
        kb.dma("sp", out_ap, LF.t[0:rows_valid, ti_all, :], reads=[LF])

    groups = [("p", list(range(g * 4, min(NT, g * 4 + 4)))) for g in range((NT + 3) // 4)] + [("s", [NPT])]
    outs = {"p": dict(fk=o_pfk, fv=o_pfv, fl=o_pfl, sk=o_psk, sv=o_psv),
            "s": dict(fk=o_sfk, fv=o_sfv, fl=o_sfl, sk=o_ssk, sv=o_ssv)}
    QT_b = {s: kb.dram("QT" + s, QT[s]) for s in "ps"}
    KT_b = {s: kb.dram("KT" + s, KT[s]) for s in "ps"}
    VV_b = {s: kb.dram("VV" + s, VV[s]) for s in "ps"}

    xjobs = [(seq, ti) for (seq, tiles) in groups for ti in tiles]
    xbuf_of = {}

    def issue_x(m):
        if m >= len(xjobs) or m in xbuf_of:
            return
        seq, ti = xjobs[m]
        xb = xt[m % 2]
        xbuf_of[m] = xb
        if seq == "p":
            kb.dma("sp", xb.t[:], x_p[ti * 128:(ti + 1) * 128, :], writes=[xb])
        else:
            kb.op("dve", lambda: dve.memset(xb.t[:], 0.0), writes=[xb])
            kb.dma("sp", xb.t[0:T, :], x_s[:, :], writes=[xb])

    wjobs = [(gi, ci) for gi in range(len(groups)) for ci in range(len(chunks))]
    wbuf_of = {}
    wslot = [0]

    def issue_w(k):
        if k >= len(wjobs) or k in wbuf_of:
            return
        gi, ci = wjobs[k]
        c0, cw, kind, h0 = chunks[ci]
        if kind == "fl":
            wb = wfl
        else:
            wb = wch[wslot[0] % 3]
            wslot[0] += 1
        wbuf_of[k] = wb
        kb.dma("sp", wb.t[:, :, 0:cw], w_in_v[:, :, c0:c0 + cw], reads=[w_in_bf_bufs[ci]], writes=[wb])

    pending = []

    def flush_pending():
        while pending:
            pending.pop(0)()

    wk = 0
    xidx = {}
    m_ = 0
    for gi_, (seq_, tiles_) in enumerate(groups):
        for li_, ti_ in enumerate(tiles_):
            xidx[(gi_, li_)] = m_
            m_ += 1

    def prep_norm(gi, li):
        m = xidx[(gi, li)]
        issue_x(m)
        issue_x(m + 1)
        rmsnorm_tile(xbuf_of[m], g_bc, hn)

    def prep_tr(gi, li):
        dst = hnTs[gi % 2][li]
        for c4 in range(DC // 4):
            transpose_to(dst, lambda c4=c4: dst.t[:, c4 * 4:(c4 + 1) * 4, :], hn,
                         lambda i, c4=c4: hn.t[:, (c4 * 4 + i) * 128:(c4 * 4 + i + 1) * 128], 4, C["ident_bf"],
                         evac=("dve" if c4 % 2 == 0 else "act"))

    cjobs = [(j, src, isk, h0) for j in range(NPT)
             for (src, isk, h0) in ((c_fk, True, 0), (c_sk, True, 8), (c_fv, False, 0), (c_sv, False, 8))]

    def cache_part_a(k):
        j, src, isk, h0 = cjobs[k]
        r0 = j * 128
        if h0 == 0 and isk:
            kb.dma("sp", LF.t[:, NT + j, :], c_fl[r0:r0 + 128, :], writes=[LF])
        cf, cb_ = cst_f[k % 2], cst_b[k % 2]
        kb.dma("sp", cf.t[:], src[r0:r0 + 128, :], writes=[cf])
        kb.op("dve", lambda: dve.tensor_copy(out=cb_.t[:], in_=cf.t[:]), reads=[cf], writes=[cb_])

    def cache_part_b(k):
        j, src, isk, h0 = cjobs[k]
        r0 = j * 128
        cb_ = cst_b[k % 2]
        if isk:
            for q4 in range(2):
                transpose_to(KTc, lambda q4=q4: KTc.t[:, h0 + q4 * 4:h0 + q4 * 4 + 4, :], cb_,
                             lambda i, q4=q4: cb_.t[:, (q4 * 4 + i) * 128:(q4 * 4 + i + 1) * 128], 4, C["ident_bf"],
                             evac="act", psl=ps_t2)
            if h0 == 8:
                kb.dma("sp", KT["s"][:, :, r0:r0 + 128].rearrange("h d t -> d h t"), KTc.t[:], reads=[KTc], writes=[KT_b["s"]])
        else:
            kb.dma("sp", VV["s"][r0:r0 + 128, h0 * 128:h0 * 128 + 1024], cb_.t[:], reads=[cb_], writes=[VV_b["s"]])

    def cache_step():
        k = cache_done[0]
        if k > len(cjobs):
            return
        if k >= 1:
            cache_part_b(k - 1)
        if k < len(cjobs):
            cache_part_a(k)
        cache_done[0] += 1

    cache_done = [0]

    issue_x(0)
    for li0 in range(len(groups[0][1])):
        prep_norm(0, li0)
        prep_tr(0, li0)
    for gi, (seq, tiles) in enumerate(groups):
        ntl = len(tiles)
        hnT = hnTs[gi % 2]
        nxt_tiles = len(groups[gi + 1][1]) if gi + 1 < len(groups) else 0
        for ci, (c0, cw, kind, h0) in enumerate(chunks):
            issue_w(wk); issue_w(wk + 1); issue_w(wk + 2)
            wb = wbuf_of[wk]
            wk += 1
            for li, ti in enumerate(tiles):
                rows = 128 if seq == "p" else T
                r0 = ti * 128 if seq == "p" else 0
                ti_all = ti if seq == "p" else NT + ti
                pm = ps_flb[li] if kind == "fl" else ps_mm[nxt("mm", 3)]
                pm_ap = pm.t[:, li, :] if kind == "fl" else pm.t[:, 0:cw]
                for c in range(DC):
                    kb.op("pe", lambda c=c, pm_ap=pm_ap, wb=wb, li=li: pe.matmul(pm_ap, hnT[li].t[:, c, :], wb.t[:, c, 0:cw],
                                                                               start=(c == 0), stop=(c == DC - 1)),
                          reads=[hnT[li], wb], writes=[pm], signal=(c == DC - 1))
                flush_pending()
                fox = h0 < NFH
                if kind == "fl":
                    logf_from_psum(ti_all, rows, outs[seq]["fl"][r0:r0 + rows, :], pm, li)
                elif kind == "q":
                    sb_ = st_b[nxt("sb", 3)]
                    kb.op("act", lambda: act.activation(out=sb_.t[:], in_=pm.t[:], func=AF.Copy, scale=SCALE),
                          reads=[pm], writes=[sb_])
                    pending.append(lambda sb_=sb_, h0=h0, li=li: transpose_to(
                        QTst, lambda: QTst.t[:, h0:h0 + 4, li * 128:(li + 1) * 128], sb_,
                        lambda i: sb_.t[:, i * 128:(i + 1) * 128], 4, C["ident_bf"], evac="dve", psl=ps_t2))
                elif kind == "k":
                    sf_ = st_f[nxt("sf", 3)]
                    sb_ = st_b[nxt("sb", 3)]
                    kb.op("act", lambda: act.activation(out=sf_.t[:], in_=pm.t[:], func=AF.Copy), reads=[pm], writes=[sf_])
                    kb.op("dve", lambda: dve.tensor_copy(out=sb_.t[:], in_=sf_.t[:]), reads=[sf_], writes=[sb_])
                    oc = (h0 % NFH) * 128
                    kb.dma("sp", outs[seq]["fk" if fox else "sk"][r0:r0 + rows, oc:oc + 512], sf_.t[0:rows, :], reads=[sf_])
                    pending.append(lambda sb_=sb_, h0=h0, li=li: transpose_to(
                        KTst, lambda: KTst.t[:, h0:h0 + 4, li * 128:(li + 1) * 128], sb_,
                        lambda i: sb_.t[:, i * 128:(i + 1) * 128], 4, C["ident_bf"], evac="act", psl=ps_t2))
                else:
                    sf_ = st_f[nxt("sf", 3)]
                    sb_ = st_b[nxt("sb", 3)]
                    kb.op("act", lambda: act.activation(out=sf_.t[:], in_=pm.t[:], func=AF.Copy), reads=[pm], writes=[sf_])
                    kb.op("dve", lambda: dve.tensor_copy(out=sb_.t[:], in_=sf_.t[:]), reads=[sf_], writes=[sb_])
                    oc = (h0 % NFH) * 128
                    kb.dma("sp", outs[seq]["fv" if fox else "sv"][r0:r0 + rows, oc:oc + 512], sf_.t[0:rows, :], reads=[sf_])
                    kb.dma("sp", VV[seq][ti * 128:(ti + 1) * 128, h0 * 128:h0 * 128 + 512], sb_.t[:], reads=[sb_], writes=[VV_b[seq]])
            if ci >= 5:
                cache_step()
            if ci >= 1 and ci - 1 < nxt_tiles:
                prep_tr(gi + 1, ci - 1)
            if ci < nxt_tiles:
                prep_norm(gi + 1, ci)
        flush_pending()
        t0 = tiles[0] * 128
        if seq == "p":
            kb.dma("sp", QT["p"][:, :, t0:t0 + ntl * 128].rearrange("h d t -> d h t"), QTst.t[:, :, 0:ntl * 128],
                   reads=[QTst], writes=[QT_b["p"]])
            kb.dma("sp", KT["p"][:, :, t0:t0 + ntl * 128].rearrange("h d t -> d h t"), KTst.t[:, :, 0:ntl * 128],
                   reads=[KTst], writes=[KT_b["p"]])
        else:
            kb.dma("sp", QT["s"][:, :, :].rearrange("h d t -> d h t"), QTst.t[:, :, 0:128], reads=[QTst], writes=[QT_b["s"]])
            kb.dma("sp", KT["s"][:, :, t0:t0 + 128].rearrange("h d t -> d h t"), KTst.t[:, :, 0:128],
                   reads=[KTst], writes=[KT_b["s"]])

    while cache_done[0] <= len(cjobs):
        cache_step()

    ps_c = ps_flb[0]
    for (a0, n) in ((0, NT), (NT, NTS)):
        for j in range(n):
            ja = a0 + j
            if j == 0:
                kb.op("dve", lambda: dve.memset(lfacc.t[:], 0.0), writes=[lfacc])
            kb.op("pe", lambda: pe.matmul(ps_c.t[:, 0, :], C["tri_incl_f"].t[:], LF.t[:, ja, :], start=True, stop=False),
                  reads=[C["tri_incl_f"], LF], writes=[ps_c], signal=False)
            kb.op("pe", lambda: pe.matmul(ps_c.t[:, 0, :], C["ones_f"].t[:], lfacc.t[:], start=False, stop=True),
                  reads=[C["ones_f"], lfacc], writes=[ps_c])
            kb.op("dve", lambda: dve.tensor_copy(out=CB.t[:, ja, :], in_=ps_c.t[:, 0, :]), reads=[ps_c], writes=[CB])
            kb.op("dve", lambda: dve.tensor_tensor(out=lfacc.t[:], in0=lfacc.t[:], in1=LF.t[:, ja, :], op=ALU.add),
                  reads=[lfacc, LF], writes=[lfacc])
    ps_cr = ps_mm[0]
    kb.op("pe", lambda: pe.matmul(ps_cr.t[:, 0:NTA * 8], C["sel0_f"].t[:], CB.t[:].rearrange("p a h -> p (a h)"), start=True, stop=True),
          reads=[C["sel0_f"], CB], writes=[ps_cr])
    kb.op("dve", lambda: dve.tensor_copy(out=CREF.t[:].rearrange("p a h -> p (a h)"), in_=ps_cr.t[:, 0:NTA * 8]), reads=[ps_cr], writes=[CREF])
    kb.end_phase()

    if stop_after <= 1:
        kb.finish()
        return nc, consts


    kb.begin_phase()
    NFT_b = kb.dram("NFT", NFT)
    gT = kb.sb("gT", [128, NH], F32)
    with nc.allow_non_contiguous_dma(reason="tiny per-head gain transpose"):
        kb.dma("sp", gT.t[:, 0:8], g_of[0:1, :].rearrange("o (h d) -> d (o h)", d=128), writes=[gT])
        kb.dma("sp", gT.t[:, 8:16], g_os[0:1, :].rearrange("o (h d) -> d (o h)", d=128), writes=[gT])
    cast_sem = kb._sem("cast_sem")
    n_cast = 0
    for e in range(NE):
        for (src, dst, rows) in ((w1, w1_bf, D), (w3, w3_bf, D), (w2, w2_bf, DEXP)):
            for hh in range(2):
                r0, r1 = hh * (rows // 2), (hh + 1) * (rows // 2)
                pool.dma_start(out=dst[e, r0:r1, :], in_=src[e, r0:r1, :]).then_inc(cast_sem, 16)
                n_cast += 1
    MAXTOK = max(S, SS)
    ktb = {(t, i): kb.sb(f"kt_{t}{i}", [128, MAXTOK], BF16) for t in "fs" for i in range(2)}
    vb = {(t, i): kb.sb(f"v_{t}{i}", [128, MAXTOK // 128, 128], BF16) for t in "fs" for i in range(2)}
    qtb = {(t, i): kb.sb(f"qt_{t}{i}", [128, 512], BF16) for t in "fs" for i in range(2)}
    nbt = [kb.sb(f"nb{i}", [128, MAXTOK // 128], F32) for i in range(2)]
    Pt = [kb.sb(f"Pt{i}", [128, 512], BF16) for i in range(3)]
    Et = [kb.sb(f"Et{i}", [128, 512], F32) for i in range(3)]
    SPt = [kb.sb(f"SPt{i}", [128, 512], BF16) for i in range(3)]
    Gt = [kb.sb(f"Gt{i}", [128, 512], BF16) for i in range(3)]
    At = [kb.sb(f"At{i}", [128, 512], BF16) for i in range(3)]
    u_sb = [kb.sb(f"u_sb{i}", [128, 512], F32) for i in range(2)]
    usq = [kb.sb(f"usq{i}", [128, 512], BF16) for i in range(2)]
    den_sb = kb.sb("den_sb", [128, 512], F32)
    d2 = kb.sb("d2", [128, 512], F32)
    xv = [kb.sb(f"xv{i}", [128, 512], F32) for i in range(2)]
    rs_t = [kb.sb(f"rs_t{i}", [128, 512], F32) for i in range(2)]
    nfst = [kb.sb(f"nfst{i}", [128, 512], BF16) for i in range(4)]
    Zf = [kb.ps(f"Zf{i}", [128, 512], F32) for i in range(2)]
    Zs = [kb.ps(f"Zs{i}", [128, 512], F32) for i in range(2)]
    OTf = kb.ps("OTf", [128, 512], F32)
    OTs = kb.ps("OTs", [128, 512], F32)
    DENf = kb.ps("DENf", [128, 512], F32)
    CUMs = kb.ps("CUMs", [128, 512], F32)
    LN128H = 0.5 * float(np.log(128.0))
    cyc = {"P": 0, "E": 0, "SP": 0, "G": 0, "A": 0, "nf": 0, "zf": 0, "zs": 0}

    def cy(key, n):
        v = cyc[key]
        cyc[key] = (v + 1) % n
        return v

    seqinfo = {"p": dict(ntok=S, nt=NT, a0=0), "s": dict(ntok=SS, nt=NTS, a0=NT)}

    def load_head(seq, hp, slot):
        n = seqinfo[seq]["ntok"]
        for t, h in (("f", hp), ("s", hp + 8)):
            kb.dma("sp", ktb[(t, slot)].t[:, 0:n], KT[seq][h, :, :], reads=[KT_b[seq]], writes=[ktb[(t, slot)]])
            kb.dma("sp", vb[(t, slot)].t[:, 0:n // 128, :],
                   VV[seq].rearrange("(j p) c -> p j c", p=128)[:, :, h * 128:(h + 1) * 128],
                   reads=[VV_b[seq]], writes=[vb[(t, slot)]])

    def normalize(h, ot_ps, den_ps, nq, z_ps, col0, idx):
        ub, uq, xb_, rb = u_sb[idx], usq[idx], xv[idx], rs_t[idx]
        if den_ps is not None:
            kb.op("dve", lambda: dve.tensor_copy(out=den_sb.t[:, 0:nq], in_=den_ps.t[:, 0:nq]), reads=[den_ps], writes=[den_sb])
            kb.op("dve", lambda: dve.reciprocal(out=d2.t[:, 0:nq], in_=den_sb.t[:, 0:nq]), reads=[den_sb], writes=[d2])
            kb.op("dve", lambda: dve.tensor_tensor(out=ub.t[:, 0:nq], in0=ot_ps.t[:, 0:nq], in1=d2.t[:, 0:nq], op=ALU.mult),
                  reads=[ot_ps, d2], writes=[ub])
        else:
            kb.op("dve", lambda: dve.tensor_copy(out=ub.t[:, 0:nq], in_=ot_ps.t[:, 0:nq]), reads=[ot_ps], writes=[ub])
        kb.op("dve", lambda: dve.tensor_tensor(out=uq.t[:, 0:nq], in0=ub.t[:, 0:nq], in1=ub.t[:, 0:nq], op=ALU.mult),
              reads=[ub], writes=[uq])

        def part1b():
            kb.op("pe", lambda: pe.matmul(z_ps.t[:, 0:nq], C["ones_bf"].t[:], uq.t[:, 0:nq], start=True, stop=True),
                  reads=[C["ones_bf"], uq], writes=[z_ps])
            kb.op("dve", lambda: dve.tensor_copy(out=xb_.t[:, 0:nq], in_=z_ps.t[:, 0:nq]), reads=[z_ps], writes=[xb_])

        def part2():
            kb.op("act", lambda: act.activation(out=xb_.t[:, 0:nq], in_=xb_.t[:, 0:nq], func=AF.Ln, bias=128.0 * EPS),
                  reads=[xb_], writes=[xb_])
            kb.op("act", lambda: act.activation(out=rb.t[:, 0:nq], in_=xb_.t[:, 0:nq], func=AF.Exp, scale=-0.5, bias=LN128H),
                  reads=[xb_], writes=[rb])
            nf = nfst[cy("nf", 4)]
            kb.op("dve", lambda: dve.scalar_tensor_tensor(out=nf.t[:, 0:nq], in0=ub.t[:, 0:nq], scalar=gT.t[:, h:h + 1],
                                                          in1=rb.t[:, 0:nq], op0=ALU.mult, op1=ALU.mult),
                  reads=[ub, gT, rb], writes=[nf])
            kb.dma("sp", NFT[h * 128:(h + 1) * 128, col0:col0 + nq], nf.t[:, 0:nq], reads=[nf], writes=[NFT_b])
        return part1b, part2

    pairs = [(seq, hp) for seq in ("p", "s") for hp in range(NFH)]
    steps = []
    glist = []
    gcount = 0
    for pi, (seq, hp) in enumerate(pairs):
        if seq == "p":
            qgroups = [(g * 4, min(4, NT - g * 4)) for g in range((NT + 3) // 4)]
        else:
            qgroups = [(NPT, 1)]
        for gi2, (qt0, nqt) in enumerate(qgroups):
            jmax = qt0 + nqt - 1
            nkb = jmax + 1
            for st in range(nkb):
                steps.append(dict(pi=pi, seq=seq, hp=hp, slot=pi % 2, gs=gcount % 2, qt0=qt0, nqt=nqt, nq=nqt * 128, jmax=jmax,
                                  nkb=nkb, step=st, jf=st, js=jmax - st, first=(st == 0), last=(st == nkb - 1), gidx=gcount,
                                  pair_first=(gi2 == 0 and st == 0)))
            glist.append(dict(seq=seq, hp=hp, qt0=qt0, nq=nqt * 128, gs=gcount % 2, nkb=nkb,
                              jref=qt0 + (2 if nqt == 4 else 0)))
            gcount += 1
    NS = len(steps)

    def load_q(gidx):
        if gidx >= len(glist):
            return
        gd = glist[gidx]
        seq, hp, nq = gd["seq"], gd["hp"], gd["nq"]
        qf_, qs_ = qtb[("f", gd["gs"])], qtb[("s", gd["gs"])]
        qcol = gd["qt0"] * 128 if seq == "p" else 0
        kb.dma("sp", qf_.t[:, 0:nq], QT[seq][hp, :, qcol:qcol + nq], reads=[QT_b[seq]], writes=[qf_])
        kb.dma("sp", qs_.t[:, 0:nq], QT[seq][hp + 8, :, qcol:qcol + nq], reads=[QT_b[seq]], writes=[qs_])
        a0 = seqinfo[seq]["a0"]
        nb = nbt[gd["gs"]]
        nkb, jref = gd["nkb"], gd["jref"]
        kb.op("dve", lambda: dve.tensor_scalar(out=nb.t[:, 0:nkb], in0=CB.t[:, a0:a0 + nkb, hp], scalar1=-1.0,
                                                scalar2=CREF.t[:, a0 + jref, hp:hp + 1], op0=ALU.mult, op1=ALU.add),
              reads=[CB, CREF], writes=[nb])

    bufs_of = {}

    def stage_A(t):
        sd = steps[t]
        seq, hp, nq = sd["seq"], sd["hp"], sd["nq"]
        a0 = seqinfo[seq]["a0"]
        qf_, qs_ = qtb[("f", sd["gs"])], qtb[("s", sd["gs"])]
        nb = nbt[sd["gs"]]
        ktf, kts = ktb[("f", sd["slot"])], ktb[("s", sd["slot"])]
        if sd["first"]:
            load_q(sd["gidx"] + 1)
        jf, js = sd["jf"], sd["js"]
        zf = Zf[cy("zf", 2)]
        zs = Zs[cy("zs", 2)]
        kb.op("pe", lambda: pe.matmul(zs.t[:, 0:nq], kts.t[:, js * 128:(js + 1) * 128], qs_.t[:, 0:nq], start=True, stop=True),
              reads=[kts, qs_], writes=[zs])
        kb.op("pe", lambda: pe.matmul(zf.t[:, 0:nq], ktf.t[:, jf * 128:(jf + 1) * 128], qf_.t[:, 0:nq], start=True, stop=True),
              reads=[ktf, qf_], writes=[zf])
        Eb = Et[cy("E", 3)]
        Pb = Pt[cy("P", 3)]
        SPb = SPt[cy("SP", 3)]
        bufs_of[t] = dict(E=Eb, P=Pb, SP=SPb)
        kb.op("act", lambda: act.activation(out=Eb.t[:, 0:nq], in_=zs.t[:, 0:nq], func=AF.Exp), reads=[zs], writes=[Eb])
        kb.op("act", lambda: act.activation(out=Pb.t[:, 0:nq], in_=zf.t[:, 0:nq], func=AF.Exp, bias=nb.t[:, jf:jf + 1]),
              reads=[zf, nb], writes=[Pb])
        rs_ = js - sd["qt0"]
        if rs_ >= 0:
            kb.op("dve", lambda: dve.tensor_tensor(out=Eb.t[:, 0:nq], in0=Eb.t[:, 0:nq], in1=C["mask_sb"].t[:, rs_, 0:nq], op=ALU.mult),
                  reads=[Eb, C["mask_sb"]], writes=[Eb])
        rf_ = jf - sd["qt0"]
        if rf_ >= 0:
            kb.op("dve", lambda: dve.tensor_tensor(out=Pb.t[:, 0:nq], in0=Pb.t[:, 0:nq], in1=C["mask_fox"].t[:, rf_, 0:nq], op=ALU.mult),
                  reads=[Pb, C["mask_fox"]], writes=[Pb])

    def stage_A2(t):
        sd = steps[t]
        nq = sd["nq"]
        Eb, SPb = bufs_of[t]["E"], bufs_of[t]["SP"]
        kb.op("act", lambda: act.activation(out=SPb.t[:, 0:nq], in_=Eb.t[:, 0:nq], func=AF.Ln, bias=1.0), reads=[Eb], writes=[SPb])

    def stage_B_pe(t):
        sd = steps[t]
        nq = sd["nq"]
        SPb = bufs_of[t]["SP"]
        kb.op("pe", lambda: pe.matmul(CUMs.t[:, 0:nq], C["negtri_bf"].t[:], SPb.t[:, 0:nq], start=sd["first"], stop=True,
                                       skip_group_check=True),
              reads=[C["negtri_bf"], SPb], writes=[CUMs])

    def stage_B_act(t):
        sd = steps[t]
        nq = sd["nq"]
        Gb = Gt[cy("G", 3)]
        bufs_of[t]["G"] = Gb
        kb.op("act", lambda: act.activation(out=Gb.t[:, 0:nq], in_=CUMs.t[:, 0:nq], func=AF.Exp), reads=[CUMs], writes=[Gb])

    def stage_C_fix(t):
        sd = steps[t]
        nq = sd["nq"]
        if not sd["last"]:
            SPb = bufs_of[t]["SP"]
            kb.op("pe", lambda: pe.matmul(CUMs.t[:, 0:nq], C["negcompl_bf"].t[:], SPb.t[:, 0:nq], start=False, stop=True,
                                           skip_group_check=True),
                  reads=[C["negcompl_bf"], SPb], writes=[CUMs])

    def stage_C(t, it):
        sd = steps[t]
        nq, jf, js = sd["nq"], sd["jf"], sd["js"]
        vf_, vs_ = vb[("f", sd["slot"])], vb[("s", sd["slot"])]
        bo = bufs_of.pop(t)
        Pb, Eb, Gb = bo["P"], bo["E"], bo["G"]
        if sd["pair_first"] and sd["pi"] + 1 < len(pairs):
            load_head(*pairs[sd["pi"] + 1], (sd["pi"] + 1) % 2)
        while norm_pending_b:
            fa, fb = norm_pending_b.pop(0)
            fa()
            fb()
        kb.op("pe", lambda: pe.matmul(OTf.t[:, 0:nq], vf_.t[:, jf, :], Pb.t[:, 0:nq], start=sd["first"], stop=sd["last"]),
              reads=[vf_, Pb], writes=[OTf], signal=False)
        kb.op("pe", lambda: pe.matmul(DENf.t[:, 0:nq], C["ones_bf"].t[:], Pb.t[:, 0:nq], start=sd["first"], stop=sd["last"]),
              reads=[C["ones_bf"], Pb], writes=[DENf])
        Ab = At[cy("A", 3)]
        kb.op("dve", lambda: dve.tensor_tensor(out=Ab.t[:, 0:nq], in0=Eb.t[:, 0:nq], in1=Gb.t[:, 0:nq], op=ALU.mult),
              reads=[Eb, Gb], writes=[Ab])
        kb.op("pe", lambda: pe.matmul(OTs.t[:, 0:nq], vs_.t[:, js, :], Ab.t[:, 0:nq], start=sd["first"], stop=sd["last"]),
              reads=[vs_, Ab], writes=[OTs])
        if sd["last"]:
            col0 = sd["qt0"] * 128 if sd["seq"] == "p" else NT * 128
            while norm_pending:
                _, q2a, q2b = norm_pending.pop(0)
                q2a()
                q2b()
            p1a, p2a = normalize(sd["hp"], OTf, DENf, nq, OTf, col0, 0)
            p1b, p2b = normalize(sd["hp"] + 8, OTs, None, nq, OTs, col0, 1)
            norm_pending_b.append((p1a, p1b))
            norm_pending.append((it + 6, p2a, p2b))

    load_head(*pairs[0], 0)
    load_q(0)
    norm_pending = []
    norm_pending_b = []
    for it in range(NS + 2):
        if it < NS:
            stage_A(it)
        if 0 <= it - 2 < NS:
            stage_C_fix(it - 2)
        if 0 <= it - 1 < NS:
            stage_B_pe(it - 1)
            stage_B_act(it - 1)
        if it < NS:
            stage_A2(it)
        while norm_pending and norm_pending[0][0] <= it:
            _, p2a, p2b = norm_pending.pop(0)
            p2a()
            p2b()
        if 0 <= it - 2 < NS:
            stage_C(it - 2, it)
        if it >= NS + 1:
            while norm_pending_b:
                fa, fb = norm_pending_b.pop(0)
                fa()
                fb()
        while norm_pending and (norm_pending[0][0] <= it or it >= NS + 1):
            _, p2a, p2b = norm_pending.pop(0)
            p2a()
            p2b()
    kb.end_phase()
    if stop_after <= 2:
        kb.finish()
        return nc, consts


    kb.begin_phase()
    NR = NG + NE
    BIG = 1.0e30
    TRASH = float(NSLOT)
    Hs_b = kb.dram("Hs", Hs)
    XS_b = kb.dram("XS", XS)
    YS_b = kb.dram("YS", YS)
    SLOT = kb.sb("SLOT", [128, NTOK_T, 2], I32, persistent=True)
    GATE = kb.sb("GATE", [128, NTOK_T, 2], F32, persistent=True)
    wo = kb.sb("wo", [128, DC, D], BF16)
    for c4 in range(4):
        kb.dma("pool", wo.t[:, c4 * 4:(c4 + 1) * 4, :], w_out.rearrange("(c p) n -> p c n", p=128)[:, c4 * 4:(c4 + 1) * 4, :], writes=[wo])
    g2_bc = kb.sb("g2_bc", [128, D], F32)
    kb.dma("sp", g2_bc.t[:], bcast_row(g_ffn[0:1, :], D), writes=[g2_bc])
    wr = kb.sb("wr", [128, DC, NR], F32)
    with nc.allow_non_contiguous_dma(reason="small router weight"):
        kb.dma("sp", wr.t[:], w_rt.rearrange("(c p) n -> p c n", p=128), writes=[wr])
    br_bc = kb.sb("br_bc", [128, NR], F32)
    kb.dma("sp", br_bc.t[:], bcast_row(b_rt[0:1, :], NR), writes=[br_bc])
    nfT = [kb.sb(f"nfT{i}", [128, DC, 128], BF16) for i in range(2)]
    x3 = [kb.sb(f"x3_{i}", [128, D], F32) for i in range(2)]
    hres = [kb.sb(f"hres{i}", [128, D], F32) for i in range(2)]
    sq3 = kb.sb("sq3", [128, D], F32)
    ssq3 = kb.sb("ssq3", [128, 1], F32)
    rstd3 = kb.sb("rstd3", [128, 1], F32)
    hn2s = [kb.sb(f"hn2_{i}", [128, D], F32) for i in range(2)]
    hn2b = [kb.sb(f"hn2b{i}", [128, D], BF16) for i in range(3)]
    hn2T = kb.sb("hn2T", [128, DC, 128], F32)
    cntacc = kb.sb("cntacc", [128, NE], BF16)
    L = kb.sb("L", [128, NR], F32)
    sm = {n: kb.sb("r_" + n, [128, w], F32) for n, w in
          (("gmax", 1), ("ngmax", 1), ("gmask", NG), ("eg", NG), ("sg", 1), ("pg", 1), ("pen", NG), ("Lm", NE), ("m1", 1),
           ("mask1", NE), ("Lm2", NE), ("m2", 1), ("mask2", NE), ("dd", 1), ("e2", 1), ("den", 1), ("sp", NE), ("valid", NE),
           ("t1", NE), ("s1", 1), ("s2", 1))}
    msums = [kb.sb(f"msum{i}", [128, NE], BF16) for i in range(2)]
    mk1s = [kb.sb(f"mk1_{i}", [128, NE], F32) for i in range(2)]
    mk2s = [kb.sb(f"mk2_{i}", [128, NE], F32) for i in range(2)]
    ps_o = [kb.ps(f"ps_o{i}", [128, 512], F32) for i in range(4)]
    ps_ht = [kb.ps(f"ps_ht{i}", [128, 4, 128], F32) for i in range(2)]
    ps_lg = kb.ps("ps_lg", [128, NR], F32)
    ps_pos = kb.ps("ps_pos", [128, NE], F32)
    kb.op("dve", lambda: dve.memset(cntacc.t[:], 0.0), writes=[cntacc])
    cy3 = {"o": 0, "ht": 0}

    def cyc3(k, n):
        v = cy3[k]
        cy3[k] = (v + 1) % n
        return v

    def load3(i):
        nb_, xb = nfT[i % 2], x3[i % 2]
        kb.dma("sp", nb_.t[:], NFT.rearrange("(c p) t -> p c t", p=128)[:, :, i * 128:(i + 1) * 128], reads=[NFT_b], writes=[nb_])
        if i < NT:
            kb.dma("sp", xb.t[:], x_p[i * 128:(i + 1) * 128, :], writes=[xb])
        else:
            kb.op("dve", lambda: dve.memset(xb.t[:], 0.0), writes=[xb])
            kb.dma("sp", xb.t[0:T, :], x_s[:, :], writes=[xb])

    def p3_stage1(i):
        if i + 1 < NTOK_T:
            load3(i + 1)
        hn2 = hn2s[i % 2]
        nb_, xb, hb, hb16 = nfT[i % 2], x3[i % 2], hres[i % 2], hn2b[i % 3]
        for n in range(4):
            po = ps_o[cyc3("o", 4)]
            for c in range(DC):
                kb.op("pe", lambda: pe.matmul(po.t[:], nb_.t[:, c, :], wo.t[:, c, n * 512:(n + 1) * 512], start=(c == 0), stop=(c == DC - 1)),
                      reads=[nb_, wo], writes=[po], signal=(c == DC - 1))
            kb.op("dve", lambda: dve.tensor_tensor(out=hb.t[:, n * 512:(n + 1) * 512], in0=po.t[:], in1=xb.t[:, n * 512:(n + 1) * 512], op=ALU.add),
                  reads=[po, xb], writes=[hb])
        kb.dma("sp", Hs[i * 128:(i + 1) * 128, :], hb.t[:], reads=[hb], writes=[Hs_b])

    def p3_stage1b(i):
        hb, hb16 = hres[i % 2], hn2b[i % 3]
        hn2 = hn2s[i % 2]
        kb.op("act", lambda: act.activation(out=sq3.t[:], in_=hb.t[:], func=AF.Square), reads=[hb], writes=[sq3])
        kb.op("dve", lambda: dve.reduce_sum(out=ssq3.t[:], in_=sq3.t[:], axis=AX.X), reads=[sq3], writes=[ssq3])
        kb.op("dve", lambda: dve.tensor_scalar(out=rstd3.t[:], in0=ssq3.t[:], scalar1=1.0 / D, scalar2=EPS, op0=ALU.mult, op1=ALU.add),
              reads=[ssq3], writes=[rstd3])
        kb.op("act", lambda: act.activation(out=rstd3.t[:], in_=rstd3.t[:], func=AF.Ln), reads=[rstd3], writes=[rstd3])
        kb.op("act", lambda: act.activation(out=rstd3.t[:], in_=rstd3.t[:], func=AF.Exp, scale=-0.5), reads=[rstd3], writes=[rstd3])
        kb.op("dve", lambda: dve.scalar_tensor_tensor(out=hn2.t[:], in0=hb.t[:], scalar=rstd3.t[:, 0:1], in1=g2_bc.t[:], op0=ALU.mult, op1=ALU.mult),
              reads=[hb, rstd3, g2_bc], writes=[hn2])
        kb.op("act", lambda: act.activation(out=hb16.t[:], in_=hn2.t[:], func=AF.Copy), reads=[hn2], writes=[hb16])


    def p3_stage2(i):
        hn2 = hn2s[i % 2]
        hb16 = hn2b[i % 3]
        for c4 in range(DC // 4):
            pt = ps_ht[cyc3("ht", 2)]
            for k in range(4):
                c = c4 * 4 + k
                kb.op("pe", lambda: pe.transpose(pt.t[:, k, :], hn2.t[:, c * 128:(c + 1) * 128], C["ident_f"].t[:]),
                      reads=[hn2, C["ident_f"]], writes=[pt], signal=(k == 3))
            kb.op("dve" if c4 % 2 == 0 else "act",
                  (lambda: dve.tensor_copy(out=hn2T.t[:, c4 * 4:(c4 + 1) * 4, :], in_=pt.t[:])) if c4 % 2 == 0 else
                  (lambda: act.activation(out=hn2T.t[:, c4 * 4:(c4 + 1) * 4, :], in_=pt.t[:], func=AF.Copy)),
                  reads=[pt], writes=[hn2T])
        for c in range(DC):
            kb.op("pe", lambda: pe.matmul(ps_lg.t[:], hn2T.t[:, c, :], wr.t[:, c, :], start=(c == 0), stop=(c == DC - 1)),
                  reads=[hn2T, wr], writes=[ps_lg], signal=(c == DC - 1))
        V = lambda n: sm[n]
        dv = lambda fn, r, w: kb.op("dve", fn, reads=r, writes=w)
        dv(lambda: dve.tensor_tensor(out=L.t[:], in0=ps_lg.t[:], in1=br_bc.t[:], op=ALU.add), [ps_lg, br_bc], [L])
        dv(lambda: dve.reduce_max(out=V("gmax").t[:], in_=L.t[:, 0:NG], axis=AX.X), [L], [V("gmax")])
        dv(lambda: dve.tensor_scalar(out=V("gmask").t[:], in0=L.t[:, 0:NG], scalar1=V("gmax").t[:, 0:1], scalar2=None, op0=ALU.is_ge),
           [L, V("gmax")], [V("gmask")])
        dv(lambda: dve.tensor_scalar(out=V("ngmax").t[:], in0=V("gmax").t[:], scalar1=-1.0, scalar2=None, op0=ALU.mult), [V("gmax")], [V("ngmax")])
        kb.op("act", lambda: act.activation(out=V("eg").t[:], in_=L.t[:, 0:NG], func=AF.Exp, bias=V("ngmax").t[:, 0:1]),
              reads=[L, V("ngmax")], writes=[V("eg")])
        dv(lambda: dve.reduce_sum(out=V("sg").t[:], in_=V("eg").t[:], axis=AX.X), [V("eg")], [V("sg")])
        dv(lambda: dve.reciprocal(out=V("pg").t[:], in_=V("sg").t[:]), [V("sg")], [V("pg")])
        dv(lambda: dve.tensor_scalar(out=V("pen").t[:], in0=V("gmask").t[:], scalar1=BIG, scalar2=-BIG, op0=ALU.mult, op1=ALU.add),
           [V("gmask")], [V("pen")])
        for g in range(NG):
            dv(lambda: dve.tensor_scalar(out=V("Lm").t[:, g * EPG:(g + 1) * EPG], in0=L.t[:, NG + g * EPG:NG + (g + 1) * EPG],
                                         scalar1=V("pen").t[:, g:g + 1], scalar2=None, op0=ALU.add), [L, V("pen")], [V("Lm")])
        dv(lambda: dve.reduce_max(out=V("m1").t[:], in_=V("Lm").t[:], axis=AX.X), [V("Lm")], [V("m1")])
        dv(lambda: dve.tensor_scalar(out=V("mask1").t[:], in0=V("Lm").t[:], scalar1=V("m1").t[:, 0:1], scalar2=None, op0=ALU.is_ge),
           [V("Lm"), V("m1")], [V("mask1")])
        dv(lambda: dve.scalar_tensor_tensor(out=V("Lm2").t[:], in0=V("mask1").t[:], scalar=-BIG, in1=V("Lm").t[:], op0=ALU.mult, op1=ALU.add),
           [V("mask1"), V("Lm")], [V("Lm2")])
        dv(lambda: dve.reduce_max(out=V("m2").t[:], in_=V("Lm2").t[:], axis=AX.X), [V("Lm2")], [V("m2")])
        dv(lambda: dve.tensor_scalar(out=V("mask2").t[:], in0=V("Lm2").t[:], scalar1=V("m2").t[:, 0:1], scalar2=None, op0=ALU.is_ge),
           [V("Lm2"), V("m2")], [V("mask2")])
        if i >= NT:
            dv(lambda: dve.tensor_scalar(out=V("mask1").t[:], in0=V("mask1").t[:], scalar1=C["rowvalid"].t[:, 0:1], scalar2=None, op0=ALU.mult),
               [V("mask1"), C["rowvalid"]], [V("mask1")])
            dv(lambda: dve.tensor_scalar(out=V("mask2").t[:], in0=V("mask2").t[:], scalar1=C["rowvalid"].t[:, 0:1], scalar2=None, op0=ALU.mult),
               [V("mask2"), C["rowvalid"]], [V("mask2")])
        dv(lambda: dve.tensor_tensor(out=V("dd").t[:], in0=V("m2").t[:], in1=V("m1").t[:], op=ALU.subtract), [V("m2"), V("m1")], [V("dd")])
        kb.op("act", lambda: act.activation(out=V("e2").t[:], in_=V("dd").t[:], func=AF.Exp), reads=[V("dd")], writes=[V("e2")])
        dv(lambda: dve.tensor_scalar(out=V("den").t[:], in0=V("e2").t[:], scalar1=1.0, scalar2=None, op0=ALU.add), [V("e2")], [V("den")])
        dv(lambda: dve.reciprocal(out=V("den").t[:], in_=V("den").t[:]), [V("den")], [V("den")])
        dv(lambda: dve.tensor_tensor(out=GATE.t[:, i, 0:1], in0=V("pg").t[:], in1=V("den").t[:], op=ALU.mult), [V("pg"), V("den")], [GATE])
        dv(lambda: dve.tensor_tensor(out=GATE.t[:, i, 1:2], in0=V("pg").t[:], in1=GATE.t[:, i, 0:1], op=ALU.subtract), [V("pg"), GATE], [GATE])
        msum = msums[i % 2]
        dv(lambda: dve.tensor_tensor(out=msum.t[:], in0=V("mask1").t[:], in1=V("mask2").t[:], op=ALU.add), [V("mask1"), V("mask2")], [msum])
        dv(lambda: dve.tensor_copy(out=mk1s[i % 2].t[:], in_=V("mask1").t[:]), [V("mask1")], [mk1s[i % 2]])
        dv(lambda: dve.tensor_copy(out=mk2s[i % 2].t[:], in_=V("mask2").t[:]), [V("mask2")], [mk2s[i % 2]])

    def p3_stage2b(i):
        msum = msums[i % 2]
        mkk = {"mask1": mk1s[i % 2], "mask2": mk2s[i % 2]}
        hb16 = hn2b[i % 3]
        V = lambda n: sm[n]
        dv = lambda fn, r, w: kb.op("dve", fn, reads=r, writes=w)
        kb.op("pe", lambda: pe.matmul(ps_pos.t[:], C["tri_strict_bf"].t[:], msum.t[:], start=True, stop=False),
              reads=[C["tri_strict_bf"], msum], writes=[ps_pos], signal=False)
        kb.op("pe", lambda: pe.matmul(ps_pos.t[:], C["ones_bf"].t[:], cntacc.t[:], start=False, stop=True),
              reads=[C["ones_bf"], cntacc], writes=[ps_pos])
        dv(lambda: dve.tensor_scalar(out=V("valid").t[:], in0=ps_pos.t[:], scalar1=float(CAP), scalar2=None, op0=ALU.is_lt), [ps_pos], [V("valid")])
        dv(lambda: dve.tensor_tensor(out=V("sp").t[:], in0=ps_pos.t[:], in1=C["ebase_f"].t[:], op=ALU.add), [ps_pos, C["ebase_f"]], [V("sp")])
        dv(lambda: dve.scalar_tensor_tensor(out=V("sp").t[:], in0=V("sp").t[:], scalar=-TRASH, in1=V("valid").t[:], op0=ALU.add, op1=ALU.mult),
           [V("sp"), V("valid")], [V("sp")])
        dv(lambda: dve.tensor_scalar(out=V("sp").t[:], in0=V("sp").t[:], scalar1=TRASH, scalar2=None, op0=ALU.add), [V("sp")], [V("sp")])
        for k, mk in ((0, "mask1"), (1, "mask2")):
            dv(lambda: dve.tensor_tensor(out=V("t1").t[:], in0=mkk[mk].t[:], in1=V("sp").t[:], op=ALU.mult), [mkk[mk], V("sp")], [V("t1")])
            dv(lambda: dve.reduce_sum(out=V("s1").t[:], in_=V("t1").t[:], axis=AX.X), [V("t1")], [V("s1")])
            if i >= NT:
                dv(lambda: dve.tensor_tensor(out=V("s1").t[:], in0=V("s1").t[:], in1=C["trash_p"].t[:], op=ALU.add), [V("s1"), C["trash_p"]], [V("s1")])
            dv(lambda: dve.tensor_copy(out=SLOT.t[:, i, k:k + 1], in_=V("s1").t[:]), [V("s1")], [SLOT])
        dv(lambda: dve.tensor_tensor(out=cntacc.t[:], in0=cntacc.t[:], in1=msum.t[:], op=ALU.add), [cntacc, msum], [cntacc])
        for k in range(2):
            kb.dma("pool", None, None, reads=[hb16, SLOT], writes=[XS_b],
                   fn=lambda: pool.indirect_dma_start(out=XS[:, :], out_offset=bass.IndirectOffsetOnAxis(ap=SLOT.t[:, i, k:k + 1], axis=0),
                                                      in_=hb16.t[:], in_offset=None))

    load3(0)
    cnt_f = kb.sb("cnt_f", [128, NE], F32)
    for it in range(NTOK_T + 2):
        if it < NTOK_T:
            p3_stage1(it)
        if 0 <= it - 2 < NTOK_T:
            p3_stage2b(it - 2)
        if 0 <= it - 1 < NTOK_T:
            p3_stage2(it - 1)
        if it < NTOK_T:
            p3_stage1b(it)
    kb.op("dve", lambda: dve.tensor_copy(out=cnt_f.t[:], in_=cntacc.t[:]), reads=[cntacc], writes=[cnt_f])
    kb.dma("sp", o_cnt[:, :], cnt_f.t[:], reads=[cnt_f])
    kb.end_phase()
    if stop_after <= 3:
        kb.finish()
        return nc, consts


    kb.begin_phase()
    XS_b = kb.dram("XS4", XS)
    YS_b = kb.dram("YS4", YS)
    nblk = CAP // 128
    FC = DEXP // 128
    xr = [kb.sb(f"xr{i}", [128, nblk, D], BF16) for i in range(2)]
    xsT = kb.sb("xsT", [128, DC, CAP], BF16)
    NU = 5
    wu = [kb.sb(f"wu{i}", [128, 8192], BF16) for i in range(NU)]
    gTt = kb.sb("gTt", [128, FC, CAP], BF16)
    s1t = [kb.sb(f"s1t{i}", [128, CAP], F32) for i in range(2)]
    ysb = [kb.sb(f"ysb{i}", [128, nblk, D], BF16) for i in range(2)]
    ps_a = [kb.ps(f"ps_a{i}", [128, CAP], F32) for i in range(2)]
    ps_b = [kb.ps(f"ps_b{i}", [128, CAP], F32) for i in range(2)]
    ps_y = [kb.ps(f"ps_y{i}", [128, 512], F32) for i in range(2)]
    ps_x = [kb.ps(f"ps_x{i}", [128, 4, 128], BF16) for i in range(2)]
    cy4 = {"a": 0, "y": 0, "x": 0, "s": 0}

    def cyc4(k, n):
        v = cy4[k]
        cy4[k] = (v + 1) % n
        return v

    ujobs = []
    for e in range(NE):
        for hf in range(2):
            ujobs.append(("w1", e, hf))
            ujobs.append(("w3", e, hf))
        for dmh in range(2):
            ujobs.append(("w2", e, dmh))
    ubuf_of = {}

    def issue_u(k):
        if k >= len(ujobs) or k in ubuf_of:
            return
        kind, e, hh = ujobs[k]
        ub = wu[k % NU]
        ubuf_of[k] = ub
        if kind in ("w1", "w3"):
            src = (w1_bf if kind == "w1" else w3_bf)[e].rearrange("(c p) f -> p c f", p=128)[:, :, hh * 512:(hh + 1) * 512]
            dst = ub.t[:, :].rearrange("p (c f) -> p c f", c=DC)
        else:
            src = w2_bf[e].rearrange("(c p) n -> p c n", p=128)[:, :, hh * 1024:(hh + 1) * 1024]
            dst = ub.t[:, :].rearrange("p (c n) -> p c n", c=FC)
        kb.dma("pool", dst, src, writes=[ub])

    def load_xr(e):
        if e < NE:
            kb.dma("sp", xr[e % 2].t[:], XS[e * CAP:(e + 1) * CAP, :].rearrange("(b p) d -> p b d", p=128), reads=[XS_b], writes=[xr[e % 2]])

    pool.wait_ge(cast_sem, 16 * n_cast)
    uk = 0
    for k in range(4):
        issue_u(k)
    load_xr(0)
    for e in range(NE):
        load_xr(e + 1)
        xrb = xr[e % 2]
        for b in range(nblk):
            for c4 in range(DC // 4):
                pt = ps_x[cyc4("x", 2)]
                for k in range(4):
                    c = c4 * 4 + k
                    kb.op("pe", lambda: pe.transpose(pt.t[:, k, :], xrb.t[:, b, c * 128:(c + 1) * 128], C["ident_bf"].t[:]),
                          reads=[xrb, C["ident_bf"]], writes=[pt], signal=(k == 3))
                if (b * 4 + c4) % 2 == 0:
                    kb.op("dve", lambda: dve.tensor_copy(out=xsT.t[:, c4 * 4:(c4 + 1) * 4, b * 128:(b + 1) * 128], in_=pt.t[:]), reads=[pt], writes=[xsT])
                else:
                    kb.op("act", lambda: act.activation(out=xsT.t[:, c4 * 4:(c4 + 1) * 4, b * 128:(b + 1) * 128], in_=pt.t[:], func=AF.Copy),
                          reads=[pt], writes=[xsT])
        for hf in range(2):
            issue_u(uk + 2); issue_u(uk + 3); issue_u(uk + 4)
            u1, u3 = ubuf_of[uk], ubuf_of[uk + 1]
            uk += 2
            u1v = u1.t[:, :].rearrange("p (c f) -> p c f", c=DC)
            u3v = u3.t[:, :].rearrange("p (c f) -> p c f", c=DC)
            for fl_ in range(4):
                fc = hf * 4 + fl_
                i_ = cyc4("a", 2)
                pa, pb = ps_a[i_], ps_b[i_]
                for c in range(DC):
                    kb.op("pe", lambda: pe.matmul(pa.t[:], u1v[:, c, fl_ * 128:(fl_ + 1) * 128], xsT.t[:, c, :], start=(c == 0), stop=(c == DC - 1)),
                          reads=[u1, xsT], writes=[pa], signal=(c == DC - 1))
                for c in range(DC):
                    kb.op("pe", lambda: pe.matmul(pb.t[:], u3v[:, c, fl_ * 128:(fl_ + 1) * 128], xsT.t[:, c, :], start=(c == 0), stop=(c == DC - 1)),
                          reads=[u3, xsT], writes=[pb], signal=(c == DC - 1))
                st_ = s1t[cyc4("s", 2)]
                kb.op("act", lambda: act.activation(out=st_.t[:], in_=pa.t[:], func=AF.Silu), reads=[pa], writes=[st_])
                kb.op("dve", lambda: dve.tensor_tensor(out=gTt.t[:, fc, :], in0=pb.t[:], in1=st_.t[:], op=ALU.mult), reads=[pb, st_], writes=[gTt])
        yb = ysb[e % 2]
        for dmh in range(2):
            issue_u(uk + 1); issue_u(uk + 2); issue_u(uk + 3); issue_u(uk + 4)
            u2 = ubuf_of[uk]
            uk += 1
            u2v = u2.t[:, :].rearrange("p (c n) -> p c n", c=FC)
            for b in range(nblk):
                for dn in range(2):
                    py = ps_y[cyc4("y", 2)]
                    for fc in range(FC):
                        kb.op("pe", lambda: pe.matmul(py.t[:], gTt.t[:, fc, b * 128:(b + 1) * 128], u2v[:, fc, dn * 512:(dn + 1) * 512],
                                                       start=(fc == 0), stop=(fc == FC - 1)),
                              reads=[gTt, u2], writes=[py], signal=(fc == FC - 1))
                    col = dmh * 1024 + dn * 512
                    if (b + dn) % 2 == 0:
                        kb.op("act", lambda: act.activation(out=yb.t[:, b, col:col + 512], in_=py.t[:], func=AF.Copy), reads=[py], writes=[yb])
                    else:
                        kb.op("dve", lambda: dve.tensor_copy(out=yb.t[:, b, col:col + 512], in_=py.t[:]), reads=[py], writes=[yb])
        kb.dma("sp", YS[e * CAP:(e + 1) * CAP, :].rearrange("(b p) d -> p b d", p=128), yb.t[:], reads=[yb], writes=[YS_b])
    kb.end_phase()
    if stop_after <= 4:
        kb.finish()
        return nc, consts

    kb.begin_phase()
    Hs_b = kb.dram("Hs5", Hs)
    YS_b = kb.dram("YS5", YS)
    gf_bc = kb.sb("gf_bc", [128, D], F32)
    kb.dma("sp", gf_bc.t[:], bcast_row(g_fin[0:1, :], D), writes=[gf_bc])
    h5 = [kb.sb(f"h5_{i}", [128, D], F32) for i in range(2)]
    yg = [[kb.sb(f"yg{i}_{k}", [128, D], BF16) for k in range(2)] for i in range(2)]
    t5 = kb.sb("t5", [128, D], F32)
    sq5 = kb.sb("sq5", [128, D], F32)
    ssq5 = kb.sb("ssq5", [128, 1], F32)
    rstd5 = kb.sb("rstd5", [128, 1], F32)
    yo = [kb.sb(f"yo{i}", [128, D], F32) for i in range(2)]

    def load5(i):
        kb.dma("sp", h5[i % 2].t[:], Hs[i * 128:(i + 1) * 128, :], reads=[Hs_b], writes=[h5[i % 2]])
        for k in range(2):
            yb_ = yg[i % 2][k]
            kb.dma("pool", None, None, reads=[YS_b, SLOT], writes=[yb_],
                   fn=lambda: pool.indirect_dma_start(out=yb_.t[:], out_offset=None, in_=YS[:, :],
                                                      in_offset=bass.IndirectOffsetOnAxis(ap=SLOT.t[:, i, k:k + 1], axis=0)))

    load5(0)
    for i in range(NTOK_T):
        if i + 1 < NTOK_T:
            load5(i + 1)
        hb, y1, y2, ob = h5[i % 2], yg[i % 2][0], yg[i % 2][1], yo[i % 2]
        kb.op("dve", lambda: dve.scalar_tensor_tensor(out=t5.t[:], in0=y1.t[:], scalar=GATE.t[:, i, 0:1], in1=hb.t[:], op0=ALU.mult, op1=ALU.add),
              reads=[y1, GATE, hb], writes=[t5])
        kb.op("dve", lambda: dve.scalar_tensor_tensor(out=t5.t[:], in0=y2.t[:], scalar=GATE.t[:, i, 1:2], in1=t5.t[:], op0=ALU.mult, op1=ALU.add),
              reads=[y2, GATE, t5], writes=[t5])
        kb.op("act", lambda: act.activation(out=sq5.t[:], in_=t5.t[:], func=AF.Square), reads=[t5], writes=[sq5])
        kb.op("dve", lambda: dve.reduce_sum(out=ssq5.t[:], in_=sq5.t[:], axis=AX.X), reads=[sq5], writes=[ssq5])
        kb.op("dve", lambda: dve.tensor_scalar(out=rstd5.t[:], in0=ssq5.t[:], scalar1=1.0 / D, scalar2=EPS, op0=ALU.mult, op1=ALU.add),
              reads=[ssq5], writes=[rstd5])
        kb.op("act", lambda: act.activation(out=rstd5.t[:], in_=rstd5.t[:], func=AF.Sqrt), reads=[rstd5], writes=[rstd5])
        kb.op("dve", lambda: dve.reciprocal(out=rstd5.t[:], in_=rstd5.t[:]), reads=[rstd5], writes=[rstd5])
        kb.op("dve", lambda: dve.scalar_tensor_tensor(out=ob.t[:], in0=t5.t[:], scalar=rstd5.t[:, 0:1], in1=gf_bc.t[:], op0=ALU.mult, op1=ALU.mult),
              reads=[t5, rstd5, gf_bc], writes=[ob])
        if i < NT:
            kb.dma("sp", y_p[i * 128:(i + 1) * 128, :], ob.t[:], reads=[ob])
        else:
            kb.dma("sp", y_s[:, :], ob.t[0:T, :], reads=[ob])
    kb.end_phase()
    kb.finish()
    return nc, consts


def make_in_maps(inp, cfg, n_cores):
    consts = make_consts(cfg)
    f = lambda a: np.ascontiguousarray(np.asarray(a, dtype=np.float32))
    w_rt = np.concatenate([np.asarray(inp["w_group"][0]), np.asarray(inp["w_router"][0])], axis=1)
    b_rt = np.concatenate([np.asarray(inp["b_group"][0]), np.asarray(inp["b_router"][0])], axis=0)[None, :]
    shared = dict(
        w_in=f(inp["w_in"][0]), b_f=f(inp["b_f"]), g_attn=f(inp["g_attn"]), g_of=f(inp["g_out_fox"]),
        g_os=f(inp["g_out_sb"]), w_out=f(inp["w_out"][0]), g_ffn=f(inp["g_ffn"]), w_rt=f(w_rt), b_rt=f(b_rt),
        w1=f(inp["w1"][0]), w3=f(inp["w3"][0]), w2=f(inp["w2"][0]), g_fin=f(np.asarray(inp["g_final"])[None, :]),
    )
    for k, v in consts.items():
        shared["k_" + k] = v
    maps = []
    for b in range(n_cores):
        m = dict(shared)
        m["x_p"] = f(inp["x_prompt"][b])
        m["x_s"] = f(inp["x_sample"][b])
        m["c_fk"] = f(np.asarray(inp["cache_fox_k"][0, b]).reshape(cfg["PAST"], 1024))
        m["c_fv"] = f(np.asarray(inp["cache_fox_v"][0, b]).reshape(cfg["PAST"], 1024))
        m["c_fl"] = f(inp["cache_fox_logf"][0, b])
        m["c_sk"] = f(np.asarray(inp["cache_sb_k"][0, b]).reshape(cfg["PAST"], 1024))
        m["c_sv"] = f(np.asarray(inp["cache_sb_v"][0, b]).reshape(cfg["PAST"], 1024))
        maps.append(m)
    return maps


def assemble(results, cfg):
    S, T = cfg["S"], cfg["T"]
    B = len(results)
    st = lambda k, shp: np.stack([np.asarray(r[k]).reshape(shp) for r in results], axis=0)
    y_p = st("y_p", (S, D)); y_s = st("y_s", (T, D))
    outs = [y_p, y_s]
    for pre, n in (("o_p", S), ("o_s", T)):
        outs.append(st(pre + "fk", (n, 8, 128))[None])
        outs.append(st(pre + "fv", (n, 8, 128))[None])
        outs.append(st(pre + "fl", (n, 8))[None])
        outs.append(st(pre + "sk", (n, 8, 128))[None])
        outs.append(st(pre + "sv", (n, 8, 128))[None])
    return tuple(np.ascontiguousarray(o.astype(np.float32)) for o in outs)


def kernel(**inputs):
    cfg = default_cfg()
    nc, _ = build(cfg)
    maps = make_in_maps(inputs, cfg, 8)
    res = run_bass_kernel_spmd(nc, maps, core_ids=list(range(8)))
    try:
        import os
        if os.environ.get("MK_DEBUG_CNT"):
            np.save(os.environ["MK_DEBUG_CNT"], np.stack([np.asarray(r["o_cnt"]).sum(0) for r in res.results]))
    except Exception:
        pass
    return assemble(res.results, cfg)
```

```python
import contextlib
import numpy as np
import ml_dtypes
import concourse.bass as bass
import concourse.mybir as mybir
from concourse.bass_utils import run_bass_kernel_spmd

F32 = mybir.dt.float32
BF16 = mybir.dt.bfloat16
I32 = mybir.dt.int32
AF = mybir.ActivationFunctionType
ALU = mybir.AluOpType
AX = mybir.AxisListType

D = 2048
DC = D // 128
HD = 128
NH = 16
NFH = 8
IN_COLS = 6152
EPS = 1e-6
DEXP = 1024


def default_cfg():
    return dict(S=4096, T=64, PAST=1024, NG=4, EPG=8, CAP=384, debug=False, stop_after=99)


class Buf:
    __slots__ = ("name", "t", "w", "rs", "sem", "cnt", "is_dram", "is_psum")

    def __init__(self, name, t, is_dram=False, is_psum=False):
        self.is_psum = is_psum
        self.name = name
        self.t = t
        self.w = None
        self.rs = {}
        self.sem = None
        self.cnt = 0
        self.is_dram = is_dram


EPOCH = 30000


class KB:
    COMPUTE = ("pe", "act", "dve", "pool")

    def __init__(self, nc):
        self.nc = nc
        self.eng = {"pe": nc.tensor, "act": nc.scalar, "dve": nc.vector, "pool": nc.gpsimd, "sp": nc.sync}
        self.cnt = {e: 0 for e in self.COMPUTE}
        self.seen = {}
        self.outer = contextlib.ExitStack()
        self.phase_stack = None
        self.sems = {}
        self.dma_toks = []
        self.nbuf = 0
        self.pbufs = []
        self.limit = 10 ** 9
        self.nops = 0
        self.trace = None
        self.free_sems = []
        self.phase_sem_bufs = []
        for e in self.COMPUTE:
            for ep in range(4):
                self._csem(e, ep)

    def _ctx(self, persistent):
        return self.outer if persistent else self.phase_stack

    def sb(self, name, shape, dt, persistent=False):
        t = self._ctx(persistent).enter_context(self.nc.sbuf_tensor(name, list(shape), dt))
        b = Buf(name, t)
        if persistent:
            self.pbufs.append(b)
        return b

    def ps(self, name, shape, dt, persistent=False):
        t = self._ctx(persistent).enter_context(self.nc.psum_tensor(name, list(shape), dt))
        return Buf(name, t, is_psum=True)

    def dram(self, name, t):
        b = Buf(name, t, is_dram=True)
        self.pbufs.append(b)
        return b

    def _sem(self, name, persistent=False):
        return self.outer.enter_context(self.nc.semaphore(name))

    def _dma_sem(self, b):
        if self.free_sems:
            sem, cnt = self.free_sems.pop(0)
        else:
            sem, cnt = self._sem(f"d_{self.nbuf}"), 0
            self.nbuf += 1
        b.sem = sem
        b.cnt = cnt
        self.phase_sem_bufs.append(b)

    def _csem(self, eng, epoch):
        k = (eng, epoch)
        if k not in self.sems:
            self.sems[k] = self._sem(f"c_{eng}_{epoch}", persistent=True)
        return self.sems[k]

    def _wait(self, eng, toks):
        for tok in toks:
            if tok is None:
                continue
            kind = tok[0]
            if kind == "c":
                _, src, epoch, val = tok
                if src == eng and eng == "pe":
                    continue
                key = ("c", src, epoch)
                sem = self._csem(src, epoch)
            else:
                _, sem, val, semid = tok
                key = ("d", semid)
            if self.seen.get((eng, key), 0) >= val:
                continue
            self.seen[(eng, key)] = val
            self.eng[eng].wait_ge(sem, val)

    def _deps(self, reads, writes):
        deps = []
        for b in reads:
            if b.w is not None:
                deps.append(b.w)
            if b.is_psum:
                deps.extend(b.rs.values())
        for b in writes:
            if b.w is not None:
                deps.append(b.w)
            deps.extend(b.rs.values())
        return deps

    @staticmethod
    def _tkey(tok):
        return tok[:3] if tok[0] == "c" else ("d", tok[3])

    def _commit(self, tok, reads, writes):
        k = self._tkey(tok)
        for b in reads:
            old = b.rs.get(k)
            if old is None or old[-1 if tok[0] == "c" else 2] <= tok[-1 if tok[0] == "c" else 2]:
                b.rs[k] = tok
        for b in writes:
            b.w = tok
            b.rs = {}

    def op(self, eng, fn, reads=(), writes=(), signal=True, extra=()):
        self.nops += 1
        if self.nops > self.limit:
            return None
        if self.trace is not None:
            import sys as _s
            f = _s._getframe(1)
            self.trace.append((self.nops, eng, f.f_lineno, f.f_code.co_name))
        self._wait(eng, self._deps(reads, writes) + list(extra))
        inst = fn()
        n = self.cnt[eng]
        if signal:
            n += 1
            self.cnt[eng] = n
            epoch, val = divmod(n - 1, EPOCH)
            inst.then_inc(self._csem(eng, epoch), 1)
            tok = ("c", eng, epoch, val + 1)
        else:
            epoch, val = divmod(n, EPOCH)
            tok = ("c", eng, epoch, val + 1)
        self._commit(tok, reads, writes)
        return tok

    def dma(self, q, out_ap, in_ap, reads=(), writes=(), fn=None, extra=()):
        self.nops += 1
        if self.nops > self.limit:
            return None
        if self.trace is not None:
            import sys as _s
            f = _s._getframe(1)
            self.trace.append((self.nops, "dma-" + q, f.f_lineno, f.f_code.co_name))
        self._wait(q, self._deps(reads, writes) + list(extra))
        cands = [b for b in list(writes) + list(reads) if not b.is_dram]
        b = cands[0] if cands else (list(writes) + list(reads))[0]
        if b.sem is None:
            self._dma_sem(b)
        sem = b.sem
        b.cnt += 16
        val = b.cnt
        if fn is None:
            inst = self.eng[q].dma_start(out=out_ap, in_=in_ap)
        else:
            inst = fn()
        inst.then_inc(sem, 16)
        tok = ("d", sem, val, id(sem))
        self.dma_toks.append(tok)
        self._commit(tok, reads, writes)
        return tok

    def begin_phase(self):
        self.phase_stack = contextlib.ExitStack()
        self.dma_toks = []

    def end_phase(self):
        toks = list(self.dma_toks)
        for e in self.COMPUTE:
            n = self.cnt[e]
            if n > 0:
                epoch, val = divmod(n - 1, EPOCH)
                toks.append(("c", e, epoch, val + 1))
        for e in ("pe", "act", "dve", "pool", "sp"):
            if self.nops <= self.limit:
                self._wait(e, toks)
        self.phase_stack.close()
        self.phase_stack = None
        self.dma_toks = []
        for b in self.phase_sem_bufs:
            self.free_sems.append((b.sem, b.cnt))
            b.sem = None
            b.cnt = 0
        self.phase_sem_bufs = []
        for b in self.pbufs:
            b.sem = None
            b.cnt = 0
            b.w = None
            b.rs = {}

    def finish(self):
        self.outer.close()


def make_consts(cfg):
    bf = ml_dtypes.bfloat16
    p = np.arange(128)
    c = {}
    c["ident_bf"] = np.eye(128, dtype=np.float32).astype(bf)
    c["ident_f"] = np.eye(128, dtype=np.float32)
    c["ones_f"] = np.ones((128, 128), np.float32)
    c["ones_bf"] = np.ones((128, 128), np.float32).astype(bf)
    c["tri_incl_f"] = (p[:, None] <= p[None, :]).astype(np.float32)
    c["tri_strict_f"] = (p[:, None] < p[None, :]).astype(np.float32)
    c["sel0_f"] = np.zeros((128, 128), np.float32)
    c["sel0_f"][0, :] = 1.0
    q = np.arange(512)
    mf = np.zeros((128, 4, 512), np.float32)
    ms = np.zeros((128, 4, 512), np.float32)
    for r in range(4):
        mf[:, r, :] = (q[None, :] >= 128 * r + p[:, None])
        ms[:, r, :] = (q[None, :] > 128 * r + p[:, None])
    c["mask_fox"] = mf.astype(bf)
    c["mask_sb"] = ms.astype(bf)
    c["negtri_bf"] = (-(p[:, None] >= p[None, :]).astype(np.float32)).astype(bf)
    c["negcompl_bf"] = (-(p[:, None] < p[None, :]).astype(np.float32)).astype(bf)
    NE = cfg["NG"] * cfg["EPG"]
    c["ebase_f"] = np.tile((np.arange(NE, dtype=np.float32) * cfg["CAP"])[None, :], (128, 1))
    c["eidx_f"] = np.tile(np.arange(NE, dtype=np.float32)[None, :], (128, 1))
    c["tri_strict_bf"] = c["tri_strict_f"].astype(bf)
    rvv = (p < cfg["T"]).astype(np.float32)
    c["rowvalid"] = rvv[:, None].copy()
    c["trash_p"] = ((1.0 - rvv) * (NE * cfg["CAP"] + p))[:, None].astype(np.float32)
    return c


CONST_DT = {"tri_strict_bf": BF16, "ident_bf": BF16, "ones_bf": BF16, "mask_fox": BF16, "mask_sb": BF16, "negtri_bf": BF16,
            "negcompl_bf": BF16}


def build(cfg):
    S, T, PAST = cfg["S"], cfg["T"], cfg["PAST"]
    NG, EPG, CAP = cfg["NG"], cfg["EPG"], cfg["CAP"]
    NE = NG * EPG
    NT = S // 128
    NPT = PAST // 128
    NTS = NPT + 1
    SS = NTS * 128
    NTOK_T = NT + 1
    debug = cfg["debug"]
    stop_after = cfg["stop_after"]

    nc = bass.Bass("TRN2", target_bir_lowering=False)
    kb = KB(nc)
    kb.limit = cfg.get("limit", 10 ** 9)
    global LAST_KB
    LAST_KB = kb
    if cfg.get("trace"):
        kb.trace = []

    def din(name, shape, dt=F32):
        return nc.dram_tensor(name, list(shape), dt, kind="ExternalInput").ap()

    def dout(name, shape, dt=F32):
        return nc.dram_tensor(name, list(shape), dt, kind="ExternalOutput").ap()

    def dscr(name, shape, dt):
        kind = "ExternalOutput" if debug else "Internal"
        return nc.dram_tensor(name, list(shape), dt, kind=kind).ap()

    x_p = din("x_p", [S, D])
    x_s = din("x_s", [T, D])
    c_fk = din("c_fk", [PAST, 1024])
    c_fv = din("c_fv", [PAST, 1024])
    c_fl = din("c_fl", [PAST, 8])
    c_sk = din("c_sk", [PAST, 1024])
    c_sv = din("c_sv", [PAST, 1024])
    w_in = din("w_in", [D, IN_COLS])
    b_f = din("b_f", [1, 8])
    g_attn = din("g_attn", [1, D])
    g_of = din("g_of", [1, 1024])
    g_os = din("g_os", [1, 1024])
    w_out = din("w_out", [D, D])
    g_ffn = din("g_ffn", [1, D])
    w_rt = din("w_rt", [D, NG + NE])
    b_rt = din("b_rt", [1, NG + NE])
    w1 = din("w1", [NE, D, DEXP])
    w3 = din("w3", [NE, D, DEXP])
    w2 = din("w2", [NE, DEXP, D])
    g_fin = din("g_fin", [1, D])
    consts = make_consts(cfg)
    cin = {k: din("k_" + k, v.shape, CONST_DT.get(k, F32)) for k, v in consts.items()}

    y_p = dout("y_p", [S, D])
    y_s = dout("y_s", [T, D])
    o_pfk = dout("o_pfk", [S, 1024]); o_pfv = dout("o_pfv", [S, 1024]); o_pfl = dout("o_pfl", [S, 8])
    o_psk = dout("o_psk", [S, 1024]); o_psv = dout("o_psv", [S, 1024])
    o_sfk = dout("o_sfk", [T, 1024]); o_sfv = dout("o_sfv", [T, 1024]); o_sfl = dout("o_sfl", [T, 8])
    o_ssk = dout("o_ssk", [T, 1024]); o_ssv = dout("o_ssv", [T, 1024])

    o_cnt = dout("o_cnt", [128, NE])
    w_in_bf = dscr("w_in_bf", [D, IN_COLS], BF16) if False else nc.dram_tensor("w_in_bf", [D, IN_COLS], BF16, kind="Internal").ap()
    QT = {"p": dscr("QT_p", [NH, 128, S], BF16), "s": dscr("QT_s", [NH, 128, 128], BF16)}
    KT = {"p": dscr("KT_p", [NH, 128, S], BF16), "s": dscr("KT_s", [NH, 128, SS], BF16)}
    VV = {"p": dscr("VV_p", [S, D], BF16), "s": dscr("VV_s", [SS, D], BF16)}
    NFT = dscr("NFT", [D, NTOK_T * 128], BF16)
    Hs = dscr("Hs", [NTOK_T * 128, D], F32)
    w1_bf = nc.dram_tensor("w1_bf", [NE, D, DEXP], BF16, kind="Internal").ap()
    w3_bf = nc.dram_tensor("w3_bf", [NE, D, DEXP], BF16, kind="Internal").ap()
    w2_bf = nc.dram_tensor("w2_bf", [NE, DEXP, D], BF16, kind="Internal").ap()
    NSLOT = NE * CAP
    XS = dscr("XS", [NSLOT + 128, D], BF16)
    YS = dscr("YS", [NSLOT + 128, D], BF16)

    E = kb.eng
    pe, act, dve, pool, sp = E["pe"], E["act"], E["dve"], E["pool"], E["sp"]

    kb.begin_phase()
    C = {}
    for k, v in consts.items():
        C[k] = kb.sb("c_" + k, v.shape, CONST_DT.get(k, F32), persistent=True)
        kb.dma("sp", C[k].t[:], cin[k][:], writes=[C[k]])
    NTA = NT + NTS
    LF = kb.sb("LF", [128, NTA, 8], F32, persistent=True)
    CB = kb.sb("CB", [128, NTA, 8], F32, persistent=True)
    CREF = kb.sb("CREF", [128, NTA, 8], F32, persistent=True)
    bf_bc = kb.sb("bf_bc", [128, 8], F32, persistent=True)

    def bcast_row(ap_row, n):
        return ap_row.broadcast_to([128, n])

    kb.dma("sp", bf_bc.t[:], bcast_row(b_f[0:1, :], 8), writes=[bf_bc])


    g_bc = kb.sb("g_bc", [128, D], F32)
    kb.dma("sp", g_bc.t[:], bcast_row(g_attn[0:1, :], D), writes=[g_bc])
    xt = [kb.sb(f"xt{i}", [128, D], F32) for i in range(2)]
    sq_junk = kb.sb("sq_junk", [128, D], BF16)
    ssq = kb.sb("ssq", [128, 1], F32)
    rstd = kb.sb("rstd", [128, 1], F32)
    hn = kb.sb("hn", [128, D], BF16)
    hnTs = [[kb.sb(f"hnT{a}_{i}", [128, DC, 128], BF16) for i in range(4)] for a in range(2)]
    wch = [kb.sb(f"wch{i}", [128, DC, 512], BF16) for i in range(3)]
    wfl = kb.sb("wfl", [128, DC, 8], BF16)
    ps_tr = [kb.ps(f"ps_tr{i}", [128, 4, 128], BF16) for i in range(2)]
    ps_mm = [kb.ps(f"ps_mm{i}", [128, 512], F32) for i in range(3)]
    ps_fl_t = kb.ps("ps_fl", [128, 4, 8], F32)
    ps_fl = ps_fl_t
    ps_flb = [Buf(f"ps_flb{i}", ps_fl_t.t, is_psum=True) for i in range(4)]
    ps_t2 = [kb.ps(f"ps_t2{i}", [128, 4, 128], BF16) for i in range(2)]
    st_f = [kb.sb(f"st_f{i}", [128, 512], F32) for i in range(3)]
    st_b = [kb.sb(f"st_b{i}", [128, 512], BF16) for i in range(3)]
    QTst = kb.sb("QTst", [128, NH, 512], BF16)
    KTst = kb.sb("KTst", [128, NH, 512], BF16)
    fl_t = kb.sb("fl_t", [128, 8], F32)
    lfacc = kb.sb("lfacc", [128, 8], F32)
    cst_f = [kb.sb(f"cst_f{i}", [128, 1024], F32) for i in range(2)]
    cst_b = [kb.sb(f"cst_b{i}", [128, 1024], BF16) for i in range(2)]
    KTc = kb.sb("KTc", [128, NH, 128], BF16)

    SCALE = float(HD) ** -0.5
    chunks = []
    for kind, c0, h0 in (("q", 0, 0), ("k", 1024, 0), ("v", 2048, 0)):
        for i in range(2):
            chunks.append((c0 + 512 * i, 512, kind, h0 + 4 * i))
    chunks.append((3072, 8, "fl", 0))
    for kind, c0, h0 in (("q", 3080, 8), ("k", 4104, 8), ("v", 5128, 8)):
        for i in range(2):
            chunks.append((c0 + 512 * i, 512, kind, h0 + 4 * i))
    w_in_v = w_in_bf.rearrange("(c p) n -> p c n", p=128)
    w_in_bf_bufs = []
    for ci_, (c0_, cw_, kind_, h0_) in enumerate(chunks):
        bb = kb.dram(f"w_in_bf{ci_}", w_in_bf)
        w_in_bf_bufs.append(bb)
        if cw_ >= 512:
            for hh in range(2):
                r0, r1 = hh * (D // 2), (hh + 1) * (D // 2)
                kb.dma("pool", w_in_bf[r0:r1, c0_:c0_ + cw_], w_in[r0:r1, c0_:c0_ + cw_], writes=[bb])
        else:
            with nc.allow_non_contiguous_dma(reason="8-column forget-gate slice"):
                kb.dma("pool", w_in_bf[:, c0_:c0_ + cw_], w_in[:, c0_:c0_ + cw_], writes=[bb])

    rr = {"x": 0, "tr": 0, "mm": 0, "t2": 0, "sf": 0, "sb": 0, "w": 0, "c": 0}

    def nxt(key, n):
        v = rr[key]
        rr[key] = (v + 1) % n
        return v

    def rmsnorm_tile(xb, g_b, out_b, rows=128):
        kb.op("act", lambda: act.activation(out=sq_junk.t[:], in_=xb.t[:], func=AF.Square), reads=[xb], writes=[sq_junk])
        kb.op("dve", lambda: dve.reduce_sum(out=ssq.t[:], in_=sq_junk.t[:], axis=AX.X), reads=[sq_junk], writes=[ssq])
        kb.op("dve", lambda: dve.tensor_scalar(out=rstd.t[:], in0=ssq.t[:], scalar1=1.0 / D, scalar2=EPS,
                                                op0=ALU.mult, op1=ALU.add), reads=[ssq], writes=[rstd])
        kb.op("act", lambda: act.activation(out=rstd.t[:], in_=rstd.t[:], func=AF.Sqrt), reads=[rstd], writes=[rstd])
        kb.op("dve", lambda: dve.reciprocal(out=rstd.t[:], in_=rstd.t[:]), reads=[rstd], writes=[rstd])
        kb.op("dve", lambda: dve.scalar_tensor_tensor(out=out_b.t[:], in0=xb.t[:], scalar=rstd.t[:, 0:1], in1=g_b.t[:],
                                                      op0=ALU.mult, op1=ALU.mult), reads=[xb, rstd, g_b], writes=[out_b])

    def transpose_to(dst_b, dst_ap_fn, src_b, src_ap_fn, n, ident, evac="dve", psl=None):
        pt = ps_tr[nxt("tr", 2)] if psl is None else psl[nxt("t2", 2)]
        for i in range(n):
            kb.op("pe", lambda i=i: pe.transpose(pt.t[:, i, :], src_ap_fn(i), ident.t[:]),
                  reads=[src_b, ident], writes=[pt], signal=(i == n - 1))
        if evac == "dve":
            kb.op("dve", lambda: dve.tensor_copy(out=dst_ap_fn(), in_=pt.t[:, 0:n, :]), reads=[pt], writes=[dst_b])
        else:
            kb.op("act", lambda: act.activation(out=dst_ap_fn(), in_=pt.t[:, 0:n, :], func=AF.Copy), reads=[pt], writes=[dst_b])

    def logf_from_psum(ti_all, rows_valid, out_ap, pbuf, li):
        kb.op("dve", lambda: dve.tensor_tensor(out=fl_t.t[:], in0=pbuf.t[:, li, :], in1=bf_bc.t[:], op=ALU.add),
              reads=[pbuf, bf_bc], writes=[fl_t])
        kb.op("act", lambda: act.activation(out=fl_t.t[:], in_=fl_t.t[:], func=AF.Exp, scale=-1.0), reads=[fl_t], writes=[fl_t])
        kb.op("act", lambda: act.activation(out=fl_t.t[:], in_=fl_t.t[:], func=AF.Ln, bias=1.0), reads=[fl_t], writes=[fl_t])
        kb.op("dve", lambda: dve.tensor_scalar(out=LF.t[:, ti_all, :], in0=fl_t.t[:], scalar1=-1.0, scalar2=None, op0=ALU.mult),
              reads=[fl_t], writes=[LF])
        kb.dma("sp", out_ap, LF.t[0:rows_valid, ti_all, :], reads=[LF])

    groups = [("p", list(range(g * 4, min(NT, g * 4 + 4)))) for g in range((NT + 3) // 4)] + [("s", [NPT])]
    outs = {"p": dict(fk=o_pfk, fv=o_pfv, fl=o_pfl, sk=o_psk, sv=o_psv),
            "s": dict(fk=o_sfk, fv=o_sfv, fl=o_sfl, sk=o_ssk, sv=o_ssv)}
    QT_b = {s: kb.dram("QT" + s, QT[s]) for s in "ps"}
    KT_b = {s: kb.dram("KT" + s, KT[s]) for s in "ps"}
    VV_b = {s: kb.dram("VV" + s, VV[s]) for s in "ps"}

    xjobs = [(seq, ti) for (seq, tiles) in groups for ti in tiles]
    xbuf_of = {}

    def issue_x(m):
        if m >= len(xjobs) or m in xbuf_of:
            return
        seq, ti = xjobs[m]
        xb = xt[m % 2]
        xbuf_of[m] = xb
        if seq == "p":
            kb.dma("sp", xb.t[:], x_p[ti * 128:(ti + 1) * 128, :], writes=[xb])
        else:
            kb.op("dve", lambda: dve.memset(xb.t[:], 0.0), writes=[xb])
            kb.dma("sp", xb.t[0:T, :], x_s[:, :], writes=[xb])

    wjobs = [(gi, ci) for gi in range(len(groups)) for ci in range(len(chunks))]
    wbuf_of = {}
    wslot = [0]

    def issue_w(k):
        if k >= len(wjobs) or k in wbuf_of:
            return
        gi, ci = wjobs[k]
        c0, cw, kind, h0 = chunks[ci]
        if kind == "fl":
            wb = wfl
        else:
            wb = wch[wslot[0] % 3]
            wslot[0] += 1
        wbuf_of[k] = wb
        kb.dma("sp", wb.t[:, :, 0:cw], w_in_v[:, :, c0:c0 + cw], reads=[w_in_bf_bufs[ci]], writes=[wb])

    pending = []

    def flush_pending():
        while pending:
            pending.pop(0)()

    wk = 0
    xidx = {}
    m_ = 0
    for gi_, (seq_, tiles_) in enumerate(groups):
        for li_, ti_ in enumerate(tiles_):
            xidx[(gi_, li_)] = m_
            m_ += 1

    def prep_norm(gi, li):
        m = xidx[(gi, li)]
        issue_x(m)
        issue_x(m + 1)
        rmsnorm_tile(xbuf_of[m], g_bc, hn)

    def prep_tr(gi, li):
        dst = hnTs[gi % 2][li]
        for c4 in range(DC // 4):
            transpose_to(dst, lambda c4=c4: dst.t[:, c4 * 4:(c4 + 1) * 4, :], hn,
                         lambda i, c4=c4: hn.t[:, (c4 * 4 + i) * 128:(c4 * 4 + i + 1) * 128], 4, C["ident_bf"],
                         evac=("dve" if c4 % 2 == 0 else "act"))

    cjobs = [(j, src, isk, h0) for j in range(NPT)
             for (src, isk, h0) in ((c_fk, True, 0), (c_sk, True, 8), (c_fv, False, 0), (c_sv, False, 8))]

    def cache_part_a(k):
        j, src, isk, h0 = cjobs[k]
        r0 = j * 128
        if h0 == 0 and isk:
            kb.dma("sp", LF.t[:, NT + j, :], c_fl[r0:r0 + 128, :], writes=[LF])
        cf, cb_ = cst_f[k % 2], cst_b[k % 2]
        kb.dma("sp", cf.t[:], src[r0:r0 + 128, :], writes=[cf])
        kb.op("dve", lambda: dve.tensor_copy(out=cb_.t[:], in_=cf.t[:]), reads=[cf], writes=[cb_])

    def cache_part_b(k):
        j, src, isk, h0 = cjobs[k]
        r0 = j * 128
        cb_ = cst_b[k % 2]
        if isk:
            for q4 in range(2):
                transpose_to(KTc, lambda q4=q4: KTc.t[:, h0 + q4 * 4:h0 + q4 * 4 + 4, :], cb_,
                             lambda i, q4=q4: cb_.t[:, (q4 * 4 + i) * 128:(q4 * 4 + i + 1) * 128], 4, C["ident_bf"],
                             evac="act", psl=ps_t2)
            if h0 == 8:
                kb.dma("sp", KT["s"][:, :, r0:r0 + 128].rearrange("h d t -> d h t"), KTc.t[:], reads=[KTc], writes=[KT_b["s"]])
        else:
            kb.dma("sp", VV["s"][r0:r0 + 128, h0 * 128:h0 * 128 + 1024], cb_.t[:], reads=[cb_], writes=[VV_b["s"]])

    def cache_step():
        k = cache_done[0]
        if k > len(cjobs):
            return
        if k >= 1:
            cache_part_b(k - 1)
        if k < len(cjobs):
            cache_part_a(k)
        cache_done[0] += 1

    cache_done = [0]

    issue_x(0)
    for li0 in range(len(groups[0][1])):
        prep_norm(0, li0)
        prep_tr(0, li0)
    for gi, (seq, tiles) in enumerate(groups):
        ntl = len(tiles)
        hnT = hnTs[gi % 2]
        nxt_tiles = len(groups[gi + 1][1]) if gi + 1 < len(groups) else 0
        for ci, (c0, cw, kind, h0) in enumerate(chunks):
            issue_w(wk); issue_w(wk + 1); issue_w(wk + 2)
            wb = wbuf_of[wk]
            wk += 1
            for li, ti in enumerate(tiles):
                rows = 128 if seq == "p" else T
                r0 = ti * 128 if seq == "p" else 0
                ti_all = ti if seq == "p" else NT + ti
                pm = ps_flb[li] if kind == "fl" else ps_mm[nxt("mm", 3)]
                pm_ap = pm.t[:, li, :] if kind == "fl" else pm.t[:, 0:cw]
                for c in range(DC):
                    kb.op("pe", lambda c=c, pm_ap=pm_ap, wb=wb, li=li: pe.matmul(pm_ap, hnT[li].t[:, c, :], wb.t[:, c, 0:cw],
                                                                               start=(c == 0), stop=(c == DC - 1)),
                          reads=[hnT[li], wb], writes=[pm], signal=(c == DC - 1))
                flush_pending()
                fox = h0 < NFH
                if kind == "fl":
                    logf_from_psum(ti_all, rows, outs[seq]["fl"][r0:r0 + rows, :], pm, li)
                elif kind == "q":
                    sb_ = st_b[nxt("sb", 3)]
                    kb.op("act", lambda: act.activation(out=sb_.t[:], in_=pm.t[:], func=AF.Copy, scale=SCALE),
                          reads=[pm], writes=[sb_])
                    pending.append(lambda sb_=sb_, h0=h0, li=li: transpose_to(
                        QTst, lambda: QTst.t[:, h0:h0 + 4, li * 128:(li + 1) * 128], sb_,
                        lambda i: sb_.t[:, i * 128:(i + 1) * 128], 4, C["ident_bf"], evac="dve", psl=ps_t2))
                elif kind == "k":
                    sf_ = st_f[nxt("sf", 3)]
                    sb_ = st_b[nxt("sb", 3)]
                    kb.op("act", lambda: act.activation(out=sf_.t[:], in_=pm.t[:], func=AF.Copy), reads=[pm], writes=[sf_])
                    kb.op("dve", lambda: dve.tensor_copy(out=sb_.t[:], in_=sf_.t[:]), reads=[sf_], writes=[sb_])
                    oc = (h0 % NFH) * 128
                    kb.dma("sp", outs[seq]["fk" if fox else "sk"][r0:r0 + rows, oc:oc + 512], sf_.t[0:rows, :], reads=[sf_])
                    pending.append(lambda sb_=sb_, h0=h0, li=li: transpose_to(
                        KTst, lambda: KTst.t[:, h0:h0 + 4, li * 128:(li + 1) * 128], sb_,
                        lambda i: sb_.t[:, i * 128:(i + 1) * 128], 4, C["ident_bf"], evac="act", psl=ps_t2))
                else:
                    sf_ = st_f[nxt("sf", 3)]
                    sb_ = st_b[nxt("sb", 3)]
                    kb.op("act", lambda: act.activation(out=sf_.t[:], in_=pm.t[:], func=AF.Copy), reads=[pm], writes=[sf_])
                    kb.op("dve", lambda: dve.tensor_copy(out=sb_.t[:], in_=sf_.t[:]), reads=[sf_], writes=[sb_])
                    oc = (h0 % NFH) * 128
                    kb.dma("sp", outs[seq]["fv" if fox else "sv"][r0:r0 + rows, oc:oc + 512], sf_.t[0:rows, :], reads=[sf_])
                    kb.dma("sp", VV[seq][ti * 128:(ti + 1) * 128, h0 * 128:h0 * 128 + 512], sb_.t[:], reads=[sb_], writes=[VV_b[seq]])
            if ci >= 5:
                cache_step()
            if ci >= 1 and ci - 1 < nxt_tiles:
                prep_tr(gi + 1, ci - 1)
            if ci < nxt_tiles:
                prep_norm(gi + 1, ci)
        flush_pending()
        t0 = tiles[0] * 128
        if seq == "p":
            kb.dma("sp", QT["p"][:, :, t0:t0 + ntl * 128].rearrange("h d t -> d h t"), QTst.t[:, :, 0:ntl * 128],
                   reads=[QTst], writes=[QT_b["p"]])
            kb.dma("sp", KT["p"][:, :, t0:t0 + ntl * 128].rearrange("h d t -> d h t"), KTst.t[:, :, 0:ntl * 128],
                   reads=[KTst], writes=[KT_b["p"]])
        else:
            kb.dma("sp", QT["s"][:, :, :].rearrange("h d t -> d h t"), QTst.t[:, :, 0:128], reads=[QTst], writes=[QT_b["s"]])
            kb.dma("sp", KT["s"][:, :, t0:t0 + 128].rearrange("h d t -> d h t"), KTst.t[:, :, 0:128],
                   reads=[KTst], writes=[KT_b["s"]])

    while cache_done[0] <= len(cjobs):
        cache_step()

    ps_c = ps_flb[0]
    for (a0, n) in ((0, NT), (NT, NTS)):
        for j in range(n):
            ja = a0 + j
            if j == 0:
                kb.op("dve", lambda: dve.memset(lfacc.t[:], 0.0), writes=[lfacc])
            kb.op("pe", lambda: pe.matmul(ps_c.t[:, 0, :], C["tri_incl_f"].t[:], LF.t[:, ja, :], start=True, stop=False),
                  reads=[C["tri_incl_f"], LF], writes=[ps_c], signal=False)
            kb.op("pe", lambda: pe.matmul(ps_c.t[:, 0, :], C["ones_f"].t[:], lfacc.t[:], start=False, stop=True),
                  reads=[C["ones_f"], lfacc], writes=[ps_c])
            kb.op("dve", lambda: dve.tensor_copy(out=CB.t[:, ja, :], in_=ps_c.t[:, 0, :]), reads=[ps_c], writes=[CB])
            kb.op("dve", lambda: dve.tensor_tensor(out=lfacc.t[:], in0=lfacc.t[:], in1=LF.t[:, ja, :], op=ALU.add),
                  reads=[lfacc, LF], writes=[lfacc])
    ps_cr = ps_mm[0]
    kb.op("pe", lambda: pe.matmul(ps_cr.t[:, 0:NTA * 8], C["sel0_f"].t[:], CB.t[:].rearrange("p a h -> p (a h)"), start=True, stop=True),
          reads=[C["sel0_f"], CB], writes=[ps_cr])
    kb.op("dve", lambda: dve.tensor_copy(out=CREF.t[:].rearrange("p a h -> p (a h)"), in_=ps_cr.t[:, 0:NTA * 8]), reads=[ps_cr], writes=[CREF])
    kb.end_phase()

    if stop_after <= 1:
        kb.finish()
        return nc, consts


    kb.begin_phase()
    NFT_b = kb.dram("NFT", NFT)
    gT = kb.sb("gT", [128, NH], F32)
    with nc.allow_non_contiguous_dma(reason="tiny per-head gain transpose"):
        kb.dma("sp", gT.t[:, 0:8], g_of[0:1, :].rearrange("o (h d) -> d (o h)", d=128), writes=[gT])
        kb.dma("sp", gT.t[:, 8:16], g_os[0:1, :].rearrange("o (h d) -> d (o h)", d=128), writes=[gT])
    cast_sem = kb._sem("cast_sem")
    PRE0 = NE // 2
    cast_jobs = []
    for e in range(PRE0, NE):
        for (src, dst, rows) in ((w1, w1_bf, D), (w3, w3_bf, D), (w2, w2_bf, DEXP)):
            for hh in range(2):
                r0, r1 = hh * (rows // 2), (hh + 1) * (rows // 2)
                cast_jobs.append((dst[e, r0:r1, :], src[e, r0:r1, :]))
    n_cast = len(cast_jobs)
    last_act_tok = [None]

    def issue_cast():
        if cast_jobs:
            dst, src = cast_jobs.pop(0)
            if last_act_tok[0] is not None:
                kb._wait("pool", [last_act_tok[0]])
            pool.dma_start(out=dst, in_=src).then_inc(cast_sem, 16)

    MAXTOK = max(S, SS)
    ktb = {(t, i): kb.sb(f"kt_{t}{i}", [128, MAXTOK], BF16) for t in "fs" for i in range(2)}
    vb = {(t, i): kb.sb(f"v_{t}{i}", [128, MAXTOK // 128, 128], BF16) for t in "fs" for i in range(2)}
    qtb = {(t, i): kb.sb(f"qt_{t}{i}", [128, 512], BF16) for t in "fs" for i in range(2)}
    nbt = [kb.sb(f"nb{i}", [128, MAXTOK // 128], F32) for i in range(2)]
    Pt = [kb.sb(f"Pt{i}", [128, 512], BF16) for i in range(3)]
    Et = [kb.sb(f"Et{i}", [128, 512], F32) for i in range(3)]
    SPt = [kb.sb(f"SPt{i}", [128, 512], BF16) for i in range(3)]
    Gt = [kb.sb(f"Gt{i}", [128, 512], BF16) for i in range(3)]
    At = [kb.sb(f"At{i}", [128, 512], BF16) for i in range(3)]
    u_sb = [kb.sb(f"u_sb{i}", [128, 512], F32) for i in range(2)]
    usq = [kb.sb(f"usq{i}", [128, 512], BF16) for i in range(2)]
    den_sb = kb.sb("den_sb", [128, 512], F32)
    d2 = kb.sb("d2", [128, 512], F32)
    xv = [kb.sb(f"xv{i}", [128, 512], F32) for i in range(2)]
    rs_t = [kb.sb(f"rs_t{i}", [128, 512], F32) for i in range(2)]
    nfst = [kb.sb(f"nfst{i}", [128, 512], BF16) for i in range(4)]
    Zf = [kb.ps(f"Zf{i}", [128, 512], F32) for i in range(2)]
    Zs = [kb.ps(f"Zs{i}", [128, 512], F32) for i in range(2)]
    OTf = kb.ps("OTf", [128, 512], F32)
    OTs = kb.ps("OTs", [128, 512], F32)
    DENf = kb.ps("DENf", [128, 512], F32)
    CUMs = kb.ps("CUMs", [128, 512], F32)
    LN128H = 0.5 * float(np.log(128.0))
    cyc = {"P": 0, "E": 0, "SP": 0, "G": 0, "A": 0, "nf": 0, "zf": 0, "zs": 0}

    def cy(key, n):
        v = cyc[key]
        cyc[key] = (v + 1) % n
        return v

    seqinfo = {"p": dict(ntok=S, nt=NT, a0=0), "s": dict(ntok=SS, nt=NTS, a0=NT)}

    def load_head(seq, hp, slot):
        n = seqinfo[seq]["ntok"]
        for t, h in (("f", hp), ("s", hp + 8)):
            kb.dma("sp", ktb[(t, slot)].t[:, 0:n], KT[seq][h, :, :], reads=[KT_b[seq]], writes=[ktb[(t, slot)]])
            kb.dma("sp", vb[(t, slot)].t[:, 0:n // 128, :],
                   VV[seq].rearrange("(j p) c -> p j c", p=128)[:, :, h * 128:(h + 1) * 128],
                   reads=[VV_b[seq]], writes=[vb[(t, slot)]])

    def normalize(h, ot_ps, den_ps, nq, z_ps, col0, idx):
        ub, uq, xb_, rb = u_sb[idx], usq[idx], xv[idx], rs_t[idx]
        if den_ps is not None:
            kb.op("dve", lambda: dve.tensor_copy(out=den_sb.t[:, 0:nq], in_=den_ps.t[:, 0:nq]), reads=[den_ps], writes=[den_sb])
            kb.op("dve", lambda: dve.reciprocal(out=d2.t[:, 0:nq], in_=den_sb.t[:, 0:nq]), reads=[den_sb], writes=[d2])
            kb.op("dve", lambda: dve.tensor_tensor(out=ub.t[:, 0:nq], in0=ot_ps.t[:, 0:nq], in1=d2.t[:, 0:nq], op=ALU.mult),
                  reads=[ot_ps, d2], writes=[ub])
        else:
            kb.op("dve", lambda: dve.tensor_copy(out=ub.t[:, 0:nq], in_=ot_ps.t[:, 0:nq]), reads=[ot_ps], writes=[ub])
        kb.op("dve", lambda: dve.tensor_tensor(out=uq.t[:, 0:nq], in0=ub.t[:, 0:nq], in1=ub.t[:, 0:nq], op=ALU.mult),
              reads=[ub], writes=[uq])

        def part1b():
            kb.op("pe", lambda: pe.matmul(z_ps.t[:, 0:nq], C["ones_bf"].t[:], uq.t[:, 0:nq], start=True, stop=True),
                  reads=[C["ones_bf"], uq], writes=[z_ps])
            kb.op("dve", lambda: dve.tensor_copy(out=xb_.t[:, 0:nq], in_=z_ps.t[:, 0:nq]), reads=[z_ps], writes=[xb_])

        def part2():
            kb.op("act", lambda: act.activation(out=xb_.t[:, 0:nq], in_=xb_.t[:, 0:nq], func=AF.Ln, bias=128.0 * EPS),
                  reads=[xb_], writes=[xb_])
            kb.op("act", lambda: act.activation(out=rb.t[:, 0:nq], in_=xb_.t[:, 0:nq], func=AF.Exp, scale=-0.5, bias=LN128H),
                  reads=[xb_], writes=[rb])
            nf = nfst[cy("nf", 4)]
            kb.op("dve", lambda: dve.scalar_tensor_tensor(out=nf.t[:, 0:nq], in0=ub.t[:, 0:nq], scalar=gT.t[:, h:h + 1],
                                                          in1=rb.t[:, 0:nq], op0=ALU.mult, op1=ALU.mult),
                  reads=[ub, gT, rb], writes=[nf])
            kb.dma("sp", NFT[h * 128:(h + 1) * 128, col0:col0 + nq], nf.t[:, 0:nq], reads=[nf], writes=[NFT_b])
        return part1b, part2

    pairs = [(seq, hp) for seq in ("p", "s") for hp in range(NFH)]
    steps = []
    glist = []
    gcount = 0
    for pi, (seq, hp) in enumerate(pairs):
        if seq == "p":
            qgroups = [(g * 4, min(4, NT - g * 4)) for g in range((NT + 3) // 4)]
        else:
            qgroups = [(NPT, 1)]
        for gi2, (qt0, nqt) in enumerate(qgroups):
            jmax = qt0 + nqt - 1
            nkb = jmax + 1
            for st in range(nkb):
                steps.append(dict(pi=pi, seq=seq, hp=hp, slot=pi % 2, gs=gcount % 2, qt0=qt0, nqt=nqt, nq=nqt * 128, jmax=jmax,
                                  nkb=nkb, step=st, jf=st, js=jmax - st, first=(st == 0), last=(st == nkb - 1), gidx=gcount,
                                  pair_first=(gi2 == 0 and st == 0)))
            glist.append(dict(seq=seq, hp=hp, qt0=qt0, nq=nqt * 128, gs=gcount % 2, nkb=nkb,
                              jref=qt0 + (2 if nqt == 4 else 0)))
            gcount += 1
    NS = len(steps)

    def load_q(gidx):
        if gidx >= len(glist):
            return
        gd = glist[gidx]
        seq, hp, nq = gd["seq"], gd["hp"], gd["nq"]
        qf_, qs_ = qtb[("f", gd["gs"])], qtb[("s", gd["gs"])]
        qcol = gd["qt0"] * 128 if seq == "p" else 0
        kb.dma("sp", qf_.t[:, 0:nq], QT[seq][hp, :, qcol:qcol + nq], reads=[QT_b[seq]], writes=[qf_])
        kb.dma("sp", qs_.t[:, 0:nq], QT[seq][hp + 8, :, qcol:qcol + nq], reads=[QT_b[seq]], writes=[qs_])
        a0 = seqinfo[seq]["a0"]
        nb = nbt[gd["gs"]]
        nkb, jref = gd["nkb"], gd["jref"]
        kb.op("dve", lambda: dve.tensor_scalar(out=nb.t[:, 0:nkb], in0=CB.t[:, a0:a0 + nkb, hp], scalar1=-1.0,
                                                scalar2=CREF.t[:, a0 + jref, hp:hp + 1], op0=ALU.mult, op1=ALU.add),
              reads=[CB, CREF], writes=[nb])

    bufs_of = {}

    def stage_A(t):
        sd = steps[t]
        seq, hp, nq = sd["seq"], sd["hp"], sd["nq"]
        a0 = seqinfo[seq]["a0"]
        qf_, qs_ = qtb[("f", sd["gs"])], qtb[("s", sd["gs"])]
        nb = nbt[sd["gs"]]
        ktf, kts = ktb[("f", sd["slot"])], ktb[("s", sd["slot"])]
        if sd["first"]:
            load_q(sd["gidx"] + 1)
        jf, js = sd["jf"], sd["js"]
        zf = Zf[cy("zf", 2)]
        zs = Zs[cy("zs", 2)]
        kb.op("pe", lambda: pe.matmul(zs.t[:, 0:nq], kts.t[:, js * 128:(js + 1) * 128], qs_.t[:, 0:nq], start=True, stop=True),
              reads=[kts, qs_], writes=[zs])
        kb.op("pe", lambda: pe.matmul(zf.t[:, 0:nq], ktf.t[:, jf * 128:(jf + 1) * 128], qf_.t[:, 0:nq], start=True, stop=True),
              reads=[ktf, qf_], writes=[zf])
        Eb = Et[cy("E", 3)]
        Pb = Pt[cy("P", 3)]
        SPb = SPt[cy("SP", 3)]
        bufs_of[t] = dict(E=Eb, P=Pb, SP=SPb)
        kb.op("act", lambda: act.activation(out=Eb.t[:, 0:nq], in_=zs.t[:, 0:nq], func=AF.Exp), reads=[zs], writes=[Eb])
        last_act_tok[0] = kb.op("act", lambda: act.activation(out=Pb.t[:, 0:nq], in_=zf.t[:, 0:nq], func=AF.Exp, bias=nb.t[:, jf:jf + 1]),
                                reads=[zf, nb], writes=[Pb])
        rs_ = js - sd["qt0"]
        if rs_ >= 0:
            kb.op("dve", lambda: dve.tensor_tensor(out=Eb.t[:, 0:nq], in0=Eb.t[:, 0:nq], in1=C["mask_sb"].t[:, rs_, 0:nq], op=ALU.mult),
                  reads=[Eb, C["mask_sb"]], writes=[Eb])
        rf_ = jf - sd["qt0"]
        if rf_ >= 0:
            kb.op("dve", lambda: dve.tensor_tensor(out=Pb.t[:, 0:nq], in0=Pb.t[:, 0:nq], in1=C["mask_fox"].t[:, rf_, 0:nq], op=ALU.mult),
                  reads=[Pb, C["mask_fox"]], writes=[Pb])

    def stage_A2(t):
        sd = steps[t]
        nq = sd["nq"]
        Eb, SPb = bufs_of[t]["E"], bufs_of[t]["SP"]
        kb.op("act", lambda: act.activation(out=SPb.t[:, 0:nq], in_=Eb.t[:, 0:nq], func=AF.Ln, bias=1.0), reads=[Eb], writes=[SPb])

    def stage_B_pe(t):
        sd = steps[t]
        nq = sd["nq"]
        SPb = bufs_of[t]["SP"]
        kb.op("pe", lambda: pe.matmul(CUMs.t[:, 0:nq], C["negtri_bf"].t[:], SPb.t[:, 0:nq], start=sd["first"], stop=True,
                                       skip_group_check=True),
              reads=[C["negtri_bf"], SPb], writes=[CUMs])

    def stage_B_act(t):
        sd = steps[t]
        nq = sd["nq"]
        Gb = Gt[cy("G", 3)]
        bufs_of[t]["G"] = Gb
        kb.op("act", lambda: act.activation(out=Gb.t[:, 0:nq], in_=CUMs.t[:, 0:nq], func=AF.Exp), reads=[CUMs], writes=[Gb])

    def stage_C_fix(t):
        sd = steps[t]
        nq = sd["nq"]
        if not sd["last"]:
            SPb = bufs_of[t]["SP"]
            kb.op("pe", lambda: pe.matmul(CUMs.t[:, 0:nq], C["negcompl_bf"].t[:], SPb.t[:, 0:nq], start=False, stop=True,
                                           skip_group_check=True),
                  reads=[C["negcompl_bf"], SPb], writes=[CUMs])

    def stage_C(t, it):
        sd = steps[t]
        nq, jf, js = sd["nq"], sd["jf"], sd["js"]
        vf_, vs_ = vb[("f", sd["slot"])], vb[("s", sd["slot"])]
        bo = bufs_of.pop(t)
        Pb, Eb, Gb = bo["P"], bo["E"], bo["G"]
        if sd["pair_first"] and sd["pi"] + 1 < len(pairs):
            load_head(*pairs[sd["pi"] + 1], (sd["pi"] + 1) % 2)
        while norm_pending_b:
            fa, fb = norm_pending_b.pop(0)
            fa()
            fb()
        kb.op("pe", lambda: pe.matmul(OTf.t[:, 0:nq], vf_.t[:, jf, :], Pb.t[:, 0:nq], start=sd["first"], stop=sd["last"]),
              reads=[vf_, Pb], writes=[OTf], signal=False)
        kb.op("pe", lambda: pe.matmul(DENf.t[:, 0:nq], C["ones_bf"].t[:], Pb.t[:, 0:nq], start=sd["first"], stop=sd["last"]),
              reads=[C["ones_bf"], Pb], writes=[DENf])
        Ab = At[cy("A", 3)]
        kb.op("dve", lambda: dve.tensor_tensor(out=Ab.t[:, 0:nq], in0=Eb.t[:, 0:nq], in1=Gb.t[:, 0:nq], op=ALU.mult),
              reads=[Eb, Gb], writes=[Ab])
        kb.op("pe", lambda: pe.matmul(OTs.t[:, 0:nq], vs_.t[:, js, :], Ab.t[:, 0:nq], start=sd["first"], stop=sd["last"]),
              reads=[vs_, Ab], writes=[OTs])
        if sd["last"]:
            col0 = sd["qt0"] * 128 if sd["seq"] == "p" else NT * 128
            while norm_pending:
                _, q2a, q2b = norm_pending.pop(0)
                q2a()
                q2b()
            p1a, p2a = normalize(sd["hp"], OTf, DENf, nq, OTf, col0, 0)
            p1b, p2b = normalize(sd["hp"] + 8, OTs, None, nq, OTs, col0, 1)
            norm_pending_b.append((p1a, p1b))
            norm_pending.append((it + 6, p2a, p2b))

    load_head(*pairs[0], 0)
    load_q(0)
    norm_pending = []
    norm_pending_b = []
    PACE = max(1, (NS - 8) // max(1, n_cast))
    for it in range(NS + 2):
        if it < NS:
            stage_A(it)
            if it % PACE == PACE - 1:
                issue_cast()
        if 0 <= it - 2 < NS:
            stage_C_fix(it - 2)
        if 0 <= it - 1 < NS:
            stage_B_pe(it - 1)
            stage_B_act(it - 1)
        if it < NS:
            stage_A2(it)
        while norm_pending and norm_pending[0][0] <= it:
            _, p2a, p2b = norm_pending.pop(0)
            p2a()
            p2b()
        if 0 <= it - 2 < NS:
            stage_C(it - 2, it)
        if it >= NS + 1:
            while norm_pending_b:
                fa, fb = norm_pending_b.pop(0)
                fa()
                fb()
        while norm_pending and (norm_pending[0][0] <= it or it >= NS + 1):
            _, p2a, p2b = norm_pending.pop(0)
            p2a()
            p2b()
    while cast_jobs:
        issue_cast()
    kb.end_phase()
    if stop_after <= 2:
        kb.finish()
        return nc, consts


    kb.begin_phase()
    NR = NG + NE
    BIG = 1.0e30
    TRASH = float(NSLOT)
    Hs_b = kb.dram("Hs", Hs)
    XS_b = kb.dram("XS", XS)
    YS_b = kb.dram("YS", YS)
    SLOT = kb.sb("SLOT", [128, NTOK_T, 2], I32, persistent=True)
    GATE = kb.sb("GATE", [128, NTOK_T, 2], F32, persistent=True)
    wo = kb.sb("wo", [128, DC, D], BF16)
    for c4 in range(4):
        kb.dma("pool", wo.t[:, c4 * 4:(c4 + 1) * 4, :], w_out.rearrange("(c p) n -> p c n", p=128)[:, c4 * 4:(c4 + 1) * 4, :], writes=[wo])
    g2_bc = kb.sb("g2_bc", [128, D], F32)
    kb.dma("sp", g2_bc.t[:], bcast_row(g_ffn[0:1, :], D), writes=[g2_bc])
    wr = kb.sb("wr", [128, DC, NR], F32)
    with nc.allow_non_contiguous_dma(reason="small router weight"):
        kb.dma("sp", wr.t[:], w_rt.rearrange("(c p) n -> p c n", p=128), writes=[wr])
    br_bc = kb.sb("br_bc", [128, NR], F32)
    kb.dma("sp", br_bc.t[:], bcast_row(b_rt[0:1, :], NR), writes=[br_bc])
    nfT = [kb.sb(f"nfT{i}", [128, DC, 128], BF16) for i in range(2)]
    x3 = [kb.sb(f"x3_{i}", [128, D], F32) for i in range(2)]
    hres = [kb.sb(f"hres{i}", [128, D], F32) for i in range(2)]
    sq3 = kb.sb("sq3", [128, D], F32)
    ssq3 = kb.sb("ssq3", [128, 1], F32)
    rstd3 = kb.sb("rstd3", [128, 1], F32)
    hn2s = [kb.sb(f"hn2_{i}", [128, D], F32) for i in range(2)]
    hn2b = [kb.sb(f"hn2b{i}", [128, D], BF16) for i in range(3)]
    hn2T = kb.sb("hn2T", [128, DC, 128], F32)
    cntacc = kb.sb("cntacc", [128, NE], BF16)
    L = kb.sb("L", [128, NR], F32)
    sm = {n: kb.sb("r_" + n, [128, w], F32) for n, w in
          (("gmax", 1), ("ngmax", 1), ("gmask", NG), ("eg", NG), ("sg", 1), ("pg", 1), ("pen", NG), ("Lm", NE), ("m1", 1),
           ("mask1", NE), ("Lm2", NE), ("m2", 1), ("mask2", NE), ("dd", 1), ("e2", 1), ("den", 1), ("sp", NE), ("valid", NE),
           ("t1", NE), ("s1", 1), ("s2", 1))}
    msums = [kb.sb(f"msum{i}", [128, NE], BF16) for i in range(2)]
    mk1s = [kb.sb(f"mk1_{i}", [128, NE], F32) for i in range(2)]
    mk2s = [kb.sb(f"mk2_{i}", [128, NE], F32) for i in range(2)]
    ps_o = [kb.ps(f"ps_o{i}", [128, 512], F32) for i in range(4)]
    ps_ht = [kb.ps(f"ps_ht{i}", [128, 4, 128], F32) for i in range(2)]
    ps_lg = kb.ps("ps_lg", [128, NR], F32)
    ps_pos = kb.ps("ps_pos", [128, NE], F32)
    kb.op("dve", lambda: dve.memset(cntacc.t[:], 0.0), writes=[cntacc])
    cy3 = {"o": 0, "ht": 0}

    def cyc3(k, n):
        v = cy3[k]
        cy3[k] = (v + 1) % n
        return v

    def load3(i):
        nb_, xb = nfT[i % 2], x3[i % 2]
        kb.dma("sp", nb_.t[:], NFT.rearrange("(c p) t -> p c t", p=128)[:, :, i * 128:(i + 1) * 128], reads=[NFT_b], writes=[nb_])
        if i < NT:
            kb.dma("sp", xb.t[:], x_p[i * 128:(i + 1) * 128, :], writes=[xb])
        else:
            kb.op("dve", lambda: dve.memset(xb.t[:], 0.0), writes=[xb])
            kb.dma("sp", xb.t[0:T, :], x_s[:, :], writes=[xb])

    def p3_stage1(i):
        if i + 1 < NTOK_T:
            load3(i + 1)
        hn2 = hn2s[i % 2]
        nb_, xb, hb, hb16 = nfT[i % 2], x3[i % 2], hres[i % 2], hn2b[i % 3]
        for n in range(4):
            po = ps_o[cyc3("o", 4)]
            for c in range(DC):
                kb.op("pe", lambda: pe.matmul(po.t[:], nb_.t[:, c, :], wo.t[:, c, n * 512:(n + 1) * 512], start=(c == 0), stop=(c == DC - 1)),
                      reads=[nb_, wo], writes=[po], signal=(c == DC - 1))
            kb.op("dve", lambda: dve.tensor_tensor(out=hb.t[:, n * 512:(n + 1) * 512], in0=po.t[:], in1=xb.t[:, n * 512:(n + 1) * 512], op=ALU.add),
                  reads=[po, xb], writes=[hb])
        kb.dma("sp", Hs[i * 128:(i + 1) * 128, :], hb.t[:], reads=[hb], writes=[Hs_b])

    def p3_stage1b(i):
        hb, hb16 = hres[i % 2], hn2b[i % 3]
        hn2 = hn2s[i % 2]
        kb.op("act", lambda: act.activation(out=sq3.t[:], in_=hb.t[:], func=AF.Square), reads=[hb], writes=[sq3])
        kb.op("dve", lambda: dve.reduce_sum(out=ssq3.t[:], in_=sq3.t[:], axis=AX.X), reads=[sq3], writes=[ssq3])
        kb.op("dve", lambda: dve.tensor_scalar(out=rstd3.t[:], in0=ssq3.t[:], scalar1=1.0 / D, scalar2=EPS, op0=ALU.mult, op1=ALU.add),
              reads=[ssq3], writes=[rstd3])
        kb.op("act", lambda: act.activation(out=rstd3.t[:], in_=rstd3.t[:], func=AF.Ln), reads=[rstd3], writes=[rstd3])
        kb.op("act", lambda: act.activation(out=rstd3.t[:], in_=rstd3.t[:], func=AF.Exp, scale=-0.5), reads=[rstd3], writes=[rstd3])
        kb.op("dve", lambda: dve.scalar_tensor_tensor(out=hn2.t[:], in0=hb.t[:], scalar=rstd3.t[:, 0:1], in1=g2_bc.t[:], op0=ALU.mult, op1=ALU.mult),
              reads=[hb, rstd3, g2_bc], writes=[hn2])
        kb.op("act", lambda: act.activation(out=hb16.t[:], in_=hn2.t[:], func=AF.Copy), reads=[hn2], writes=[hb16])


    def p3_stage2(i):
        hn2 = hn2s[i % 2]
        hb16 = hn2b[i % 3]
        for c4 in range(DC // 4):
            pt = ps_ht[cyc3("ht", 2)]
            for k in range(4):
                c = c4 * 4 + k
                kb.op("pe", lambda: pe.transpose(pt.t[:, k, :], hn2.t[:, c * 128:(c + 1) * 128], C["ident_f"].t[:]),
                      reads=[hn2, C["ident_f"]], writes=[pt], signal=(k == 3))
            kb.op("dve" if c4 % 2 == 0 else "act",
                  (lambda: dve.tensor_copy(out=hn2T.t[:, c4 * 4:(c4 + 1) * 4, :], in_=pt.t[:])) if c4 % 2 == 0 else
                  (lambda: act.activation(out=hn2T.t[:, c4 * 4:(c4 + 1) * 4, :], in_=pt.t[:], func=AF.Copy)),
                  reads=[pt], writes=[hn2T])
        for c in range(DC):
            kb.op("pe", lambda: pe.matmul(ps_lg.t[:], hn2T.t[:, c, :], wr.t[:, c, :], start=(c == 0), stop=(c == DC - 1)),
                  reads=[hn2T, wr], writes=[ps_lg], signal=(c == DC - 1))
        V = lambda n: sm[n]
        dv = lambda fn, r, w: kb.op("dve", fn, reads=r, writes=w)
        dv(lambda: dve.tensor_tensor(out=L.t[:], in0=ps_lg.t[:], in1=br_bc.t[:], op=ALU.add), [ps_lg, br_bc], [L])
        dv(lambda: dve.reduce_max(out=V("gmax").t[:], in_=L.t[:, 0:NG], axis=AX.X), [L], [V("gmax")])
        dv(lambda: dve.tensor_scalar(out=V("gmask").t[:], in0=L.t[:, 0:NG], scalar1=V("gmax").t[:, 0:1], scalar2=None, op0=ALU.is_ge),
           [L, V("gmax")], [V("gmask")])
        dv(lambda: dve.tensor_scalar(out=V("ngmax").t[:], in0=V("gmax").t[:], scalar1=-1.0, scalar2=None, op0=ALU.mult), [V("gmax")], [V("ngmax")])
        kb.op("act", lambda: act.activation(out=V("eg").t[:], in_=L.t[:, 0:NG], func=AF.Exp, bias=V("ngmax").t[:, 0:1]),
              reads=[L, V("ngmax")], writes=[V("eg")])
        dv(lambda: dve.reduce_sum(out=V("sg").t[:], in_=V("eg").t[:], axis=AX.X), [V("eg")], [V("sg")])
        dv(lambda: dve.reciprocal(out=V("pg").t[:], in_=V("sg").t[:]), [V("sg")], [V("pg")])
        dv(lambda: dve.tensor_scalar(out=V("pen").t[:], in0=V("gmask").t[:], scalar1=BIG, scalar2=-BIG, op0=ALU.mult, op1=ALU.add),
           [V("gmask")], [V("pen")])
        for g in range(NG):
            dv(lambda: dve.tensor_scalar(out=V("Lm").t[:, g * EPG:(g + 1) * EPG], in0=L.t[:, NG + g * EPG:NG + (g + 1) * EPG],
                                         scalar1=V("pen").t[:, g:g + 1], scalar2=None, op0=ALU.add), [L, V("pen")], [V("Lm")])
        dv(lambda: dve.reduce_max(out=V("m1").t[:], in_=V("Lm").t[:], axis=AX.X), [V("Lm")], [V("m1")])
        dv(lambda: dve.tensor_scalar(out=V("mask1").t[:], in0=V("Lm").t[:], scalar1=V("m1").t[:, 0:1], scalar2=None, op0=ALU.is_ge),
           [V("Lm"), V("m1")], [V("mask1")])
        dv(lambda: dve.scalar_tensor_tensor(out=V("Lm2").t[:], in0=V("mask1").t[:], scalar=-BIG, in1=V("Lm").t[:], op0=ALU.mult, op1=ALU.add),
           [V("mask1"), V("Lm")], [V("Lm2")])
        dv(lambda: dve.reduce_max(out=V("m2").t[:], in_=V("Lm2").t[:], axis=AX.X), [V("Lm2")], [V("m2")])
        dv(lambda: dve.tensor_scalar(out=V("mask2").t[:], in0=V("Lm2").t[:], scalar1=V("m2").t[:, 0:1], scalar2=None, op0=ALU.is_ge),
           [V("Lm2"), V("m2")], [V("mask2")])
        if i >= NT:
            dv(lambda: dve.tensor_scalar(out=V("mask1").t[:], in0=V("mask1").t[:], scalar1=C["rowvalid"].t[:, 0:1], scalar2=None, op0=ALU.mult),
               [V("mask1"), C["rowvalid"]], [V("mask1")])
            dv(lambda: dve.tensor_scalar(out=V("mask2").t[:], in0=V("mask2").t[:], scalar1=C["rowvalid"].t[:, 0:1], scalar2=None, op0=ALU.mult),
               [V("mask2"), C["rowvalid"]], [V("mask2")])
        dv(lambda: dve.tensor_tensor(out=V("dd").t[:], in0=V("m2").t[:], in1=V("m1").t[:], op=ALU.subtract), [V("m2"), V("m1")], [V("dd")])
        kb.op("act", lambda: act.activation(out=V("e2").t[:], in_=V("dd").t[:], func=AF.Exp), reads=[V("dd")], writes=[V("e2")])
        dv(lambda: dve.tensor_scalar(out=V("den").t[:], in0=V("e2").t[:], scalar1=1.0, scalar2=None, op0=ALU.add), [V("e2")], [V("den")])
        dv(lambda: dve.reciprocal(out=V("den").t[:], in_=V("den").t[:]), [V("den")], [V("den")])
        dv(lambda: dve.tensor_tensor(out=GATE.t[:, i, 0:1], in0=V("pg").t[:], in1=V("den").t[:], op=ALU.mult), [V("pg"), V("den")], [GATE])
        dv(lambda: dve.tensor_tensor(out=GATE.t[:, i, 1:2], in0=V("pg").t[:], in1=GATE.t[:, i, 0:1], op=ALU.subtract), [V("pg"), GATE], [GATE])
        msum = msums[i % 2]
        dv(lambda: dve.tensor_tensor(out=msum.t[:], in0=V("mask1").t[:], in1=V("mask2").t[:], op=ALU.add), [V("mask1"), V("mask2")], [msum])
        dv(lambda: dve.tensor_copy(out=mk1s[i % 2].t[:], in_=V("mask1").t[:]), [V("mask1")], [mk1s[i % 2]])
        dv(lambda: dve.tensor_copy(out=mk2s[i % 2].t[:], in_=V("mask2").t[:]), [V("mask2")], [mk2s[i % 2]])

    def p3_stage2b(i):
        msum = msums[i % 2]
        mkk = {"mask1": mk1s[i % 2], "mask2": mk2s[i % 2]}
        hb16 = hn2b[i % 3]
        V = lambda n: sm[n]
        dv = lambda fn, r, w: kb.op("dve", fn, reads=r, writes=w)
        kb.op("pe", lambda: pe.matmul(ps_pos.t[:], C["tri_strict_bf"].t[:], msum.t[:], start=True, stop=False),
              reads=[C["tri_strict_bf"], msum], writes=[ps_pos], signal=False)
        kb.op("pe", lambda: pe.matmul(ps_pos.t[:], C["ones_bf"].t[:], cntacc.t[:], start=False, stop=True),
              reads=[C["ones_bf"], cntacc], writes=[ps_pos])
        dv(lambda: dve.tensor_scalar(out=V("valid").t[:], in0=ps_pos.t[:], scalar1=float(CAP), scalar2=None, op0=ALU.is_lt), [ps_pos], [V("valid")])
        dv(lambda: dve.tensor_tensor(out=V("sp").t[:], in0=ps_pos.t[:], in1=C["ebase_f"].t[:], op=ALU.add), [ps_pos, C["ebase_f"]], [V("sp")])
        dv(lambda: dve.scalar_tensor_tensor(out=V("sp").t[:], in0=V("sp").t[:], scalar=-TRASH, in1=V("valid").t[:], op0=ALU.add, op1=ALU.mult),
           [V("sp"), V("valid")], [V("sp")])
        dv(lambda: dve.tensor_scalar(out=V("sp").t[:], in0=V("sp").t[:], scalar1=TRASH, scalar2=None, op0=ALU.add), [V("sp")], [V("sp")])
        for k, mk in ((0, "mask1"), (1, "mask2")):
            dv(lambda: dve.tensor_tensor(out=V("t1").t[:], in0=mkk[mk].t[:], in1=V("sp").t[:], op=ALU.mult), [mkk[mk], V("sp")], [V("t1")])
            dv(lambda: dve.reduce_sum(out=V("s1").t[:], in_=V("t1").t[:], axis=AX.X), [V("t1")], [V("s1")])
            if i >= NT:
                dv(lambda: dve.tensor_tensor(out=V("s1").t[:], in0=V("s1").t[:], in1=C["trash_p"].t[:], op=ALU.add), [V("s1"), C["trash_p"]], [V("s1")])
            dv(lambda: dve.tensor_copy(out=SLOT.t[:, i, k:k + 1], in_=V("s1").t[:]), [V("s1")], [SLOT])
        dv(lambda: dve.tensor_tensor(out=cntacc.t[:], in0=cntacc.t[:], in1=msum.t[:], op=ALU.add), [cntacc, msum], [cntacc])
        for k in range(2):
            kb.dma("pool", None, None, reads=[hb16, SLOT], writes=[XS_b],
                   fn=lambda: pool.indirect_dma_start(out=XS[:, :], out_offset=bass.IndirectOffsetOnAxis(ap=SLOT.t[:, i, k:k + 1], axis=0),
                                                      in_=hb16.t[:], in_offset=None))

    load3(0)
    cnt_f = kb.sb("cnt_f", [128, NE], F32)
    for it in range(NTOK_T + 2):
        if it < NTOK_T:
            p3_stage1(it)
        if 0 <= it - 2 < NTOK_T:
            p3_stage2b(it - 2)
        if 0 <= it - 1 < NTOK_T:
            p3_stage2(it - 1)
        if it < NTOK_T:
            p3_stage1b(it)
    kb.op("dve", lambda: dve.tensor_copy(out=cnt_f.t[:], in_=cntacc.t[:]), reads=[cntacc], writes=[cnt_f])
    kb.dma("sp", o_cnt[:, :], cnt_f.t[:], reads=[cnt_f])
    kb.end_phase()
    if stop_after <= 3:
        kb.finish()
        return nc, consts


    kb.begin_phase()
    XS_b = kb.dram("XS4", XS)
    YS_b = kb.dram("YS4", YS)
    nblk = CAP // 128
    FC = DEXP // 128
    xr = [kb.sb(f"xr{i}", [128, nblk, D], BF16) for i in range(2)]
    xsT = kb.sb("xsT", [128, DC, CAP], BF16)
    NU = 5
    wu = [kb.sb(f"wu{i}", [128, 8192], BF16) for i in range(NU)]
    gTt = kb.sb("gTt", [128, FC, CAP], BF16)
    s1t = [kb.sb(f"s1t{i}", [128, CAP], F32) for i in range(2)]
    ysb = [kb.sb(f"ysb{i}", [128, nblk, D], BF16) for i in range(2)]
    ps_a = [kb.ps(f"ps_a{i}", [128, CAP], F32) for i in range(2)]
    ps_b = [kb.ps(f"ps_b{i}", [128, CAP], F32) for i in range(2)]
    ps_y = [kb.ps(f"ps_y{i}", [128, 512], F32) for i in range(2)]
    ps_x = [kb.ps(f"ps_x{i}", [128, 4, 128], BF16) for i in range(2)]
    cy4 = {"a": 0, "y": 0, "x": 0, "s": 0}

    def cyc4(k, n):
        v = cy4[k]
        cy4[k] = (v + 1) % n
        return v

    ujobs = []
    for e in range(NE):
        for hf in range(2):
            ujobs.append(("w1", e, hf))
            ujobs.append(("w3", e, hf))
        for dmh in range(2):
            ujobs.append(("w2", e, dmh))
    ubuf_of = {}

    def issue_u(k):
        if k >= len(ujobs) or k in ubuf_of:
            return
        kind, e, hh = ujobs[k]
        ub = wu[k % NU]
        ubuf_of[k] = ub
        pre = e >= PRE0
        if pre and not cast_waited[0]:
            pool.wait_ge(cast_sem, 16 * n_cast)
            cast_waited[0] = True
        if kind in ("w1", "w3"):
            srcw = ((w1_bf if kind == "w1" else w3_bf) if pre else (w1 if kind == "w1" else w3))
            src = srcw[e].rearrange("(c p) f -> p c f", p=128)[:, :, hh * 512:(hh + 1) * 512]
            dst = ub.t[:, :].rearrange("p (c f) -> p c f", c=DC)
        else:
            src = (w2_bf if pre else w2)[e].rearrange("(c p) n -> p c n", p=128)[:, :, hh * 1024:(hh + 1) * 1024]
            dst = ub.t[:, :].rearrange("p (c n) -> p c n", c=FC)
        kb.dma("pool", dst, src, writes=[ub])

    def load_xr(e):
        if e < NE:
            kb.dma("sp", xr[e % 2].t[:], XS[e * CAP:(e + 1) * CAP, :].rearrange("(b p) d -> p b d", p=128), reads=[XS_b], writes=[xr[e % 2]])

    cast_waited = [False]
    uk = 0
    for k in range(4):
        issue_u(k)
    load_xr(0)
    for e in range(NE):
        load_xr(e + 1)
        xrb = xr[e % 2]
        for b in range(nblk):
            for c4 in range(DC // 4):
                pt = ps_x[cyc4("x", 2)]
                for k in range(4):
                    c = c4 * 4 + k
                    kb.op("pe", lambda: pe.transpose(pt.t[:, k, :], xrb.t[:, b, c * 128:(c + 1) * 128], C["ident_bf"].t[:]),
                          reads=[xrb, C["ident_bf"]], writes=[pt], signal=(k == 3))
                if (b * 4 + c4) % 2 == 0:
                    kb.op("dve", lambda: dve.tensor_copy(out=xsT.t[:, c4 * 4:(c4 + 1) * 4, b * 128:(b + 1) * 128], in_=pt.t[:]), reads=[pt], writes=[xsT])
                else:
                    kb.op("act", lambda: act.activation(out=xsT.t[:, c4 * 4:(c4 + 1) * 4, b * 128:(b + 1) * 128], in_=pt.t[:], func=AF.Copy),
                          reads=[pt], writes=[xsT])
        for hf in range(2):
            issue_u(uk + 2); issue_u(uk + 3); issue_u(uk + 4)
            u1, u3 = ubuf_of[uk], ubuf_of[uk + 1]
            uk += 2
            u1v = u1.t[:, :].rearrange("p (c f) -> p c f", c=DC)
            u3v = u3.t[:, :].rearrange("p (c f) -> p c f", c=DC)
            for fl_ in range(4):
                fc = hf * 4 + fl_
                i_ = cyc4("a", 2)
                pa, pb = ps_a[i_], ps_b[i_]
                for c in range(DC):
                    kb.op("pe", lambda: pe.matmul(pa.t[:], u1v[:, c, fl_ * 128:(fl_ + 1) * 128], xsT.t[:, c, :], start=(c == 0), stop=(c == DC - 1)),
                          reads=[u1, xsT], writes=[pa], signal=(c == DC - 1))
                for c in range(DC):
                    kb.op("pe", lambda: pe.matmul(pb.t[:], u3v[:, c, fl_ * 128:(fl_ + 1) * 128], xsT.t[:, c, :], start=(c == 0), stop=(c == DC - 1)),
                          reads=[u3, xsT], writes=[pb], signal=(c == DC - 1))
                st_ = s1t[cyc4("s", 2)]
                kb.op("act", lambda: act.activation(out=st_.t[:], in_=pa.t[:], func=AF.Silu), reads=[pa], writes=[st_])
                kb.op("dve", lambda: dve.tensor_tensor(out=gTt.t[:, fc, :], in0=pb.t[:], in1=st_.t[:], op=ALU.mult), reads=[pb, st_], writes=[gTt])
        yb = ysb[e % 2]
        for dmh in range(2):
            issue_u(uk + 1); issue_u(uk + 2); issue_u(uk + 3); issue_u(uk + 4)
            u2 = ubuf_of[uk]
            uk += 1
            u2v = u2.t[:, :].rearrange("p (c n) -> p c n", c=FC)
            for b in range(nblk):
                for dn in range(2):
                    py = ps_y[cyc4("y", 2)]
                    for fc in range(FC):
                        kb.op("pe", lambda: pe.matmul(py.t[:], gTt.t[:, fc, b * 128:(b + 1) * 128], u2v[:, fc, dn * 512:(dn + 1) * 512],
                                                       start=(fc == 0), stop=(fc == FC - 1)),
                              reads=[gTt, u2], writes=[py], signal=(fc == FC - 1))
                    col = dmh * 1024 + dn * 512
                    if (b + dn) % 2 == 0:
                        kb.op("act", lambda: act.activation(out=yb.t[:, b, col:col + 512], in_=py.t[:], func=AF.Copy), reads=[py], writes=[yb])
                    else:
                        kb.op("dve", lambda: dve.tensor_copy(out=yb.t[:, b, col:col + 512], in_=py.t[:]), reads=[py], writes=[yb])
        kb.dma("sp", YS[e * CAP:(e + 1) * CAP, :].rearrange("(b p) d -> p b d", p=128), yb.t[:], reads=[yb], writes=[YS_b])
    kb.end_phase()
    if stop_after <= 4:
        kb.finish()
        return nc, consts

    kb.begin_phase()
    Hs_b = kb.dram("Hs5", Hs)
    YS_b = kb.dram("YS5", YS)
    gf_bc = kb.sb("gf_bc", [128, D], F32)
    kb.dma("sp", gf_bc.t[:], bcast_row(g_fin[0:1, :], D), writes=[gf_bc])
    h5 = [kb.sb(f"h5_{i}", [128, D], F32) for i in range(2)]
    yg = [[kb.sb(f"yg{i}_{k}", [128, D], BF16) for k in range(2)] for i in range(2)]
    t5 = kb.sb("t5", [128, D], F32)
    sq5 = kb.sb("sq5", [128, D], F32)
    ssq5 = kb.sb("ssq5", [128, 1], F32)
    rstd5 = kb.sb("rstd5", [128, 1], F32)
    yo = [kb.sb(f"yo{i}", [128, D], F32) for i in range(2)]

    def load5(i):
        kb.dma("sp", h5[i % 2].t[:], Hs[i * 128:(i + 1) * 128, :], reads=[Hs_b], writes=[h5[i % 2]])
        for k in range(2):
            yb_ = yg[i % 2][k]
            kb.dma("pool", None, None, reads=[YS_b, SLOT], writes=[yb_],
                   fn=lambda: pool.indirect_dma_start(out=yb_.t[:], out_offset=None, in_=YS[:, :],
                                                      in_offset=bass.IndirectOffsetOnAxis(ap=SLOT.t[:, i, k:k + 1], axis=0)))

    load5(0)
    for i in range(NTOK_T):
        if i + 1 < NTOK_T:
            load5(i + 1)
        hb, y1, y2, ob = h5[i % 2], yg[i % 2][0], yg[i % 2][1], yo[i % 2]
        kb.op("dve", lambda: dve.scalar_tensor_tensor(out=t5.t[:], in0=y1.t[:], scalar=GATE.t[:, i, 0:1], in1=hb.t[:], op0=ALU.mult, op1=ALU.add),
              reads=[y1, GATE, hb], writes=[t5])
        kb.op("dve", lambda: dve.scalar_tensor_tensor(out=t5.t[:], in0=y2.t[:], scalar=GATE.t[:, i, 1:2], in1=t5.t[:], op0=ALU.mult, op1=ALU.add),
              reads=[y2, GATE, t5], writes=[t5])
        kb.op("act", lambda: act.activation(out=sq5.t[:], in_=t5.t[:], func=AF.Square), reads=[t5], writes=[sq5])
        kb.op("dve", lambda: dve.reduce_sum(out=ssq5.t[:], in_=sq5.t[:], axis=AX.X), reads=[sq5], writes=[ssq5])
        kb.op("dve", lambda: dve.tensor_scalar(out=rstd5.t[:], in0=ssq5.t[:], scalar1=1.0 / D, scalar2=EPS, op0=ALU.mult, op1=ALU.add),
              reads=[ssq5], writes=[rstd5])
        kb.op("act", lambda: act.activation(out=rstd5.t[:], in_=rstd5.t[:], func=AF.Sqrt), reads=[rstd5], writes=[rstd5])
        kb.op("dve", lambda: dve.reciprocal(out=rstd5.t[:], in_=rstd5.t[:]), reads=[rstd5], writes=[rstd5])
        kb.op("dve", lambda: dve.scalar_tensor_tensor(out=ob.t[:], in0=t5.t[:], scalar=rstd5.t[:, 0:1], in1=gf_bc.t[:], op0=ALU.mult, op1=ALU.mult),
              reads=[t5, rstd5, gf_bc], writes=[ob])
        if i < NT:
            kb.dma("sp", y_p[i * 128:(i + 1) * 128, :], ob.t[:], reads=[ob])
        else:
            kb.dma("sp", y_s[:, :], ob.t[0:T, :], reads=[ob])
    kb.end_phase()
    kb.finish()
    return nc, consts


def make_in_maps(inp, cfg, n_cores):
    consts = make_consts(cfg)
    f = lambda a: np.ascontiguousarray(np.asarray(a, dtype=np.float32))
    w_rt = np.concatenate([np.asarray(inp["w_group"][0]), np.asarray(inp["w_router"][0])], axis=1)
    b_rt = np.concatenate([np.asarray(inp["b_group"][0]), np.asarray(inp["b_router"][0])], axis=0)[None, :]
    shared = dict(
        w_in=f(inp["w_in"][0]), b_f=f(inp["b_f"]), g_attn=f(inp["g_attn"]), g_of=f(inp["g_out_fox"]),
        g_os=f(inp["g_out_sb"]), w_out=f(inp["w_out"][0]), g_ffn=f(inp["g_ffn"]), w_rt=f(w_rt), b_rt=f(b_rt),
        w1=f(inp["w1"][0]), w3=f(inp["w3"][0]), w2=f(inp["w2"][0]), g_fin=f(np.asarray(inp["g_final"])[None, :]),
    )
    for k, v in consts.items():
        shared["k_" + k] = v
    maps = []
    for b in range(n_cores):
        m = dict(shared)
        m["x_p"] = f(inp["x_prompt"][b])
        m["x_s"] = f(inp["x_sample"][b])
        m["c_fk"] = f(np.asarray(inp["cache_fox_k"][0, b]).reshape(cfg["PAST"], 1024))
        m["c_fv"] = f(np.asarray(inp["cache_fox_v"][0, b]).reshape(cfg["PAST"], 1024))
        m["c_fl"] = f(inp["cache_fox_logf"][0, b])
        m["c_sk"] = f(np.asarray(inp["cache_sb_k"][0, b]).reshape(cfg["PAST"], 1024))
        m["c_sv"] = f(np.asarray(inp["cache_sb_v"][0, b]).reshape(cfg["PAST"], 1024))
        maps.append(m)
    return maps


def assemble(results, cfg):
    S, T = cfg["S"], cfg["T"]
    B = len(results)
    st = lambda k, shp: np.stack([np.asarray(r[k]).reshape(shp) for r in results], axis=0)
    y_p = st("y_p", (S, D)); y_s = st("y_s", (T, D))
    outs = [y_p, y_s]
    for pre, n in (("o_p", S), ("o_s", T)):
        outs.append(st(pre + "fk", (n, 8, 128))[None])
        outs.append(st(pre + "fv", (n, 8, 128))[None])
        outs.append(st(pre + "fl", (n, 8))[None])
        outs.append(st(pre + "sk", (n, 8, 128))[None])
        outs.append(st(pre + "sv", (n, 8, 128))[None])
    return tuple(np.ascontiguousarray(o.astype(np.float32)) for o in outs)


def kernel(**inputs):
    cfg = default_cfg()
    nc, _ = build(cfg)
    maps = make_in_maps(inputs, cfg, 8)
    res = run_bass_kernel_spmd(nc, maps, core_ids=list(range(8)))
    try:
        import os
        if os.environ.get("MK_DEBUG_CNT"):
            np.save(os.environ["MK_DEBUG_CNT"], np.stack([np.asarray(r["o_cnt"]).sum(0) for r in res.results]))
    except Exception:
        pass
    return assemble(res.results, cfg)
```

```python
import contextlib
import numpy as np
import ml_dtypes
import concourse.bass as bass
import concourse.mybir as mybir
from concourse.bass_utils import run_bass_kernel_spmd

F32 = mybir.dt.float32
BF16 = mybir.dt.bfloat16
I32 = mybir.dt.int32
AF = mybir.ActivationFunctionType
ALU = mybir.AluOpType
AX = mybir.AxisListType

D = 2048
DC = D // 128
HD = 128
NH = 16
NFH = 8
IN_COLS = 6152
EPS = 1e-6
DEXP = 1024


def default_cfg():
    return dict(S=4096, T=64, PAST=1024, NG=4, EPG=8, CAP=384, debug=False, stop_after=99)


class Buf:
    __slots__ = ("name", "t", "w", "rs", "sem", "cnt", "is_dram", "is_psum")

    def __init__(self, name, t, is_dram=False, is_psum=False):
        self.is_psum = is_psum
        self.name = name
        self.t = t
        self.w = None
        self.rs = {}
        self.sem = None
        self.cnt = 0
        self.is_dram = is_dram


EPOCH = 30000


class KB:
    COMPUTE = ("pe", "act", "dve", "pool")

    def __init__(self, nc):
        self.nc = nc
        self.eng = {"pe": nc.tensor, "act": nc.scalar, "dve": nc.vector, "pool": nc.gpsimd, "sp": nc.sync}
        self.cnt = {e: 0 for e in self.COMPUTE}
        self.seen = {}
        self.outer = contextlib.ExitStack()
        self.phase_stack = None
        self.sems = {}
        self.dma_toks = []
        self.nbuf = 0
        self.pbufs = []
        self.limit = 10 ** 9
        self.nops = 0
        self.trace = None
        self.free_sems = []
        self.phase_sem_bufs = []
        for e in self.COMPUTE:
            for ep in range(4):
                self._csem(e, ep)

    def _ctx(self, persistent):
        return self.outer if persistent else self.phase_stack

    def sb(self, name, shape, dt, persistent=False):
        t = self._ctx(persistent).enter_context(self.nc.sbuf_tensor(name, list(shape), dt))
        b = Buf(name, t)
        if persistent:
            self.pbufs.append(b)
        return b

    def ps(self, name, shape, dt, persistent=False):
        t = self._ctx(persistent).enter_context(self.nc.psum_tensor(name, list(shape), dt))
        return Buf(name, t, is_psum=True)

    def dram(self, name, t):
        b = Buf(name, t, is_dram=True)
        self.pbufs.append(b)
        return b

    def _sem(self, name, persistent=False):
        return self.outer.enter_context(self.nc.semaphore(name))

    def _dma_sem(self, b):
        if self.free_sems:
            sem, cnt = self.free_sems.pop(0)
        else:
            sem, cnt = self._sem(f"d_{self.nbuf}"), 0
            self.nbuf += 1
        b.sem = sem
        b.cnt = cnt
        self.phase_sem_bufs.append(b)

    def _csem(self, eng, epoch):
        k = (eng, epoch)
        if k not in self.sems:
            self.sems[k] = self._sem(f"c_{eng}_{epoch}", persistent=True)
        return self.sems[k]

    def _wait(self, eng, toks):
        for tok in toks:
            if tok is None:
                continue
            kind = tok[0]
            if kind == "c":
                _, src, epoch, val = tok
                if src == eng and eng == "pe":
                    continue
                key = ("c", src, epoch)
                sem = self._csem(src, epoch)
            else:
                _, sem, val, semid = tok
                key = ("d", semid)
            if self.seen.get((eng, key), 0) >= val:
                continue
            self.seen[(eng, key)] = val
            self.eng[eng].wait_ge(sem, val)

    def _deps(self, reads, writes):
        deps = []
        for b in reads:
            if b.w is not None:
                deps.append(b.w)
            if b.is_psum:
                deps.extend(b.rs.values())
        for b in writes:
            if b.w is not None:
                deps.append(b.w)
            deps.extend(b.rs.values())
        return deps

    @staticmethod
    def _tkey(tok):
        return tok[:3] if tok[0] == "c" else ("d", tok[3])

    def _commit(self, tok, reads, writes):
        k = self._tkey(tok)
        for b in reads:
            old = b.rs.get(k)
            if old is None or old[-1 if tok[0] == "c" else 2] <= tok[-1 if tok[0] == "c" else 2]:
                b.rs[k] = tok
        for b in writes:
            b.w = tok
            b.rs = {}

    def op(self, eng, fn, reads=(), writes=(), signal=True, extra=()):
        self.nops += 1
        if self.nops > self.limit:
            return None
        if self.trace is not None:
            import sys as _s
            f = _s._getframe(1)
            self.trace.append((self.nops, eng, f.f_lineno, f.f_code.co_name))
        self._wait(eng, self._deps(reads, writes) + list(extra))
        inst = fn()
        n = self.cnt[eng]
        if signal:
            n += 1
            self.cnt[eng] = n
            epoch, val = divmod(n - 1, EPOCH)
            inst.then_inc(self._csem(eng, epoch), 1)
            tok = ("c", eng, epoch, val + 1)
        else:
            epoch, val = divmod(n, EPOCH)
            tok = ("c", eng, epoch, val + 1)
        self._commit(tok, reads, writes)
        return tok

    def dma(self, q, out_ap, in_ap, reads=(), writes=(), fn=None, extra=()):
        self.nops += 1
        if self.nops > self.limit:
            return None
        if self.trace is not None:
            import sys as _s
            f = _s._getframe(1)
            self.trace.append((self.nops, "dma-" + q, f.f_lineno, f.f_code.co_name))
        self._wait(q, self._deps(reads, writes) + list(extra))
        cands = [b for b in list(writes) + list(reads) if not b.is_dram]
        b = cands[0] if cands else (list(writes) + list(reads))[0]
        if b.sem is None:
            self._dma_sem(b)
        sem = b.sem
        b.cnt += 16
        val = b.cnt
        if fn is None:
            inst = self.eng[q].dma_start(out=out_ap, in_=in_ap)
        else:
            inst = fn()
        inst.then_inc(sem, 16)
        tok = ("d", sem, val, id(sem))
        self.dma_toks.append(tok)
        self._commit(tok, reads, writes)
        return tok

    def begin_phase(self):
        self.phase_stack = contextlib.ExitStack()
        self.dma_toks = []

    def end_phase(self):
        toks = list(self.dma_toks)
        for e in self.COMPUTE:
            n = self.cnt[e]
            if n > 0:
                epoch, val = divmod(n - 1, EPOCH)
                toks.append(("c", e, epoch, val + 1))
        for e in ("pe", "act", "dve", "pool", "sp"):
            if self.nops <= self.limit:
                self._wait(e, toks)
        self.phase_stack.close()
        self.phase_stack = None
        self.dma_toks = []
        for b in self.phase_sem_bufs:
            self.free_sems.append((b.sem, b.cnt))
            b.sem = None
            b.cnt = 0
        self.phase_sem_bufs = []
        for b in self.pbufs:
            b.sem = None
            b.cnt = 0
            b.w = None
            b.rs = {}

    def finish(self):
        self.outer.close()


def make_consts(cfg):
    bf = ml_dtypes.bfloat16
    p = np.arange(128)
    c = {}
    c["ident_bf"] = np.eye(128, dtype=np.float32).astype(bf)
    c["ident_f"] = np.eye(128, dtype=np.float32)
    c["ones_f"] = np.ones((128, 128), np.float32)
    c["ones_bf"] = np.ones((128, 128), np.float32).astype(bf)
    c["tri_incl_f"] = (p[:, None] <= p[None, :]).astype(np.float32)
    c["tri_strict_f"] = (p[:, None] < p[None, :]).astype(np.float32)
    c["sel0_f"] = np.zeros((128, 128), np.float32)
    c["sel0_f"][0, :] = 1.0
    q = np.arange(512)
    mf = np.zeros((128, 4, 512), np.float32)
    ms = np.zeros((128, 4, 512), np.float32)
    for r in range(4):
        mf[:, r, :] = (q[None, :] >= 128 * r + p[:, None])
        ms[:, r, :] = (q[None, :] > 128 * r + p[:, None])
    c["mask_fox"] = mf.astype(bf)
    c["mask_sb"] = ms.astype(bf)
    c["negtri_bf"] = (-(p[:, None] >= p[None, :]).astype(np.float32)).astype(bf)
    c["negcompl_bf"] = (-(p[:, None] < p[None, :]).astype(np.float32)).astype(bf)
    NE = cfg["NG"] * cfg["EPG"]
    c["ebase_f"] = np.tile((np.arange(NE, dtype=np.float32) * cfg["CAP"])[None, :], (128, 1))
    c["eidx_f"] = np.tile(np.arange(NE, dtype=np.float32)[None, :], (128, 1))
    c["tri_strict_bf"] = c["tri_strict_f"].astype(bf)
    rvv = (p < cfg["T"]).astype(np.float32)
    c["rowvalid"] = rvv[:, None].copy()
    c["trash_p"] = ((1.0 - rvv) * (NE * cfg["CAP"] + p))[:, None].astype(np.float32)
    return c


CONST_DT = {"tri_strict_bf": BF16, "ident_bf": BF16, "ones_bf": BF16, "mask_fox": BF16, "mask_sb": BF16, "negtri_bf": BF16,
            "negcompl_bf": BF16}


def build(cfg):
    S, T, PAST = cfg["S"], cfg["T"], cfg["PAST"]
    NG, EPG, CAP = cfg["NG"], cfg["EPG"], cfg["CAP"]
    NE = NG * EPG
    NT = S // 128
    NPT = PAST // 128
    NTS = NPT + 1
    SS = NTS * 128
    NTOK_T = NT + 1
    debug = cfg["debug"]
    stop_after = cfg["stop_after"]

    nc = bass.Bass("TRN2", target_bir_lowering=False)
    kb = KB(nc)
    kb.limit = cfg.get("limit", 10 ** 9)
    global LAST_KB
    LAST_KB = kb
    if cfg.get("trace"):
        kb.trace = []

    def din(name, shape, dt=F32):
        return nc.dram_tensor(name, list(shape), dt, kind="ExternalInput").ap()

    def dout(name, shape, dt=F32):
        return nc.dram_tensor(name, list(shape), dt, kind="ExternalOutput").ap()

    def dscr(name, shape, dt):
        kind = "ExternalOutput" if debug else "Internal"
        return nc.dram_tensor(name, list(shape), dt, kind=kind).ap()

    x_p = din("x_p", [S, D])
    x_s = din("x_s", [T, D])
    c_fk = din("c_fk", [PAST, 1024])
    c_fv = din("c_fv", [PAST, 1024])
    c_fl = din("c_fl", [PAST, 8])
    c_sk = din("c_sk", [PAST, 1024])
    c_sv = din("c_sv", [PAST, 1024])
    w_in = din("w_in", [D, IN_COLS])
    b_f = din("b_f", [1, 8])
    g_attn = din("g_attn", [1, D])
    g_of = din("g_of", [1, 1024])
    g_os = din("g_os", [1, 1024])
    w_out = din("w_out", [D, D])
    g_ffn = din("g_ffn", [1, D])
    w_rt = din("w_rt", [D, NG + NE])
    b_rt = din("b_rt", [1, NG + NE])
    w1 = din("w1", [NE, D, DEXP])
    w3 = din("w3", [NE, D, DEXP])
    w2 = din("w2", [NE, DEXP, D])
    g_fin = din("g_fin", [1, D])
    consts = make_consts(cfg)
    cin = {k: din("k_" + k, v.shape, CONST_DT.get(k, F32)) for k, v in consts.items()}

    y_p = dout("y_p", [S, D])
    y_s = dout("y_s", [T, D])
    o_pfk = dout("o_pfk", [S, 1024]); o_pfv = dout("o_pfv", [S, 1024]); o_pfl = dout("o_pfl", [S, 8])
    o_psk = dout("o_psk", [S, 1024]); o_psv = dout("o_psv", [S, 1024])
    o_sfk = dout("o_sfk", [T, 1024]); o_sfv = dout("o_sfv", [T, 1024]); o_sfl = dout("o_sfl", [T, 8])
    o_ssk = dout("o_ssk", [T, 1024]); o_ssv = dout("o_ssv", [T, 1024])

    o_cnt = dout("o_cnt", [128, NE])
    w_in_bf = dscr("w_in_bf", [D, IN_COLS], BF16) if False else nc.dram_tensor("w_in_bf", [D, IN_COLS], BF16, kind="Internal").ap()
    QT = {"p": dscr("QT_p", [NH, 128, S], BF16), "s": dscr("QT_s", [NH, 128, 128], BF16)}
    KT = {"p": dscr("KT_p", [NH, 128, S], BF16), "s": dscr("KT_s", [NH, 128, SS], BF16)}
    VV = {"p": dscr("VV_p", [S, D], BF16), "s": dscr("VV_s", [SS, D], BF16)}
    NFT = dscr("NFT", [D, NTOK_T * 128], BF16)
    Hs = dscr("Hs", [NTOK_T * 128, D], F32)
    NSLOT = NE * CAP
    XS = dscr("XS", [NSLOT + 128, D], BF16)
    YS = dscr("YS", [NSLOT + 128, D], BF16)

    E = kb.eng
    pe, act, dve, pool, sp = E["pe"], E["act"], E["dve"], E["pool"], E["sp"]

    kb.begin_phase()
    C = {}
    LATE = ("mask_fox", "mask_sb", "negtri_bf", "negcompl_bf", "ebase_f", "eidx_f", "tri_strict_bf", "tri_strict_f",
            "rowvalid", "trash_p", "sel0_f", "ones_f", "tri_incl_f", "ident_f")
    for k, v in consts.items():
        C[k] = kb.sb("c_" + k, v.shape, CONST_DT.get(k, F32), persistent=True)
        if k not in LATE:
            kb.dma("sp", C[k].t[:], cin[k][:], writes=[C[k]])
    NTA = NT + NTS
    LF = kb.sb("LF", [128, NTA, 8], F32, persistent=True)
    CB = kb.sb("CB", [128, NTA, 8], F32, persistent=True)
    CREF = kb.sb("CREF", [128, NTA, 8], F32, persistent=True)
    bf_bc = kb.sb("bf_bc", [128, 8], F32, persistent=True)

    def bcast_row(ap_row, n):
        return ap_row.broadcast_to([128, n])

    kb.dma("sp", bf_bc.t[:], bcast_row(b_f[0:1, :], 8), writes=[bf_bc])


    g_bc = kb.sb("g_bc", [128, D], F32)
    kb.dma("sp", g_bc.t[:], bcast_row(g_attn[0:1, :], D), writes=[g_bc])
    xt = [kb.sb(f"xt{i}", [128, D], F32) for i in range(2)]
    sq_junk = kb.sb("sq_junk", [128, D], BF16)
    ssq = kb.sb("ssq", [128, 1], F32)
    rstd = kb.sb("rstd", [128, 1], F32)
    hn = kb.sb("hn", [128, D], BF16)
    hnTs = [[kb.sb(f"hnT{a}_{i}", [128, DC, 128], BF16) for i in range(4)] for a in range(2)]
    wch = [kb.sb(f"wch{i}", [128, DC, 512], BF16) for i in range(3)]
    wfl = kb.sb("wfl", [128, DC, 8], BF16)
    ps_tr = [kb.ps(f"ps_tr{i}", [128, 4, 128], BF16) for i in range(2)]
    ps_mm = [kb.ps(f"ps_mm{i}", [128, 512], F32) for i in range(3)]
    ps_fl_t = kb.ps("ps_fl", [128, 4, 8], F32)
    ps_fl = ps_fl_t
    ps_flb = [Buf(f"ps_flb{i}", ps_fl_t.t, is_psum=True) for i in range(4)]
    ps_t2 = [kb.ps(f"ps_t2{i}", [128, 4, 128], BF16) for i in range(2)]
    st_f = [kb.sb(f"st_f{i}", [128, 512], F32) for i in range(3)]
    st_b = [kb.sb(f"st_b{i}", [128, 512], BF16) for i in range(3)]
    QTst = kb.sb("QTst", [128, NH, 512], BF16)
    KTst = kb.sb("KTst", [128, NH, 512], BF16)
    fl_t = kb.sb("fl_t", [128, 8], F32)
    lfacc = kb.sb("lfacc", [128, 8], F32)
    cst_f = [kb.sb(f"cst_f{i}", [128, 1024], F32) for i in range(2)]
    cst_b = [kb.sb(f"cst_b{i}", [128, 1024], BF16) for i in range(2)]
    KTc = kb.sb("KTc", [128, NH, 128], BF16)

    SCALE = float(HD) ** -0.5
    chunks = []
    for kind, c0, h0 in (("q", 0, 0), ("k", 1024, 0), ("v", 2048, 0)):
        for i in range(2):
            chunks.append((c0 + 512 * i, 512, kind, h0 + 4 * i))
    chunks.append((3072, 8, "fl", 0))
    for kind, c0, h0 in (("q", 3080, 8), ("k", 4104, 8), ("v", 5128, 8)):
        for i in range(2):
            chunks.append((c0 + 512 * i, 512, kind, h0 + 4 * i))
    w_in_v = w_in_bf.rearrange("(c p) n -> p c n", p=128)
    w_in_bf_bufs = []
    for ci_, (c0_, cw_, kind_, h0_) in enumerate(chunks):
        bb = kb.dram(f"w_in_bf{ci_}", w_in_bf)
        w_in_bf_bufs.append(bb)
        if cw_ >= 512:
            for hh in range(2):
                r0, r1 = hh * (D // 2), (hh + 1) * (D // 2)
                kb.dma("pool", w_in_bf[r0:r1, c0_:c0_ + cw_], w_in[r0:r1, c0_:c0_ + cw_], writes=[bb])
        else:
            with nc.allow_non_contiguous_dma(reason="8-column forget-gate slice"):
                kb.dma("pool", w_in_bf[:, c0_:c0_ + cw_], w_in[:, c0_:c0_ + cw_], writes=[bb])

    rr = {"x": 0, "tr": 0, "mm": 0, "t2": 0, "sf": 0, "sb": 0, "w": 0, "c": 0}

    def nxt(key, n):
        v = rr[key]
        rr[key] = (v + 1) % n
        return v

    def rmsnorm_tile(xb, g_b, out_b, rows=128):
        kb.op("act", lambda: act.activation(out=sq_junk.t[:], in_=xb.t[:], func=AF.Square), reads=[xb], writes=[sq_junk])
        kb.op("dve", lambda: dve.reduce_sum(out=ssq.t[:], in_=sq_junk.t[:], axis=AX.X), reads=[sq_junk], writes=[ssq])
        kb.op("dve", lambda: dve.tensor_scalar(out=rstd.t[:], in0=ssq.t[:], scalar1=1.0 / D, scalar2=EPS,
                                                op0=ALU.mult, op1=ALU.add), reads=[ssq], writes=[rstd])
        kb.op("act", lambda: act.activation(out=rstd.t[:], in_=rstd.t[:], func=AF.Sqrt), reads=[rstd], writes=[rstd])
        kb.op("dve", lambda: dve.reciprocal(out=rstd.t[:], in_=rstd.t[:]), reads=[rstd], writes=[rstd])
        kb.op("dve", lambda: dve.scalar_tensor_tensor(out=out_b.t[:], in0=xb.t[:], scalar=rstd.t[:, 0:1], in1=g_b.t[:],
                                                      op0=ALU.mult, op1=ALU.mult), reads=[xb, rstd, g_b], writes=[out_b])

    def transpose_to(dst_b, dst_ap_fn, src_b, src_ap_fn, n, ident, evac="dve", psl=None):
        pt = ps_tr[nxt("tr", 2)] if psl is None else psl[nxt("t2", 2)]
        for i in range(n):
            kb.op("pe", lambda i=i: pe.transpose(pt.t[:, i, :], src_ap_fn(i), ident.t[:]),
                  reads=[src_b, ident], writes=[pt], signal=(i == n - 1))
        if evac == "dve":
            kb.op("dve", lambda: dve.tensor_copy(out=dst_ap_fn(), in_=pt.t[:, 0:n, :]), reads=[pt], writes=[dst_b])
        else:
            kb.op("act", lambda: act.activation(out=dst_ap_fn(), in_=pt.t[:, 0:n, :], func=AF.Copy), reads=[pt], writes=[dst_b])

    def logf_from_psum(ti_all, rows_valid, out_ap, pbuf, li):
        kb.op("dve", lambda: dve.tensor_tensor(out=fl_t.t[:], in0=pbuf.t[:, li, :], in1=bf_bc.t[:], op=ALU.add),
              reads=[pbuf, bf_bc], writes=[fl_t])
        kb.op("act", lambda: act.activation(out=fl_t.t[:], in_=fl_t.t[:], func=AF.Exp, scale=-1.0), reads=[fl_t], writes=[fl_t])
        kb.op("act", lambda: act.activation(out=fl_t.t[:], in_=fl_t.t[:], func=AF.Ln, bias=1.0), reads=[fl_t], writes=[fl_t])
        kb.op("dve", lambda: dve.tensor_scalar(out=LF.t[:, ti_all, :], in0=fl_t.t[:], scalar1=-1.0, scalar2=None, op0=ALU.mult),
              reads=[fl_t], writes=[LF])
        kb.dma("sp", out_ap, LF.t[0:rows_valid, ti_all, :], reads=[LF])

    groups = [("p", list(range(g * 4, min(NT, g * 4 + 4)))) for g in range((NT + 3) // 4)] + [("s", [NPT])]
    outs = {"p": dict(fk=o_pfk, fv=o_pfv, fl=o_pfl, sk=o_psk, sv=o_psv),
            "s": dict(fk=o_sfk, fv=o_sfv, fl=o_sfl, sk=o_ssk, sv=o_ssv)}
    QT_b = {s: kb.dram("QT" + s, QT[s]) for s in "ps"}
    KT_b = {s: kb.dram("KT" + s, KT[s]) for s in "ps"}
    VV_b = {s: kb.dram("VV" + s, VV[s]) for s in "ps"}

    xjobs = [(seq, ti) for (seq, tiles) in groups for ti in tiles]
    xbuf_of = {}

    def issue_x(m):
        if m >= len(xjobs) or m in xbuf_of:
            return
        seq, ti = xjobs[m]
        xb = xt[m % 2]
        xbuf_of[m] = xb
        if seq == "p":
            kb.dma("sp", xb.t[:], x_p[ti * 128:(ti + 1) * 128, :], writes=[xb])
        else:
            kb.op("dve", lambda: dve.memset(xb.t[:], 0.0), writes=[xb])
            kb.dma("sp", xb.t[0:T, :], x_s[:, :], writes=[xb])

    wjobs = [(gi, ci) for gi in range(len(groups)) for ci in range(len(chunks))]
    wbuf_of = {}
    wslot = [0]

    def issue_w(k):
        if k >= len(wjobs) or k in wbuf_of:
            return
        gi, ci = wjobs[k]
        c0, cw, kind, h0 = chunks[ci]
        if kind == "fl":
            wb = wfl
        else:
            wb = wch[wslot[0] % 3]
            wslot[0] += 1
        wbuf_of[k] = wb
        kb.dma("sp", wb.t[:, :, 0:cw], w_in_v[:, :, c0:c0 + cw], reads=[w_in_bf_bufs[ci]], writes=[wb])

    pending = []

    def flush_pending():
        while pending:
            pending.pop(0)()

    wk = 0
    xidx = {}
    m_ = 0
    for gi_, (seq_, tiles_) in enumerate(groups):
        for li_, ti_ in enumerate(tiles_):
            xidx[(gi_, li_)] = m_
            m_ += 1

    def prep_norm(gi, li):
        m = xidx[(gi, li)]
        issue_x(m)
        issue_x(m + 1)
        rmsnorm_tile(xbuf_of[m], g_bc, hn)

    def prep_tr(gi, li):
        dst = hnTs[gi % 2][li]
        for c4 in range(DC // 4):
            transpose_to(dst, lambda c4=c4: dst.t[:, c4 * 4:(c4 + 1) * 4, :], hn,
                         lambda i, c4=c4: hn.t[:, (c4 * 4 + i) * 128:(c4 * 4 + i + 1) * 128], 4, C["ident_bf"],
                         evac=("dve" if c4 % 2 == 0 else "act"))

    cjobs = [(j, src, isk, h0) for j in range(NPT)
             for (src, isk, h0) in ((c_fk, True, 0), (c_sk, True, 8), (c_fv, False, 0), (c_sv, False, 8))]

    def cache_part_a(k):
        j, src, isk, h0 = cjobs[k]
        r0 = j * 128
        if h0 == 0 and isk:
            kb.dma("sp", LF.t[:, NT + j, :], c_fl[r0:r0 + 128, :], writes=[LF])
        cf, cb_ = cst_f[k % 2], cst_b[k % 2]
        kb.dma("sp", cf.t[:], src[r0:r0 + 128, :], writes=[cf])
        kb.op("dve", lambda: dve.tensor_copy(out=cb_.t[:], in_=cf.t[:]), reads=[cf], writes=[cb_])

    def cache_part_b(k):
        j, src, isk, h0 = cjobs[k]
        r0 = j * 128
        cb_ = cst_b[k % 2]
        if isk:
            for q4 in range(2):
                transpose_to(KTc, lambda q4=q4: KTc.t[:, h0 + q4 * 4:h0 + q4 * 4 + 4, :], cb_,
                             lambda i, q4=q4: cb_.t[:, (q4 * 4 + i) * 128:(q4 * 4 + i + 1) * 128], 4, C["ident_bf"],
                             evac="act", psl=ps_t2)
            if h0 == 8:
                kb.dma("sp", KT["s"][:, :, r0:r0 + 128].rearrange("h d t -> d h t"), KTc.t[:], reads=[KTc], writes=[KT_b["s"]])
        else:
            kb.dma("sp", VV["s"][r0:r0 + 128, h0 * 128:h0 * 128 + 1024], cb_.t[:], reads=[cb_], writes=[VV_b["s"]])

    def cache_step():
        k = cache_done[0]
        if k > len(cjobs):
            return
        if k >= 1:
            cache_part_b(k - 1)
        if k < len(cjobs):
            cache_part_a(k)
        cache_done[0] += 1

    cache_done = [0]

    issue_x(0)
    for li0 in range(len(groups[0][1])):
        prep_norm(0, li0)
        prep_tr(0, li0)
    for k in LATE:
        kb.dma("sp", C[k].t[:], cin[k][:], writes=[C[k]])
    for gi, (seq, tiles) in enumerate(groups):
        ntl = len(tiles)
        hnT = hnTs[gi % 2]
        nxt_tiles = len(groups[gi + 1][1]) if gi + 1 < len(groups) else 0
        for ci, (c0, cw, kind, h0) in enumerate(chunks):
            issue_w(wk); issue_w(wk + 1); issue_w(wk + 2)
            wb = wbuf_of[wk]
            wk += 1
            for li, ti in enumerate(tiles):
                rows = 128 if seq == "p" else T
                r0 = ti * 128 if seq == "p" else 0
                ti_all = ti if seq == "p" else NT + ti
                pm = ps_flb[li] if kind == "fl" else ps_mm[nxt("mm", 3)]
                pm_ap = pm.t[:, li, :] if kind == "fl" else pm.t[:, 0:cw]
                for c in range(DC):
                    kb.op("pe", lambda c=c, pm_ap=pm_ap, wb=wb, li=li: pe.matmul(pm_ap, hnT[li].t[:, c, :], wb.t[:, c, 0:cw],
                                                                               start=(c == 0), stop=(c == DC - 1)),
                          reads=[hnT[li], wb], writes=[pm], signal=(c == DC - 1))
                flush_pending()
                fox = h0 < NFH
                if kind == "fl":
                    logf_from_psum(ti_all, rows, outs[seq]["fl"][r0:r0 + rows, :], pm, li)
                elif kind == "q":
                    sb_ = st_b[nxt("sb", 3)]
                    kb.op("act", lambda: act.activation(out=sb_.t[:], in_=pm.t[:], func=AF.Copy, scale=SCALE),
                          reads=[pm], writes=[sb_])
                    pending.append(lambda sb_=sb_, h0=h0, li=li: transpose_to(
                        QTst, lambda: QTst.t[:, h0:h0 + 4, li * 128:(li + 1) * 128], sb_,
                        lambda i: sb_.t[:, i * 128:(i + 1) * 128], 4, C["ident_bf"], evac="dve", psl=ps_t2))
                elif kind == "k":
                    sf_ = st_f[nxt("sf", 3)]
                    sb_ = st_b[nxt("sb", 3)]
                    kb.op("act", lambda: act.activation(out=sf_.t[:], in_=pm.t[:], func=AF.Copy), reads=[pm], writes=[sf_])
                    kb.op("dve", lambda: dve.tensor_copy(out=sb_.t[:], in_=sf_.t[:]), reads=[sf_], writes=[sb_])
                    oc = (h0 % NFH) * 128
                    kb.dma("sp", outs[seq]["fk" if fox else "sk"][r0:r0 + rows, oc:oc + 512], sf_.t[0:rows, :], reads=[sf_])
                    pending.append(lambda sb_=sb_, h0=h0, li=li: transpose_to(
                        KTst, lambda: KTst.t[:, h0:h0 + 4, li * 128:(li + 1) * 128], sb_,
                        lambda i: sb_.t[:, i * 128:(i + 1) * 128], 4, C["ident_bf"], evac="act", psl=ps_t2))
                else:
                    sf_ = st_f[nxt("sf", 3)]
                    sb_ = st_b[nxt("sb", 3)]
                    kb.op("act", lambda: act.activation(out=sf_.t[:], in_=pm.t[:], func=AF.Copy), reads=[pm], writes=[sf_])
                    kb.op("dve", lambda: dve.tensor_copy(out=sb_.t[:], in_=sf_.t[:]), reads=[sf_], writes=[sb_])
                    oc = (h0 % NFH) * 128
                    kb.dma("sp", outs[seq]["fv" if fox else "sv"][r0:r0 + rows, oc:oc + 512], sf_.t[0:rows, :], reads=[sf_])
                    kb.dma("sp", VV[seq][ti * 128:(ti + 1) * 128, h0 * 128:h0 * 128 + 512], sb_.t[:], reads=[sb_], writes=[VV_b[seq]])
            if ci >= 5:
                cache_step()
            if ci >= 1 and ci - 1 < nxt_tiles:
                prep_tr(gi + 1, ci - 1)
            if ci < nxt_tiles:
                prep_norm(gi + 1, ci)
        flush_pending()
        t0 = tiles[0] * 128
        if seq == "p":
            kb.dma("sp", QT["p"][:, :, t0:t0 + ntl * 128].rearrange("h d t -> d h t"), QTst.t[:, :, 0:ntl * 128],
                   reads=[QTst], writes=[QT_b["p"]])
            kb.dma("sp", KT["p"][:, :, t0:t0 + ntl * 128].rearrange("h d t -> d h t"), KTst.t[:, :, 0:ntl * 128],
                   reads=[KTst], writes=[KT_b["p"]])
        else:
            kb.dma("sp", QT["s"][:, :, :].rearrange("h d t -> d h t"), QTst.t[:, :, 0:128], reads=[QTst], writes=[QT_b["s"]])
            kb.dma("sp", KT["s"][:, :, t0:t0 + 128].rearrange("h d t -> d h t"), KTst.t[:, :, 0:128],
                   reads=[KTst], writes=[KT_b["s"]])

    while cache_done[0] <= len(cjobs):
        cache_step()

    ps_c = ps_flb[0]
    for (a0, n) in ((0, NT), (NT, NTS)):
        for j in range(n):
            ja = a0 + j
            if j == 0:
                kb.op("dve", lambda: dve.memset(lfacc.t[:], 0.0), writes=[lfacc])
            kb.op("pe", lambda: pe.matmul(ps_c.t[:, 0, :], C["tri_incl_f"].t[:], LF.t[:, ja, :], start=True, stop=False),
                  reads=[C["tri_incl_f"], LF], writes=[ps_c], signal=False)
            kb.op("pe", lambda: pe.matmul(ps_c.t[:, 0, :], C["ones_f"].t[:], lfacc.t[:], start=False, stop=True),
                  reads=[C["ones_f"], lfacc], writes=[ps_c])
            kb.op("dve", lambda: dve.tensor_copy(out=CB.t[:, ja, :], in_=ps_c.t[:, 0, :]), reads=[ps_c], writes=[CB])
            kb.op("dve", lambda: dve.tensor_tensor(out=lfacc.t[:], in0=lfacc.t[:], in1=LF.t[:, ja, :], op=ALU.add),
                  reads=[lfacc, LF], writes=[lfacc])
    ps_cr = ps_mm[0]
    kb.op("pe", lambda: pe.matmul(ps_cr.t[:, 0:NTA * 8], C["sel0_f"].t[:], CB.t[:].rearrange("p a h -> p (a h)"), start=True, stop=True),
          reads=[C["sel0_f"], CB], writes=[ps_cr])
    kb.op("dve", lambda: dve.tensor_copy(out=CREF.t[:].rearrange("p a h -> p (a h)"), in_=ps_cr.t[:, 0:NTA * 8]), reads=[ps_cr], writes=[CREF])
    kb.end_phase()

    if stop_after <= 1:
        kb.finish()
        return nc, consts


    kb.begin_phase()
    NFT_b = kb.dram("NFT", NFT)
    gT = kb.sb("gT", [128, NH], F32)
    with nc.allow_non_contiguous_dma(reason="tiny per-head gain transpose"):
        kb.dma("sp", gT.t[:, 0:8], g_of[0:1, :].rearrange("o (h d) -> d (o h)", d=128), writes=[gT])
        kb.dma("sp", gT.t[:, 8:16], g_os[0:1, :].rearrange("o (h d) -> d (o h)", d=128), writes=[gT])
    MAXTOK = max(S, SS)
    ktb = {(t, i): kb.sb(f"kt_{t}{i}", [128, MAXTOK], BF16) for t in "fs" for i in range(2)}
    vb = {(t, i): kb.sb(f"v_{t}{i}", [128, MAXTOK // 128, 128], BF16) for t in "fs" for i in range(2)}
    qtb = {(t, i): kb.sb(f"qt_{t}{i}", [128, 512], BF16) for t in "fs" for i in range(2)}
    nbt = [kb.sb(f"nb{i}", [128, MAXTOK // 128], F32) for i in range(2)]
    Pt = [kb.sb(f"Pt{i}", [128, 512], BF16) for i in range(3)]
    Et = [kb.sb(f"Et{i}", [128, 512], F32) for i in range(3)]
    SPt = [kb.sb(f"SPt{i}", [128, 512], BF16) for i in range(3)]
    Gt = [kb.sb(f"Gt{i}", [128, 512], BF16) for i in range(3)]
    At = [kb.sb(f"At{i}", [128, 512], BF16) for i in range(3)]
    u_sb = [kb.sb(f"u_sb{i}", [128, 512], F32) for i in range(2)]
    usq = [kb.sb(f"usq{i}", [128, 512], BF16) for i in range(2)]
    den_sb = kb.sb("den_sb", [128, 512], F32)
    d2 = kb.sb("d2", [128, 512], F32)
    xv = [kb.sb(f"xv{i}", [128, 512], F32) for i in range(2)]
    rs_t = [kb.sb(f"rs_t{i}", [128, 512], F32) for i in range(2)]
    nfst = [kb.sb(f"nfst{i}", [128, 512], BF16) for i in range(4)]
    Zf = [kb.ps(f"Zf{i}", [128, 512], F32) for i in range(2)]
    Zs = [kb.ps(f"Zs{i}", [128, 512], F32) for i in range(2)]
    OTf = kb.ps("OTf", [128, 512], F32)
    OTs = kb.ps("OTs", [128, 512], F32)
    DENf = kb.ps("DENf", [128, 512], F32)
    CUMs = kb.ps("CUMs", [128, 512], F32)
    LN128H = 0.5 * float(np.log(128.0))
    cyc = {"P": 0, "E": 0, "SP": 0, "G": 0, "A": 0, "nf": 0, "zf": 0, "zs": 0}

    def cy(key, n):
        v = cyc[key]
        cyc[key] = (v + 1) % n
        return v

    seqinfo = {"p": dict(ntok=S, nt=NT, a0=0), "s": dict(ntok=SS, nt=NTS, a0=NT)}

    def load_head(seq, hp, slot):
        n = seqinfo[seq]["ntok"]
        for t, h in (("f", hp), ("s", hp + 8)):
            kb.dma("sp", ktb[(t, slot)].t[:, 0:n], KT[seq][h, :, :], reads=[KT_b[seq]], writes=[ktb[(t, slot)]])
            kb.dma("sp", vb[(t, slot)].t[:, 0:n // 128, :],
                   VV[seq].rearrange("(j p) c -> p j c", p=128)[:, :, h * 128:(h + 1) * 128],
                   reads=[VV_b[seq]], writes=[vb[(t, slot)]])

    def normalize(h, ot_ps, den_ps, nq, z_ps, col0, idx):
        ub, uq, xb_, rb = u_sb[idx], usq[idx], xv[idx], rs_t[idx]
        if den_ps is not None:
            kb.op("dve", lambda: dve.tensor_copy(out=den_sb.t[:, 0:nq], in_=den_ps.t[:, 0:nq]), reads=[den_ps], writes=[den_sb])
            kb.op("dve", lambda: dve.reciprocal(out=d2.t[:, 0:nq], in_=den_sb.t[:, 0:nq]), reads=[den_sb], writes=[d2])
            kb.op("dve", lambda: dve.tensor_tensor(out=ub.t[:, 0:nq], in0=ot_ps.t[:, 0:nq], in1=d2.t[:, 0:nq], op=ALU.mult),
                  reads=[ot_ps, d2], writes=[ub])
        else:
            kb.op("dve", lambda: dve.tensor_copy(out=ub.t[:, 0:nq], in_=ot_ps.t[:, 0:nq]), reads=[ot_ps], writes=[ub])
        kb.op("dve", lambda: dve.tensor_tensor(out=uq.t[:, 0:nq], in0=ub.t[:, 0:nq], in1=ub.t[:, 0:nq], op=ALU.mult),
              reads=[ub], writes=[uq])
        kb.op("pe", lambda: pe.matmul(z_ps.t[:, 0:nq], C["ones_bf"].t[:], uq.t[:, 0:nq], start=True, stop=True),
              reads=[C["ones_bf"], uq], writes=[z_ps])
        kb.op("dve", lambda: dve.tensor_copy(out=xb_.t[:, 0:nq], in_=z_ps.t[:, 0:nq]), reads=[z_ps], writes=[xb_])

        def part2():
            kb.op("act", lambda: act.activation(out=xb_.t[:, 0:nq], in_=xb_.t[:, 0:nq], func=AF.Ln, bias=128.0 * EPS),
                  reads=[xb_], writes=[xb_])
            kb.op("act", lambda: act.activation(out=rb.t[:, 0:nq], in_=xb_.t[:, 0:nq], func=AF.Exp, scale=-0.5, bias=LN128H),
                  reads=[xb_], writes=[rb])
            nf = nfst[cy("nf", 4)]
            kb.op("dve", lambda: dve.scalar_tensor_tensor(out=nf.t[:, 0:nq], in0=ub.t[:, 0:nq], scalar=gT.t[:, h:h + 1],
                                                          in1=rb.t[:, 0:nq], op0=ALU.mult, op1=ALU.mult),
                  reads=[ub, gT, rb], writes=[nf])
            kb.dma("sp", NFT[h * 128:(h + 1) * 128, col0:col0 + nq], nf.t[:, 0:nq], reads=[nf], writes=[NFT_b])
        return part2

    pairs = [(seq, hp) for seq in ("p", "s") for hp in range(NFH)]
    steps = []
    glist = []
    gcount = 0
    for pi, (seq, hp) in enumerate(pairs):
        if seq == "p":
            qgroups = [(g * 4, min(4, NT - g * 4)) for g in range((NT + 3) // 4)]
        else:
            qgroups = [(NPT, 1)]
        for gi2, (qt0, nqt) in enumerate(qgroups):
            jmax = qt0 + nqt - 1
            nkb = jmax + 1
            for st in range(nkb):
                steps.append(dict(pi=pi, seq=seq, hp=hp, slot=pi % 2, gs=gcount % 2, qt0=qt0, nqt=nqt, nq=nqt * 128, jmax=jmax,
                                  nkb=nkb, step=st, jf=st, js=jmax - st, first=(st == 0), last=(st == nkb - 1), gidx=gcount,
                                  pair_first=(gi2 == 0 and st == 0)))
            glist.append(dict(seq=seq, hp=hp, qt0=qt0, nq=nqt * 128, gs=gcount % 2, nkb=nkb,
                              jref=qt0 + (2 if nqt == 4 else 0)))
            gcount += 1
    NS = len(steps)

    def load_q(gidx):
        if gidx >= len(glist):
            return
        gd = glist[gidx]
        seq, hp, nq = gd["seq"], gd["hp"], gd["nq"]
        qf_, qs_ = qtb[("f", gd["gs"])], qtb[("s", gd["gs"])]
        qcol = gd["qt0"] * 128 if seq == "p" else 0
        kb.dma("sp", qf_.t[:, 0:nq], QT[seq][hp, :, qcol:qcol + nq], reads=[QT_b[seq]], writes=[qf_])
        kb.dma("sp", qs_.t[:, 0:nq], QT[seq][hp + 8, :, qcol:qcol + nq], reads=[QT_b[seq]], writes=[qs_])
        a0 = seqinfo[seq]["a0"]
        nb = nbt[gd["gs"]]
        nkb, jref = gd["nkb"], gd["jref"]
        kb.op("dve", lambda: dve.tensor_scalar(out=nb.t[:, 0:nkb], in0=CB.t[:, a0:a0 + nkb, hp], scalar1=-1.0,
                                                scalar2=CREF.t[:, a0 + jref, hp:hp + 1], op0=ALU.mult, op1=ALU.add),
              reads=[CB, CREF], writes=[nb])

    bufs_of = {}

    def stage_A(t):
        sd = steps[t]
        seq, hp, nq = sd["seq"], sd["hp"], sd["nq"]
        a0 = seqinfo[seq]["a0"]
        qf_, qs_ = qtb[("f", sd["gs"])], qtb[("s", sd["gs"])]
        nb = nbt[sd["gs"]]
        ktf, kts = ktb[("f", sd["slot"])], ktb[("s", sd["slot"])]
        if sd["first"]:
            load_q(sd["gidx"] + 1)
        jf, js = sd["jf"], sd["js"]
        zf = Zf[cy("zf", 2)]
        zs = Zs[cy("zs", 2)]
        kb.op("pe", lambda: pe.matmul(zs.t[:, 0:nq], kts.t[:, js * 128:(js + 1) * 128], qs_.t[:, 0:nq], start=True, stop=True),
              reads=[kts, qs_], writes=[zs])
        kb.op("pe", lambda: pe.matmul(zf.t[:, 0:nq], ktf.t[:, jf * 128:(jf + 1) * 128], qf_.t[:, 0:nq], start=True, stop=True),
              reads=[ktf, qf_], writes=[zf])
        Eb = Et[cy("E", 3)]
        Pb = Pt[cy("P", 3)]
        SPb = SPt[cy("SP", 3)]
        bufs_of[t] = dict(E=Eb, P=Pb, SP=SPb)
        kb.op("act", lambda: act.activation(out=Eb.t[:, 0:nq], in_=zs.t[:, 0:nq], func=AF.Exp), reads=[zs], writes=[Eb])
        kb.op("act", lambda: act.activation(out=Pb.t[:, 0:nq], in_=zf.t[:, 0:nq], func=AF.Exp, bias=nb.t[:, jf:jf + 1]),
              reads=[zf, nb], writes=[Pb])
        rs_ = js - sd["qt0"]
        if rs_ >= 0:
            kb.op("dve", lambda: dve.tensor_tensor(out=Eb.t[:, 0:nq], in0=Eb.t[:, 0:nq], in1=C["mask_sb"].t[:, rs_, 0:nq], op=ALU.mult),
                  reads=[Eb, C["mask_sb"]], writes=[Eb])
        rf_ = jf - sd["qt0"]
        if rf_ >= 0:
            kb.op("pool", lambda: pool.tensor_tensor(out=Pb.t[:, 0:nq], in0=Pb.t[:, 0:nq], in1=C["mask_fox"].t[:, rf_, 0:nq], op=ALU.mult),
                  reads=[Pb, C["mask_fox"]], writes=[Pb])

    def stage_A2(t):
        sd = steps[t]
        nq = sd["nq"]
        Eb, SPb = bufs_of[t]["E"], bufs_of[t]["SP"]
        kb.op("act", lambda: act.activation(out=SPb.t[:, 0:nq], in_=Eb.t[:, 0:nq], func=AF.Ln, bias=1.0), reads=[Eb], writes=[SPb])

    def stage_B_pe(t):
        sd = steps[t]
        nq = sd["nq"]
        SPb = bufs_of[t]["SP"]
        kb.op("pe", lambda: pe.matmul(CUMs.t[:, 0:nq], C["negtri_bf"].t[:], SPb.t[:, 0:nq], start=sd["first"], stop=True,
                                       skip_group_check=True),
              reads=[C["negtri_bf"], SPb], writes=[CUMs])

    def stage_B_act(t):
        sd = steps[t]
        nq = sd["nq"]
        Gb = Gt[cy("G", 3)]
        bufs_of[t]["G"] = Gb
        kb.op("act", lambda: act.activation(out=Gb.t[:, 0:nq], in_=CUMs.t[:, 0:nq], func=AF.Exp), reads=[CUMs], writes=[Gb])

    def stage_C_fix(t):
        sd = steps[t]
        nq = sd["nq"]
        if not sd["last"]:
            SPb = bufs_of[t]["SP"]
            kb.op("pe", lambda: pe.matmul(CUMs.t[:, 0:nq], C["negcompl_bf"].t[:], SPb.t[:, 0:nq], start=False, stop=True,
                                           skip_group_check=True),
                  reads=[C["negcompl_bf"], SPb], writes=[CUMs])

    def stage_C(t, it):
        sd = steps[t]
        nq, jf, js = sd["nq"], sd["jf"], sd["js"]
        vf_, vs_ = vb[("f", sd["slot"])], vb[("s", sd["slot"])]
        bo = bufs_of.pop(t)
        Pb, Eb, Gb = bo["P"], bo["E"], bo["G"]
        if sd["pair_first"] and sd["pi"] + 1 < len(pairs):
            load_head(*pairs[sd["pi"] + 1], (sd["pi"] + 1) % 2)
        kb.op("pe", lambda: pe.matmul(OTf.t[:, 0:nq], vf_.t[:, jf, :], Pb.t[:, 0:nq], start=sd["first"], stop=sd["last"]),
              reads=[vf_, Pb], writes=[OTf], signal=False)
        kb.op("pe", lambda: pe.matmul(DENf.t[:, 0:nq], C["ones_bf"].t[:], Pb.t[:, 0:nq], start=sd["first"], stop=sd["last"]),
              reads=[C["ones_bf"], Pb], writes=[DENf])
        Ab = At[cy("A", 3)]
        kb.op("dve", lambda: dve.tensor_tensor(out=Ab.t[:, 0:nq], in0=Eb.t[:, 0:nq], in1=Gb.t[:, 0:nq], op=ALU.mult),
              reads=[Eb, Gb], writes=[Ab])
        kb.op("pe", lambda: pe.matmul(OTs.t[:, 0:nq], vs_.t[:, js, :], Ab.t[:, 0:nq], start=sd["first"], stop=sd["last"]),
              reads=[vs_, Ab], writes=[OTs])
        if sd["last"]:
            col0 = sd["qt0"] * 128 if sd["seq"] == "p" else NT * 128
            p2a = normalize(sd["hp"], OTf, DENf, nq, OTf, col0, 0)
            p2b = normalize(sd["hp"] + 8, OTs, None, nq, OTs, col0, 1)
            norm_pending.append((it + 2, p2a, p2b))

    load_head(*pairs[0], 0)
    load_q(0)
    norm_pending = []
    for it in range(NS + 2):
        if it < NS:
            stage_A(it)
        if 0 <= it - 2 < NS:
            stage_C_fix(it - 2)
        if 0 <= it - 1 < NS:
            stage_B_pe(it - 1)
            stage_B_act(it - 1)
        if it < NS:
            stage_A2(it)
        if 0 <= it - 2 < NS:
            stage_C(it - 2, it)
        while norm_pending and (norm_pending[0][0] <= it or it >= NS + 1):
            _, p2a, p2b = norm_pending.pop(0)
            p2a()
            p2b()
    kb.end_phase()
    if stop_after <= 2:
        kb.finish()
        return nc, consts


    kb.begin_phase()
    NR = NG + NE
    BIG = 1.0e30
    TRASH = float(NSLOT)
    Hs_b = kb.dram("Hs", Hs)
    XS_b = kb.dram("XS", XS)
    YS_b = kb.dram("YS", YS)
    SLOT = kb.sb("SLOT", [128, NTOK_T, 2], I32, persistent=True)
    GATE = kb.sb("GATE", [128, NTOK_T, 2], F32, persistent=True)
    wo = kb.sb("wo", [128, DC, D], BF16)
    for c4 in range(4):
        kb.dma("pool", wo.t[:, c4 * 4:(c4 + 1) * 4, :], w_out.rearrange("(c p) n -> p c n", p=128)[:, c4 * 4:(c4 + 1) * 4, :], writes=[wo])
    g2_bc = kb.sb("g2_bc", [128, D], F32)
    kb.dma("sp", g2_bc.t[:], bcast_row(g_ffn[0:1, :], D), writes=[g2_bc])
    wr = kb.sb("wr", [128, DC, NR], F32)
    with nc.allow_non_contiguous_dma(reason="small router weight"):
        kb.dma("sp", wr.t[:], w_rt.rearrange("(c p) n -> p c n", p=128), writes=[wr])
    br_bc = kb.sb("br_bc", [128, NR], F32)
    kb.dma("sp", br_bc.t[:], bcast_row(b_rt[0:1, :], NR), writes=[br_bc])
    nfT = [kb.sb(f"nfT{i}", [128, DC, 128], BF16) for i in range(2)]
    x3 = [kb.sb(f"x3_{i}", [128, D], F32) for i in range(2)]
    hres = [kb.sb(f"hres{i}", [128, D], F32) for i in range(2)]
    sq3 = kb.sb("sq3", [128, D], F32)
    ssq3 = kb.sb("ssq3", [128, 1], F32)
    rstd3 = kb.sb("rstd3", [128, 1], F32)
    hn2s = [kb.sb(f"hn2_{i}", [128, D], F32) for i in range(2)]
    hn2b = [kb.sb(f"hn2b{i}", [128, D], BF16) for i in range(3)]
    hn2T = kb.sb("hn2T", [128, DC, 128], F32)
    cntacc = kb.sb("cntacc", [128, NE], BF16)
    L = kb.sb("L", [128, NR], F32)
    sm = {n: kb.sb("r_" + n, [128, w], F32) for n, w in
          (("gmax", 1), ("ngmax", 1), ("gmask", NG), ("eg", NG), ("sg", 1), ("pg", 1), ("pen", NG), ("Lm", NE), ("m1", 1),
           ("mask1", NE), ("Lm2", NE), ("m2", 1), ("mask2", NE), ("dd", 1), ("e2", 1), ("den", 1), ("sp", NE), ("valid", NE),
           ("t1", NE), ("s1", 1), ("s2", 1))}
    msums = [kb.sb(f"msum{i}", [128, NE], BF16) for i in range(2)]
    mk1s = [kb.sb(f"mk1_{i}", [128, NE], F32) for i in range(2)]
    mk2s = [kb.sb(f"mk2_{i}", [128, NE], F32) for i in range(2)]
    ps_o = [kb.ps(f"ps_o{i}", [128, 512], F32) for i in range(4)]
    ps_ht = [kb.ps(f"ps_ht{i}", [128, 4, 128], F32) for i in range(2)]
    ps_lg = kb.ps("ps_lg", [128, NR], F32)
    ps_pos = kb.ps("ps_pos", [128, NE], F32)
    kb.op("dve", lambda: dve.memset(cntacc.t[:], 0.0), writes=[cntacc])
    cy3 = {"o": 0, "ht": 0}

    def cyc3(k, n):
        v = cy3[k]
        cy3[k] = (v + 1) % n
        return v

    def load3(i):
        nb_, xb = nfT[i % 2], x3[i % 2]
        kb.dma("sp", nb_.t[:], NFT.rearrange("(c p) t -> p c t", p=128)[:, :, i * 128:(i + 1) * 128], reads=[NFT_b], writes=[nb_])
        if i < NT:
            kb.dma("sp", xb.t[:], x_p[i * 128:(i + 1) * 128, :], writes=[xb])
        else:
            kb.op("dve", lambda: dve.memset(xb.t[:], 0.0), writes=[xb])
            kb.dma("sp", xb.t[0:T, :], x_s[:, :], writes=[xb])

    def p3_stage1(i):
        if i + 1 < NTOK_T:
            load3(i + 1)
        hn2 = hn2s[i % 2]
        nb_, xb, hb, hb16 = nfT[i % 2], x3[i % 2], hres[i % 2], hn2b[i % 3]
        for n in range(4):
            po = ps_o[cyc3("o", 4)]
            for c in range(DC):
                kb.op("pe", lambda: pe.matmul(po.t[:], nb_.t[:, c, :], wo.t[:, c, n * 512:(n + 1) * 512], start=(c == 0), stop=(c == DC - 1)),
                      reads=[nb_, wo], writes=[po], signal=(c == DC - 1))
            kb.op("dve", lambda: dve.tensor_tensor(out=hb.t[:, n * 512:(n + 1) * 512], in0=po.t[:], in1=xb.t[:, n * 512:(n + 1) * 512], op=ALU.add),
                  reads=[po, xb], writes=[hb])
        kb.dma("sp", Hs[i * 128:(i + 1) * 128, :], hb.t[:], reads=[hb], writes=[Hs_b])

    def p3_stage1b(i):
        hb, hb16 = hres[i % 2], hn2b[i % 3]
        hn2 = hn2s[i % 2]
        kb.op("pool", lambda: pool.tensor_tensor(out=sq3.t[:], in0=hb.t[:], in1=hb.t[:], op=ALU.mult), reads=[hb], writes=[sq3])
        kb.op("dve", lambda: dve.reduce_sum(out=ssq3.t[:], in_=sq3.t[:], axis=AX.X), reads=[sq3], writes=[ssq3])
        kb.op("dve", lambda: dve.tensor_scalar(out=rstd3.t[:], in0=ssq3.t[:], scalar1=1.0 / D, scalar2=EPS, op0=ALU.mult, op1=ALU.add),
              reads=[ssq3], writes=[rstd3])
        kb.op("act", lambda: act.activation(out=rstd3.t[:], in_=rstd3.t[:], func=AF.Ln), reads=[rstd3], writes=[rstd3])
        kb.op("act", lambda: act.activation(out=rstd3.t[:], in_=rstd3.t[:], func=AF.Exp, scale=-0.5), reads=[rstd3], writes=[rstd3])
        kb.op("dve", lambda: dve.scalar_tensor_tensor(out=hn2.t[:], in0=hb.t[:], scalar=rstd3.t[:, 0:1], in1=g2_bc.t[:], op0=ALU.mult, op1=ALU.mult),
              reads=[hb, rstd3, g2_bc], writes=[hn2])
        kb.op("act", lambda: act.activation(out=hb16.t[:], in_=hn2.t[:], func=AF.Copy), reads=[hn2], writes=[hb16])


    def p3_stage2(i):
        hn2 = hn2s[i % 2]
        hb16 = hn2b[i % 3]
        for c4 in range(DC // 4):
            pt = ps_ht[cyc3("ht", 2)]
            for k in range(4):
                c = c4 * 4 + k
                kb.op("pe", lambda: pe.transpose(pt.t[:, k, :], hn2.t[:, c * 128:(c + 1) * 128], C["ident_f"].t[:]),
                      reads=[hn2, C["ident_f"]], writes=[pt], signal=(k == 3))
            kb.op("dve" if c4 % 2 == 0 else "act",
                  (lambda: dve.tensor_copy(out=hn2T.t[:, c4 * 4:(c4 + 1) * 4, :], in_=pt.t[:])) if c4 % 2 == 0 else
                  (lambda: act.activation(out=hn2T.t[:, c4 * 4:(c4 + 1) * 4, :], in_=pt.t[:], func=AF.Copy)),
                  reads=[pt], writes=[hn2T])
        for c in range(DC):
            kb.op("pe", lambda: pe.matmul(ps_lg.t[:], hn2T.t[:, c, :], wr.t[:, c, :], start=(c == 0), stop=(c == DC - 1)),
                  reads=[hn2T, wr], writes=[ps_lg], signal=(c == DC - 1))
        V = lambda n: sm[n]
        dv = lambda fn, r, w: kb.op("dve", fn, reads=r, writes=w)
        dv(lambda: dve.tensor_tensor(out=L.t[:], in0=ps_lg.t[:], in1=br_bc.t[:], op=ALU.add), [ps_lg, br_bc], [L])
        dv(lambda: dve.reduce_max(out=V("gmax").t[:], in_=L.t[:, 0:NG], axis=AX.X), [L], [V("gmax")])
        dv(lambda: dve.tensor_scalar(out=V("gmask").t[:], in0=L.t[:, 0:NG], scalar1=V("gmax").t[:, 0:1], scalar2=None, op0=ALU.is_ge),
           [L, V("gmax")], [V("gmask")])
        dv(lambda: dve.tensor_scalar(out=V("ngmax").t[:], in0=V("gmax").t[:], scalar1=-1.0, scalar2=None, op0=ALU.mult), [V("gmax")], [V("ngmax")])
        kb.op("act", lambda: act.activation(out=V("eg").t[:], in_=L.t[:, 0:NG], func=AF.Exp, bias=V("ngmax").t[:, 0:1]),
              reads=[L, V("ngmax")], writes=[V("eg")])
        dv(lambda: dve.reduce_sum(out=V("sg").t[:], in_=V("eg").t[:], axis=AX.X), [V("eg")], [V("sg")])
        dv(lambda: dve.reciprocal(out=V("pg").t[:], in_=V("sg").t[:]), [V("sg")], [V("pg")])
        dv(lambda: dve.tensor_scalar(out=V("pen").t[:], in0=V("gmask").t[:], scalar1=BIG, scalar2=-BIG, op0=ALU.mult, op1=ALU.add),
           [V("gmask")], [V("pen")])
        for g in range(NG):
            dv(lambda: dve.tensor_scalar(out=V("Lm").t[:, g * EPG:(g + 1) * EPG], in0=L.t[:, NG + g * EPG:NG + (g + 1) * EPG],
                                         scalar1=V("pen").t[:, g:g + 1], scalar2=None, op0=ALU.add), [L, V("pen")], [V("Lm")])
        dv(lambda: dve.reduce_max(out=V("m1").t[:], in_=V("Lm").t[:], axis=AX.X), [V("Lm")], [V("m1")])
        dv(lambda: dve.tensor_scalar(out=V("mask1").t[:], in0=V("Lm").t[:], scalar1=V("m1").t[:, 0:1], scalar2=None, op0=ALU.is_ge),
           [V("Lm"), V("m1")], [V("mask1")])
        dv(lambda: dve.scalar_tensor_tensor(out=V("Lm2").t[:], in0=V("mask1").t[:], scalar=-BIG, in1=V("Lm").t[:], op0=ALU.mult, op1=ALU.add),
           [V("mask1"), V("Lm")], [V("Lm2")])
        dv(lambda: dve.reduce_max(out=V("m2").t[:], in_=V("Lm2").t[:], axis=AX.X), [V("Lm2")], [V("m2")])
        dv(lambda: dve.tensor_scalar(out=V("mask2").t[:], in0=V("Lm2").t[:], scalar1=V("m2").t[:, 0:1], scalar2=None, op0=ALU.is_ge),
           [V("Lm2"), V("m2")], [V("mask2")])
        if i >= NT:
            dv(lambda: dve.tensor_scalar(out=V("mask1").t[:], in0=V("mask1").t[:], scalar1=C["rowvalid"].t[:, 0:1], scalar2=None, op0=ALU.mult),
               [V("mask1"), C["rowvalid"]], [V("mask1")])
            dv(lambda: dve.tensor_scalar(out=V("mask2").t[:], in0=V("mask2").t[:], scalar1=C["rowvalid"].t[:, 0:1], scalar2=None, op0=ALU.mult),
               [V("mask2"), C["rowvalid"]], [V("mask2")])
        dv(lambda: dve.tensor_tensor(out=V("dd").t[:], in0=V("m2").t[:], in1=V("m1").t[:], op=ALU.subtract), [V("m2"), V("m1")], [V("dd")])
        kb.op("act", lambda: act.activation(out=V("e2").t[:], in_=V("dd").t[:], func=AF.Exp), reads=[V("dd")], writes=[V("e2")])
        dv(lambda: dve.tensor_scalar(out=V("den").t[:], in0=V("e2").t[:], scalar1=1.0, scalar2=None, op0=ALU.add), [V("e2")], [V("den")])
        dv(lambda: dve.reciprocal(out=V("den").t[:], in_=V("den").t[:]), [V("den")], [V("den")])
        dv(lambda: dve.tensor_tensor(out=GATE.t[:, i, 0:1], in0=V("pg").t[:], in1=V("den").t[:], op=ALU.mult), [V("pg"), V("den")], [GATE])
        dv(lambda: dve.tensor_tensor(out=GATE.t[:, i, 1:2], in0=V("pg").t[:], in1=GATE.t[:, i, 0:1], op=ALU.subtract), [V("pg"), GATE], [GATE])
        msum = msums[i % 2]
        dv(lambda: dve.tensor_tensor(out=msum.t[:], in0=V("mask1").t[:], in1=V("mask2").t[:], op=ALU.add), [V("mask1"), V("mask2")], [msum])
        dv(lambda: dve.tensor_copy(out=mk1s[i % 2].t[:], in_=V("mask1").t[:]), [V("mask1")], [mk1s[i % 2]])
        dv(lambda: dve.tensor_copy(out=mk2s[i % 2].t[:], in_=V("mask2").t[:]), [V("mask2")], [mk2s[i % 2]])

    def p3_stage2b(i):
        msum = msums[i % 2]
        mkk = {"mask1": mk1s[i % 2], "mask2": mk2s[i % 2]}
        hb16 = hn2b[i % 3]
        V = lambda n: sm[n]
        dv = lambda fn, r, w: kb.op("dve", fn, reads=r, writes=w)
        kb.op("pe", lambda: pe.matmul(ps_pos.t[:], C["tri_strict_bf"].t[:], msum.t[:], start=True, stop=False),
              reads=[C["tri_strict_bf"], msum], writes=[ps_pos], signal=False)
        kb.op("pe", lambda: pe.matmul(ps_pos.t[:], C["ones_bf"].t[:], cntacc.t[:], start=False, stop=True),
              reads=[C["ones_bf"], cntacc], writes=[ps_pos])
        dv(lambda: dve.tensor_scalar(out=V("valid").t[:], in0=ps_pos.t[:], scalar1=float(CAP), scalar2=None, op0=ALU.is_lt), [ps_pos], [V("valid")])
        dv(lambda: dve.tensor_tensor(out=V("sp").t[:], in0=ps_pos.t[:], in1=C["ebase_f"].t[:], op=ALU.add), [ps_pos, C["ebase_f"]], [V("sp")])
        dv(lambda: dve.scalar_tensor_tensor(out=V("sp").t[:], in0=V("sp").t[:], scalar=-TRASH, in1=V("valid").t[:], op0=ALU.add, op1=ALU.mult),
           [V("sp"), V("valid")], [V("sp")])
        dv(lambda: dve.tensor_scalar(out=V("sp").t[:], in0=V("sp").t[:], scalar1=TRASH, scalar2=None, op0=ALU.add), [V("sp")], [V("sp")])
        for k, mk in ((0, "mask1"), (1, "mask2")):
            dv(lambda: dve.tensor_tensor(out=V("t1").t[:], in0=mkk[mk].t[:], in1=V("sp").t[:], op=ALU.mult), [mkk[mk], V("sp")], [V("t1")])
            dv(lambda: dve.reduce_sum(out=V("s1").t[:], in_=V("t1").t[:], axis=AX.X), [V("t1")], [V("s1")])
            if i >= NT:
                dv(lambda: dve.tensor_tensor(out=V("s1").t[:], in0=V("s1").t[:], in1=C["trash_p"].t[:], op=ALU.add), [V("s1"), C["trash_p"]], [V("s1")])
            dv(lambda: dve.tensor_copy(out=SLOT.t[:, i, k:k + 1], in_=V("s1").t[:]), [V("s1")], [SLOT])
        dv(lambda: dve.tensor_tensor(out=cntacc.t[:], in0=cntacc.t[:], in1=msum.t[:], op=ALU.add), [cntacc, msum], [cntacc])
        for k in range(2):
            kb.dma("pool", None, None, reads=[hb16, SLOT], writes=[XS_b],
                   fn=lambda: pool.indirect_dma_start(out=XS[:, :], out_offset=bass.IndirectOffsetOnAxis(ap=SLOT.t[:, i, k:k + 1], axis=0),
                                                      in_=hb16.t[:], in_offset=None))

    load3(0)
    cnt_f = kb.sb("cnt_f", [128, NE], F32)
    for it in range(NTOK_T + 2):
        if it < NTOK_T:
            p3_stage1(it)
        if 0 <= it - 2 < NTOK_T:
            p3_stage2b(it - 2)
        if 0 <= it - 1 < NTOK_T:
            p3_stage2(it - 1)
        if it < NTOK_T:
            p3_stage1b(it)
    kb.op("dve", lambda: dve.tensor_copy(out=cnt_f.t[:], in_=cntacc.t[:]), reads=[cntacc], writes=[cnt_f])
    kb.dma("sp", o_cnt[:, :], cnt_f.t[:], reads=[cnt_f])
    kb.end_phase()
    if stop_after <= 3:
        kb.finish()
        return nc, consts


    kb.begin_phase()
    XS_b = kb.dram("XS4", XS)
    YS_b = kb.dram("YS4", YS)
    nblk = CAP // 128
    FC = DEXP // 128
    xr = [kb.sb(f"xr{i}", [128, nblk, D], BF16) for i in range(2)]
    xsT = kb.sb("xsT", [128, DC, CAP], BF16)
    NU = 5
    wu = [kb.sb(f"wu{i}", [128, 8192], BF16) for i in range(NU)]
    gTt = kb.sb("gTt", [128, FC, CAP], BF16)
    s1t = [kb.sb(f"s1t{i}", [128, CAP], F32) for i in range(2)]
    ysb = [kb.sb(f"ysb{i}", [128, nblk, D], BF16) for i in range(2)]
    ps_a = [kb.ps(f"ps_a{i}", [128, CAP], F32) for i in range(2)]
    ps_b = [kb.ps(f"ps_b{i}", [128, CAP], F32) for i in range(2)]
    ps_y = [kb.ps(f"ps_y{i}", [128, 512], F32) for i in range(2)]
    ps_x = [kb.ps(f"ps_x{i}", [128, 4, 128], BF16) for i in range(2)]
    cy4 = {"a": 0, "y": 0, "x": 0, "s": 0}

    def cyc4(k, n):
        v = cy4[k]
        cy4[k] = (v + 1) % n
        return v

    ujobs = []
    for e in range(NE):
        for hf in range(2):
            ujobs.append(("w1", e, hf))
            ujobs.append(("w3", e, hf))
        for dmh in range(2):
            ujobs.append(("w2", e, dmh))
    ubuf_of = {}

    def issue_u(k):
        if k >= len(ujobs) or k in ubuf_of:
            return
        kind, e, hh = ujobs[k]
        ub = wu[k % NU]
        ubuf_of[k] = ub
        if kind in ("w1", "w3"):
            src = (w1 if kind == "w1" else w3)[e].rearrange("(c p) f -> p c f", p=128)[:, :, hh * 512:(hh + 1) * 512]
            dst = ub.t[:, :].rearrange("p (c f) -> p c f", c=DC)
        else:
            src = w2[e].rearrange("(c p) n -> p c n", p=128)[:, :, hh * 1024:(hh + 1) * 1024]
            dst = ub.t[:, :].rearrange("p (c n) -> p c n", c=FC)
        kb.dma("pool", dst, src, writes=[ub])

    def load_xr(e):
        if e < NE:
            kb.dma("sp", xr[e % 2].t[:], XS[e * CAP:(e + 1) * CAP, :].rearrange("(b p) d -> p b d", p=128), reads=[XS_b], writes=[xr[e % 2]])

    uk = 0
    for k in range(4):
        issue_u(k)
    load_xr(0)
    for e in range(NE):
        load_xr(e + 1)
        xrb = xr[e % 2]
        for b in range(nblk):
            for c4 in range(DC // 4):
                pt = ps_x[cyc4("x", 2)]
                for k in range(4):
                    c = c4 * 4 + k
                    kb.op("pe", lambda: pe.transpose(pt.t[:, k, :], xrb.t[:, b, c * 128:(c + 1) * 128], C["ident_bf"].t[:]),
                          reads=[xrb, C["ident_bf"]], writes=[pt], signal=(k == 3))
                if (b * 4 + c4) % 2 == 0:
                    kb.op("dve", lambda: dve.tensor_copy(out=xsT.t[:, c4 * 4:(c4 + 1) * 4, b * 128:(b + 1) * 128], in_=pt.t[:]), reads=[pt], writes=[xsT])
                else:
                    kb.op("act", lambda: act.activation(out=xsT.t[:, c4 * 4:(c4 + 1) * 4, b * 128:(b + 1) * 128], in_=pt.t[:], func=AF.Copy),
                          reads=[pt], writes=[xsT])
        for hf in range(2):
            issue_u(uk + 2); issue_u(uk + 3); issue_u(uk + 4)
            u1, u3 = ubuf_of[uk], ubuf_of[uk + 1]
            uk += 2
            u1v = u1.t[:, :].rearrange("p (c f) -> p c f", c=DC)
            u3v = u3.t[:, :].rearrange("p (c f) -> p c f", c=DC)
            for fl_ in range(4):
                fc = hf * 4 + fl_
                i_ = cyc4("a", 2)
                pa, pb = ps_a[i_], ps_b[i_]
                for c in range(DC):
                    kb.op("pe", lambda: pe.matmul(pa.t[:], u1v[:, c, fl_ * 128:(fl_ + 1) * 128], xsT.t[:, c, :], start=(c == 0), stop=(c == DC - 1)),
                          reads=[u1, xsT], writes=[pa], signal=(c == DC - 1))
                for c in range(DC):
                    kb.op("pe", lambda: pe.matmul(pb.t[:], u3v[:, c, fl_ * 128:(fl_ + 1) * 128], xsT.t[:, c, :], start=(c == 0), stop=(c == DC - 1)),
                          reads=[u3, xsT], writes=[pb], signal=(c == DC - 1))
                st_ = s1t[cyc4("s", 2)]
                kb.op("act", lambda: act.activation(out=st_.t[:], in_=pa.t[:], func=AF.Silu), reads=[pa], writes=[st_])
                kb.op("dve", lambda: dve.tensor_tensor(out=gTt.t[:, fc, :], in0=pb.t[:], in1=st_.t[:], op=ALU.mult), reads=[pb, st_], writes=[gTt])
        yb = ysb[e % 2]
        for dmh in range(2):
            issue_u(uk + 1); issue_u(uk + 2); issue_u(uk + 3); issue_u(uk + 4)
            u2 = ubuf_of[uk]
            uk += 1
            u2v = u2.t[:, :].rearrange("p (c n) -> p c n", c=FC)
            for b in range(nblk):
                for dn in range(2):
                    py = ps_y[cyc4("y", 2)]
                    for fc in range(FC):
                        kb.op("pe", lambda: pe.matmul(py.t[:], gTt.t[:, fc, b * 128:(b + 1) * 128], u2v[:, fc, dn * 512:(dn + 1) * 512],
                                                       start=(fc == 0), stop=(fc == FC - 1)),
                              reads=[gTt, u2], writes=[py], signal=(fc == FC - 1))
                    col = dmh * 1024 + dn * 512
                    if (b + dn) % 2 == 0:
                        kb.op("act", lambda: act.activation(out=yb.t[:, b, col:col + 512], in_=py.t[:], func=AF.Copy), reads=[py], writes=[yb])
                    else:
                        kb.op("dve", lambda: dve.tensor_copy(out=yb.t[:, b, col:col + 512], in_=py.t[:]), reads=[py], writes=[yb])
        kb.dma("sp", YS[e * CAP:(e + 1) * CAP, :].rearrange("(b p) d -> p b d", p=128), yb.t[:], reads=[yb], writes=[YS_b])
    kb.end_phase()
    if stop_after <= 4:
        kb.finish()
        return nc, consts

    kb.begin_phase()
    Hs_b = kb.dram("Hs5", Hs)
    YS_b = kb.dram("YS5", YS)
    gf_bc = kb.sb("gf_bc", [128, D], F32)
    kb.dma("sp", gf_bc.t[:], bcast_row(g_fin[0:1, :], D), writes=[gf_bc])
    h5 = [kb.sb(f"h5_{i}", [128, D], F32) for i in range(2)]
    yg = [[kb.sb(f"yg{i}_{k}", [128, D], BF16) for k in range(2)] for i in range(2)]
    t5 = kb.sb("t5", [128, D], F32)
    sq5 = kb.sb("sq5", [128, D], F32)
    ssq5 = kb.sb("ssq5", [128, 1], F32)
    rstd5 = kb.sb("rstd5", [128, 1], F32)
    yo = [kb.sb(f"yo{i}", [128, D], F32) for i in range(2)]

    def load5(i):
        kb.dma("sp", h5[i % 2].t[:], Hs[i * 128:(i + 1) * 128, :], reads=[Hs_b], writes=[h5[i % 2]])
        for k in range(2):
            yb_ = yg[i % 2][k]
            kb.dma("pool", None, None, reads=[YS_b, SLOT], writes=[yb_],
                   fn=lambda: pool.indirect_dma_start(out=yb_.t[:], out_offset=None, in_=YS[:, :],
                                                      in_offset=bass.IndirectOffsetOnAxis(ap=SLOT.t[:, i, k:k + 1], axis=0)))

    load5(0)
    for i in range(NTOK_T):
        if i + 1 < NTOK_T:
            load5(i + 1)
        hb, y1, y2, ob = h5[i % 2], yg[i % 2][0], yg[i % 2][1], yo[i % 2]
        kb.op("dve", lambda: dve.scalar_tensor_tensor(out=t5.t[:], in0=y1.t[:], scalar=GATE.t[:, i, 0:1], in1=hb.t[:], op0=ALU.mult, op1=ALU.add),
              reads=[y1, GATE, hb], writes=[t5])
        kb.op("dve", lambda: dve.scalar_tensor_tensor(out=t5.t[:], in0=y2.t[:], scalar=GATE.t[:, i, 1:2], in1=t5.t[:], op0=ALU.mult, op1=ALU.add),
              reads=[y2, GATE, t5], writes=[t5])
        kb.op("act", lambda: act.activation(out=sq5.t[:], in_=t5.t[:], func=AF.Square), reads=[t5], writes=[sq5])
        kb.op("dve", lambda: dve.reduce_sum(out=ssq5.t[:], in_=sq5.t[:], axis=AX.X), reads=[sq5], writes=[ssq5])
        kb.op("dve", lambda: dve.tensor_scalar(out=rstd5.t[:], in0=ssq5.t[:], scalar1=1.0 / D, scalar2=EPS, op0=ALU.mult, op1=ALU.add),
              reads=[ssq5], writes=[rstd5])
        kb.op("act", lambda: act.activation(out=rstd5.t[:], in_=rstd5.t[:], func=AF.Sqrt), reads=[rstd5], writes=[rstd5])
        kb.op("dve", lambda: dve.reciprocal(out=rstd5.t[:], in_=rstd5.t[:]), reads=[rstd5], writes=[rstd5])
        kb.op("dve", lambda: dve.scalar_tensor_tensor(out=ob.t[:], in0=t5.t[:], scalar=rstd5.t[:, 0:1], in1=gf_bc.t[:], op0=ALU.mult, op1=ALU.mult),
              reads=[t5, rstd5, gf_bc], writes=[ob])
        if i < NT:
            kb.dma("sp", y_p[i * 128:(i + 1) * 128, :], ob.t[:], reads=[ob])
        else:
            kb.dma("sp", y_s[:, :], ob.t[0:T, :], reads=[ob])
    kb.end_phase()
    kb.finish()
    return nc, consts


def make_in_maps(inp, cfg, n_cores):
    consts = make_consts(cfg)
    f = lambda a: np.ascontiguousarray(np.asarray(a, dtype=np.float32))
    w_rt = np.concatenate([np.asarray(inp["w_group"][0]), np.asarray(inp["w_router"][0])], axis=1)
    b_rt = np.concatenate([np.asarray(inp["b_group"][0]), np.asarray(inp["b_router"][0])], axis=0)[None, :]
    shared = dict(
        w_in=f(inp["w_in"][0]), b_f=f(inp["b_f"]), g_attn=f(inp["g_attn"]), g_of=f(inp["g_out_fox"]),
        g_os=f(inp["g_out_sb"]), w_out=f(inp["w_out"][0]), g_ffn=f(inp["g_ffn"]), w_rt=f(w_rt), b_rt=f(b_rt),
        w1=f(inp["w1"][0]), w3=f(inp["w3"][0]), w2=f(inp["w2"][0]), g_fin=f(np.asarray(inp["g_final"])[None, :]),
    )
    for k, v in consts.items():
        shared["k_" + k] = v
    maps = []
    for b in range(n_cores):
        m = dict(shared)
        m["x_p"] = f(inp["x_prompt"][b])
        m["x_s"] = f(inp["x_sample"][b])
        m["c_fk"] = f(np.asarray(inp["cache_fox_k"][0, b]).reshape(cfg["PAST"], 1024))
        m["c_fv"] = f(np.asarray(inp["cache_fox_v"][0, b]).reshape(cfg["PAST"], 1024))
        m["c_fl"] = f(inp["cache_fox_logf"][0, b])
        m["c_sk"] = f(np.asarray(inp["cache_sb_k"][0, b]).reshape(cfg["PAST"], 1024))
        m["c_sv"] = f(np.asarray(inp["cache_sb_v"][0, b]).reshape(cfg["PAST"], 1024))
        maps.append(m)
    return maps


def assemble(results, cfg):
    S, T = cfg["S"], cfg["T"]
    B = len(results)
    st = lambda k, shp: np.stack([np.asarray(r[k]).reshape(shp) for r in results], axis=0)
    y_p = st("y_p", (S, D)); y_s = st("y_s", (T, D))
    outs = [y_p, y_s]
    for pre, n in (("o_p", S), ("o_s", T)):
        outs.append(st(pre + "fk", (n, 8, 128))[None])
        outs.append(st(pre + "fv", (n, 8, 128))[None])
        outs.append(st(pre + "fl", (n, 8))[None])
        outs.append(st(pre + "sk", (n, 8, 128))[None])
        outs.append(st(pre + "sv", (n, 8, 128))[None])
    return tuple(np.ascontiguousarray(o.astype(np.float32)) for o in outs)


def kernel(**inputs):
    cfg = default_cfg()
    nc, _ = build(cfg)
    maps = make_in_maps(inputs, cfg, 8)
    res = run_bass_kernel_spmd(nc, maps, core_ids=list(range(8)))
    try:
        import os
        if os.environ.get("MK_DEBUG_CNT"):
            np.save(os.environ["MK_DEBUG_CNT"], np.stack([np.asarray(r["o_cnt"]).sum(0) for r in res.results]))
    except Exception:
        pass
    return assemble(res.results, cfg)
```
